# Optimizing a Trainium2 kernel written in Bass

```python
import jax, jax.numpy as jnp
from jax import lax
import numpy as np

D_MODEL = 1024
BATCH = 8
SEQ = 2048
DEPTH = 1

GRID_W = 64
CTX_LEN = 256
D_MIX = D_MODEL
SGU_HEADS = 4
SGU_HEAD_DIM = 128
SGU_WIDTH = SGU_HEADS * SGU_HEAD_DIM
SGU_CHUNK = 2 * GRID_W
HGRN_HEADS = 4
HGRN_HEAD_DIM = 128
HGRN_WIDTH = HGRN_HEADS * HGRN_HEAD_DIM
HGRN_CHUNK = 64
N_PROJ = 7
D_IN = N_PROJ * SGU_WIDTH
N_EXPERTS = 32
TOP_K = 4
D_EXPERT = 1024
SWIGLU_LIMIT = 7.0
SWIGLU_ALPHA = 1.702
MOE_BLOCK = 128
EPS = 1e-6

kernel_name = "hybrid_sgu_hgrn2_moe_dit_block"


def rms_norm(x, g):
    xf = x.astype(jnp.float32)
    y = xf * lax.rsqrt(jnp.mean(xf * xf, axis=-1, keepdims=True) + EPS)
    return (y * g.astype(jnp.float32)).astype(x.dtype)


def modulate(h, shift, scale):
    return h * (1.0 + scale) + shift


def flip(a):
    return jnp.flip(a, axis=1)


def lower_bound(table, layer):
    return jnp.cumsum(jax.nn.softmax(table.astype(jnp.float32), axis=0), axis=0)[layer]


def chunk_sgu(u, v, ln_g, w_s, b_s):
    bsz, n, _ = u.shape
    u = jax.nn.gelu(u)
    v = jax.nn.gelu(v)
    vh = v.reshape(bsz, n // SGU_CHUNK, SGU_CHUNK, SGU_HEADS, SGU_HEAD_DIM).astype(jnp.float32)
    mu = jnp.mean(vh, axis=-1, keepdims=True)
    var = jnp.mean(jnp.square(vh - mu), axis=-1, keepdims=True)
    vn = ((vh - mu) * lax.rsqrt(var + EPS) * ln_g.astype(jnp.float32)).astype(v.dtype)
    z = jnp.einsum('hts,bnshd->bnthd', w_s, vn) + b_s.T[:, :, None]
    return u * z.reshape(bsz, n, SGU_WIDTH)


def hgrn_chunk_scan(q, k, v, log_f, s0):
    bsz, n, nh, dk = q.shape
    n_chunks = n // HGRN_CHUNK

    def to_chunks(a):
        return a.reshape(bsz, n_chunks, HGRN_CHUNK, nh, a.shape[-1]).transpose(1, 0, 3, 2, 4)

    mask = jnp.tril(jnp.ones((HGRN_CHUNK, HGRN_CHUNK), dtype=bool))[:, :, None]

    def step(s, inp):
        qc, kc, vc, gc = inp
        b = jnp.cumsum(gc, axis=2)
        diff = b[:, :, :, None, :] - b[:, :, None, :, :]
        decay = jnp.exp(jnp.where(mask, diff, -jnp.inf))
        scores = jnp.einsum('bhtk,bhsk,bhtsk->bhts', qc, kc, decay)
        o = jnp.einsum('bhts,bhsv->bhtv', scores, vc) \
            + jnp.einsum('bhtk,bhkv->bhtv', qc * jnp.exp(b), s)
        b_last = b[:, :, -1:, :]
        s_new = jnp.exp(b_last[:, :, 0, :])[..., None] * s \
            + jnp.einsum('bhsk,bhsv->bhkv', kc * jnp.exp(b_last - b), vc)
        return s_new, o

    s_fin, o = lax.scan(step, s0, (to_chunks(q), to_chunks(k), to_chunks(v), to_chunks(log_f)))
    o = o.transpose(1, 0, 3, 2, 4).reshape(bsz, n, nh, v.shape[-1])
    return o, s_fin


def hgrn_direction(q, i, z, lb, s0):
    bsz, n, _ = q.shape
    f = lb + (1.0 - lb) * jax.nn.sigmoid(z.astype(jnp.float32))
    k = 1.0 - f
    sh = (bsz, n, HGRN_HEADS, HGRN_HEAD_DIM)
    return hgrn_chunk_scan(q.astype(jnp.float32).reshape(sh), k.reshape(sh),
                           i.astype(jnp.float32).reshape(sh), jnp.log(f).reshape(sh), s0)


def merge_heads(u, v, o, g, sgu_ln_l, sgu_w_l, sgu_b_l, hgrn_norm_l, w_out_l):
    y_a = chunk_sgu(u, v, sgu_ln_l, sgu_w_l, sgu_b_l)
    bsz, n = o.shape[:2]
    on = o * lax.rsqrt(jnp.mean(o * o, axis=-1, keepdims=True) + EPS)
    y_b = (on.reshape(bsz, n, HGRN_WIDTH) * hgrn_norm_l.astype(jnp.float32)).astype(g.dtype) \
        * jax.nn.silu(g)
    return jnp.concatenate([y_a, y_b], axis=-1) @ w_out_l


def moe(h, router_w, router_b, w1, b1, w2, b2):
    bsz, n, d = h.shape
    n_tok = bsz * n
    hf = h.reshape(n_tok, d)
    logits = (hf @ router_w + router_b).astype(jnp.float32)
    top_val, top_idx = lax.top_k(logits, TOP_K)
    gate_w = jax.nn.softmax(top_val, axis=-1)
    n_assign = n_tok * TOP_K
    e_flat = top_idx.reshape(-1)
    tok_flat = jnp.repeat(jnp.arange(n_tok, dtype=jnp.int32), TOP_K)
    w_flat = gate_w.reshape(-1)
    order = jnp.argsort(e_flat)
    e_s, tok_s, w_s = e_flat[order], tok_flat[order], w_flat[order]
    counts = jnp.bincount(e_flat, length=N_EXPERTS)
    padded = (counts + MOE_BLOCK - 1) // MOE_BLOCK * MOE_BLOCK
    start = jnp.cumsum(counts) - counts
    pend = jnp.cumsum(padded)
    pstart = pend - padded
    dest = pstart[e_s] + jnp.arange(n_assign, dtype=jnp.int32) - start[e_s]
    n_blocks = -(-(n_assign + N_EXPERTS * (MOE_BLOCK - 1)) // MOE_BLOCK)
    n_rows = n_blocks * MOE_BLOCK
    row_tok = jnp.zeros((n_rows,), jnp.int32).at[dest].set(tok_s)
    row_w = jnp.zeros((n_rows,), jnp.float32).at[dest].set(w_s)
    block_start = jnp.arange(n_blocks, dtype=jnp.int32) * MOE_BLOCK
    block_e = jnp.minimum(jnp.sum(block_start[:, None] >= pend[None, :], axis=1), N_EXPERTS - 1)

    def expert_block(args):
        toks, e = args
        xb = hf[toks]
        gu = xb @ w1[e] + b1[e]
        gate, up = jnp.split(gu, 2, axis=-1)
        gate = jnp.minimum(gate, SWIGLU_LIMIT)
        up = jnp.clip(up, -SWIGLU_LIMIT, SWIGLU_LIMIT)
        act = gate * jax.nn.sigmoid(SWIGLU_ALPHA * gate) * (up + 1.0)
        return act @ w2[e] + b2[e]

    out = lax.map(expert_block, (row_tok.reshape(n_blocks, MOE_BLOCK), block_e))
    out = out.reshape(n_rows, d) * row_w[:, None].astype(h.dtype)
    y = jnp.zeros_like(hf).at[row_tok].add(out)
    return y.reshape(bsz, n, d)


def setup_inputs(seed: int = 0) -> dict:
    key = jax.random.key(seed)
    ks = jax.random.split(key, 24)
    nrm = jax.random.normal
    f32 = jnp.float32
    d = D_MODEL
    return {
        "x": nrm(ks[0], (BATCH, SEQ, d), f32),
        "c": nrm(ks[1], (BATCH, d), f32),
        "ctx": nrm(ks[2], (BATCH, CTX_LEN, d), f32),
        "c_ctx": nrm(ks[3], (d,), f32),
        "w_mod": nrm(ks[4], (DEPTH, d, 6 * d), f32) * (0.5 * d ** -0.5),
        "b_mod": nrm(ks[5], (DEPTH, 6 * d), f32) * 0.02,
        "norm1": 1.0 + 0.02 * nrm(ks[6], (DEPTH, d), f32),
        "w_in": nrm(ks[7], (DEPTH, d, D_IN), f32) * d ** -0.5,
        "sgu_ln": 1.0 + 0.02 * nrm(ks[8], (DEPTH, SGU_HEADS, SGU_HEAD_DIM), f32),
        "sgu_w": nrm(ks[9], (DEPTH, SGU_HEADS, SGU_CHUNK, SGU_CHUNK), f32) * SGU_CHUNK ** -0.5,
        "sgu_b": 1.0 + 0.02 * nrm(ks[10], (DEPTH, SGU_HEADS, SGU_CHUNK), f32),
        "lb_fwd": nrm(ks[11], (DEPTH + 1, HGRN_WIDTH), f32),
        "lb_bwd": nrm(ks[12], (DEPTH + 1, HGRN_WIDTH), f32),
        "hgrn_norm": 1.0 + 0.02 * nrm(ks[13], (DEPTH, HGRN_WIDTH), f32),
        "w_out": nrm(ks[14], (DEPTH, D_MIX, d), f32) * D_MIX ** -0.5,
        "norm2": 1.0 + 0.02 * nrm(ks[15], (DEPTH, d), f32),
        "router_w": nrm(ks[16], (DEPTH, d, N_EXPERTS), f32) * d ** -0.5,
        "router_b": nrm(ks[17], (DEPTH, N_EXPERTS), f32) * 0.01,
        "w1": nrm(ks[18], (DEPTH, N_EXPERTS, d, 2 * D_EXPERT), f32) * d ** -0.5,
        "b1": nrm(ks[19], (DEPTH, N_EXPERTS, 2 * D_EXPERT), f32) * 0.01,
        "w2": nrm(ks[20], (DEPTH, N_EXPERTS, D_EXPERT, d), f32) * D_EXPERT ** -0.5,
        "b2": nrm(ks[21], (DEPTH, N_EXPERTS, d), f32) * 0.01,
        "final_norm": 1.0 + 0.02 * nrm(ks[22], (d,), f32),
    }


def reference(x, c, ctx, c_ctx, w_mod, b_mod, norm1, w_in, sgu_ln, sgu_w, sgu_b,
              lb_fwd, lb_bwd, hgrn_norm, w_out, norm2, router_w, router_b,
              w1, b1, w2, b2, final_norm):
    bsz = x.shape[0]
    s_zero = jnp.zeros((bsz, HGRN_HEADS, HGRN_HEAD_DIM, HGRN_HEAD_DIM), jnp.float32)
    x_ctx = ctx
    for l in range(DEPTH):
        mod = jax.nn.silu(c) @ w_mod[l] + b_mod[l]
        sh1, sc1, g1, sh2, sc2, g2 = jnp.split(mod[:, None, :], 6, axis=-1)
        mod_c = jax.nn.silu(c_ctx) @ w_mod[l] + b_mod[l]
        csh1, csc1, cg1, csh2, csc2, cg2 = jnp.split(mod_c, 6, axis=-1)
        lbf = lower_bound(lb_fwd, l)
        lbb = lower_bound(lb_bwd, l)

        hc = modulate(rms_norm(x_ctx, norm1[l]), csh1, csc1)
        uc, vc, qc, zfc, zbc, ic, gc = jnp.split(hc @ w_in[l], N_PROJ, axis=-1)
        oc_f, st_f = hgrn_direction(qc, ic, zfc, lbf, s_zero)
        oc_b, st_b = hgrn_direction(flip(qc), flip(ic), flip(zbc), lbb, s_zero)

        h = modulate(rms_norm(x, norm1[l]), sh1, sc1)
        u, v, q, zf, zb, i, g = jnp.split(h @ w_in[l], N_PROJ, axis=-1)
        o_f, _ = hgrn_direction(q, i, zf, lbf, st_f)
        o_b, _ = hgrn_direction(flip(q), flip(i), flip(zb), lbb, st_b)
        y = merge_heads(u, v, o_f + flip(o_b), g, sgu_ln[l], sgu_w[l], sgu_b[l],
                        hgrn_norm[l], w_out[l])
        x = x + g1 * y
        h2 = modulate(rms_norm(x, norm2[l]), sh2, sc2)
        x = x + g2 * moe(h2, router_w[l], router_b[l], w1[l], b1[l], w2[l], b2[l])

        if l < DEPTH - 1:
            yc = merge_heads(uc, vc, oc_f + flip(oc_b), gc, sgu_ln[l], sgu_w[l], sgu_b[l],
                             hgrn_norm[l], w_out[l])
            x_ctx = x_ctx + cg1 * yc
            hc2 = modulate(rms_norm(x_ctx, norm2[l]), csh2, csc2)
            x_ctx = x_ctx + cg2 * moe(hc2, router_w[l], router_b[l], w1[l], b1[l], w2[l], b2[l])
    return rms_norm(x, final_norm)
```

```python
import numpy as np
from contextlib import ExitStack
import concourse.bass as bass
import concourse.mybir as mybir
from concourse.bass_utils import run_bass_kernel_spmd

F32 = mybir.dt.float32
BF16 = mybir.dt.bfloat16
AF = mybir.ActivationFunctionType
ALU = mybir.AluOpType
AX = mybir.AxisListType

EPOCH = 8192
NCORES = 8
EPS = 1e-6


class Region:
    __slots__ = ("name", "writers", "readers", "chan", "dma_count")

    def __init__(self, name, chan=None):
        self.name = name
        self.writers = []
        self.readers = []
        self.chan = chan
        self.dma_count = 0


class Op:
    __slots__ = ("eng", "fn", "deps", "idx", "is_dma", "token", "name", "kind", "flag_ap", "ext", "cid", "chain")

    def __init__(self, eng, fn, name=""):
        self.eng = eng
        self.fn = fn
        self.deps = []
        self.idx = None
        self.is_dma = False
        self.token = None
        self.name = name
        self.kind = "op"
        self.flag_ap = None
        self.ext = None
        self.cid = None
        self.chain = False


class Tl:
    __slots__ = ("t", "r")

    def __init__(self, t, r):
        self.t = t
        self.r = r


class Prog:
    ENGS = ("pe", "act", "dve", "pool", "sp")

    def __init__(self, nc, stack):
        self.nc = nc
        self.stack = stack
        self.ops = {e: [] for e in self.ENGS}
        self.count = {e: 0 for e in self.ENGS}
        self.sems = {e: [] for e in self.ENGS}
        self.n_chan = 0
        self.final_tokens = {}
        self.last_dma = {}

    def sem(self, name):
        return self.stack.enter_context(self.nc.semaphore(name))

    def region(self, name, dma=False):
        ch = None
        if dma:
            self.n_chan += 1
            ch = self.sem(f"c{self.n_chan}_{name}")
        return Region(name, ch)

    def tile(self, name, shape, dtype, dma=False, stack=None):
        st = stack or self.stack
        t = st.enter_context(self.nc.sbuf_tensor("sb_" + name, list(shape), dtype))
        return Tl(t, self.region(name, dma))

    def psum(self, name, shape, dtype=F32):
        t = self.stack.enter_context(self.nc.psum_tensor(name, list(shape), dtype))
        return Tl(t, self.region(name))

    def _add(self, eng, fn, reads, writes, name, dma_region=None):
        op = Op(eng, fn, name)
        is_dma = dma_region is not None
        deps = []
        for r in reads:
            deps.extend(r.writers)
        for w in writes:
            deps.extend(w.readers)
            if is_dma and not w.readers and w.writers and all(x.is_dma for x in w.writers):
                pass
            else:
                deps.extend(w.writers)
        seen = set()
        for d in deps:
            if id(d) in seen:
                continue
            seen.add(id(d))
            if d.eng == "pe" and eng == "pe" and not d.is_dma and not is_dma:
                continue
            op.deps.append(d)
        if is_dma:
            op.is_dma = True
            dma_region.dma_count += 16
            op.token = (dma_region.chan, dma_region.dma_count)
            self.final_tokens[id(dma_region.chan)] = op.token
            self.last_dma[id(dma_region.chan)] = op
        else:
            self.count[eng] += 1
            op.idx = self.count[eng]
        for r in reads:
            r.readers.append(op)
        for w in writes:
            if is_dma and not w.readers and w.writers and all(x.is_dma for x in w.writers):
                w.writers.append(op)
            else:
                w.writers = [op]
            w.readers = []
        self.ops[eng].append(op)
        return op

    def op(self, eng, fn, reads=(), writes=(), name=""):
        return self._add(eng, fn, [x.r if isinstance(x, Tl) else x for x in reads],
                         [x.r if isinstance(x, Tl) else x for x in writes], name)

    def dma(self, eng, fn, dst, reads=(), writes=None, name="", serialize=False):
        dst_r = dst.r if isinstance(dst, Tl) else dst
        w = [dst_r] if writes is None else [x.r if isinstance(x, Tl) else x for x in writes]
        rd = [x.r if isinstance(x, Tl) else x for x in reads]
        if serialize:
            rd = rd + [dst_r]
        op = self._add(eng, fn, rd, w, name, dma_region=dst_r)
        op.chain = serialize
        return op

    def barrier(self, dummy):
        lasts = []
        for e in ("pe", "act", "dve", "pool"):
            for o in reversed(self.ops[e]):
                if not o.is_dma and o.kind == "op":
                    lasts.append(o)
                    break
        dmas = list(self.last_dma.values())
        new_ops = []
        for e in ("act", "dve", "pool"):
            if e == "act":
                fn = (lambda en: en.memzero(dummy["act"].t[:]))
            else:
                fn = (lambda en, e=e: en.memset(dummy[e].t[:], 0.0))
            new_ops.append(self.op(e, fn, writes=[dummy[e]]))
        new_ops.append(self.dma("sp", lambda en: en.dma_start(out=dummy["sp"].t[:], in_=dummy["src"]), dummy["sp"]))
        for op in new_ops:
            for d in lasts + dmas:
                if d is not op and all(d is not x for x in op.deps):
                    op.deps.append(d)

    def cond_begin(self, flag_tl, flag_ap):
        if not hasattr(self, "_cstack"):
            self._cstack = []
        self._cstack.append({e: len(self.ops[e]) for e in self.ENGS})
        for e in self.ENGS:
            m = Op(e, None, "cbegin")
            m.kind = "cbegin"
            m.flag_ap = flag_ap
            m.deps = list(flag_tl.r.writers)
            self.ops[e].append(m)

    def cond_end(self):
        cstart = self._cstack.pop()
        body = set()
        for e in self.ENGS:
            for o in self.ops[e][cstart[e] + 1:]:
                body.add(id(o))
        ext_ops = []
        for e in self.ENGS:
            for o in self.ops[e][cstart[e] + 1:]:
                for d in o.deps:
                    if id(d) not in body:
                        only = e if (o.chain and d.chain and d.is_dma and o.is_dma and d.token[0] is o.token[0]) else None
                        ext_ops.append((d, only))
        for e in self.ENGS:
            m = Op(e, None, "cend")
            m.kind = "cend"
            m.ext = ext_ops
            self.ops[e].append(m)

    def _tok(self, d):
        if d.is_dma:
            return d.token
        e = d.eng
        ep = (d.idx - 1) // EPOCH
        return (self.sems[e][ep], (d.idx - 1) % EPOCH + 1)

    def emit(self):
        nc = self.nc
        for e in self.ENGS:
            n_ep = (self.count[e] + EPOCH - 1) // EPOCH
            for i in range(n_ep):
                self.sems[e].append(self.sem(f"s_{e}{i}"))
        prog = self

        def run(e, engine):
            waited = {}

            def emit_op(op):
                need = {}
                for d in op.deps:
                    sem, val = prog._tok(d)
                    k = id(sem)
                    if need.get(k, (sem, 0))[1] < val:
                        need[k] = (sem, val)
                for k, (sem, val) in need.items():
                    if waited.get(k, 0) >= val:
                        continue
                    waited[k] = val
                    engine.wait_ge(sem, val)
                if op.kind != "op":
                    return
                ins = op.fn(engine)
                if op.is_dma:
                    ins.then_inc(op.token[0], 16)
                else:
                    ep = (op.idx - 1) // EPOCH
                    ins.then_inc(prog.sems[e][ep], 1)

            ops = prog.ops[e]

            def emit_range(lo, hi):
                i = lo
                while i < hi:
                    op = ops[i]
                    if op.kind == "cbegin":
                        depth = 1
                        j = i + 1
                        while True:
                            if ops[j].kind == "cbegin":
                                depth += 1
                            elif ops[j].kind == "cend":
                                depth -= 1
                                if depth == 0:
                                    break
                            j += 1
                        real = [b for b in ops[i + 1:j] if b.kind == "op"]
                        if real:
                            emit_op(op)
                            incs = {}
                            for b in real:
                                if b.is_dma:
                                    sem, n = b.token[0], 16
                                else:
                                    sem, n = prog.sems[e][(b.idx - 1) // EPOCH], 1
                                k = id(sem)
                                incs[k] = (sem, incs.get(k, (sem, 0))[1] + n)
                            with engine.register() as freg:
                                engine.reg_load(freg, op.flag_ap)
                                saved = dict(waited)
                                with engine.If_eq(freg, 0):
                                    ext = {}
                                    for d_, only_ in ops[j].ext:
                                        if only_ is not None and only_ != e:
                                            continue
                                        sem_, v_ = prog._tok(d_)
                                        if ext.get(id(sem_), (sem_, 0))[1] < v_:
                                            ext[id(sem_)] = (sem_, v_)
                                    pre = None
                                    for o in reversed(ops[:i]):
                                        if o.kind == "op" and not o.is_dma:
                                            pre = o
                                            break
                                    if pre is not None:
                                        sem, v = prog._tok(pre)
                                        if ext.get(id(sem), (sem, 0))[1] < v:
                                            ext[id(sem)] = (sem, v)
                                    for k, (sem, v) in ext.items():
                                        if waited.get(k, 0) >= v:
                                            continue
                                        engine.wait_ge(sem, v)
                                    for sem, n in incs.values():
                                        engine.sem_inc(sem, n)
                                with engine.Else():
                                    emit_range(i + 1, j)
                                waited.clear()
                                waited.update(saved)
                        i = j + 1
                        continue
                    if op.kind == "cend":
                        i += 1
                        continue
                    emit_op(op)
                    i += 1

            emit_range(0, len(ops))
            if e == "sp":
                for sem, val in prog.final_tokens.values():
                    engine.wait_ge(sem, val)
                for ce in ("pe", "act", "dve", "pool"):
                    c = prog.count[ce]
                    if c:
                        ep = (c - 1) // EPOCH
                        engine.wait_ge(prog.sems[ce][ep], (c - 1) % EPOCH + 1)

        with nc.Block() as block:
            @block.tensor
            def _(eng):
                run("pe", eng)

            @block.scalar
            def _(eng):
                run("act", eng)

            @block.vector
            def _(eng):
                run("dve", eng)

            @block.gpsimd
            def _(eng):
                run("pool", eng)

            @block.sync
            def _(eng):
                run("sp", eng)


def k_mm(lst):
    def f(e):
        ins = None
        for (o, l, r, s, t) in lst:
            ins = e.matmul(o, lhsT=l, rhs=r, start=s, stop=t)
        return ins
    return f


def k_tr(lst):
    def f(e):
        ins = None
        for (o, i, idn) in lst:
            ins = e.transpose(out=o, in_=i, identity=idn)
        return ins
    return f


def k_act(out, in_, func, **kw):
    return lambda e: e.activation(out=out, in_=in_, func=func, **kw)


def k_tt(out, a, b, op):
    return lambda e: e.tensor_tensor(out=out, in0=a, in1=b, op=op)


def k_ts(out, a, s1, op0, s2=None, op1=None):
    if op1 is None:
        return lambda e: e.tensor_scalar(out=out, in0=a, scalar1=s1, scalar2=None, op0=op0)
    return lambda e: e.tensor_scalar(out=out, in0=a, scalar1=s1, scalar2=s2, op0=op0, op1=op1)


def k_stt(out, a, s, b, op0, op1):
    return lambda e: e.scalar_tensor_tensor(out=out, in0=a, scalar=s, in1=b, op0=op0, op1=op1)


def k_cp(out, in_):
    return lambda e: e.tensor_copy(out=out, in_=in_)


def k_acp(out, in_):
    return lambda e: e.copy(out=out, in_=in_)


def k_red(out, in_):
    return lambda e: e.reduce_sum(out=out, in_=in_, axis=AX.X)


def k_rcp(out, in_):
    return lambda e: e.reciprocal(out=out, in_=in_)


def k_dma(out, in_):
    return lambda e: e.dma_start(out=out, in_=in_)


def k_memset(ap, v):
    return lambda e: e.memset(ap, v)


G_U, G_V, G_Q, G_ZF, G_ZB, G_I, G_G = range(7)
NPC = 16 + 48 + 24 + 4 + 512
PC_C, PC_BMOD, PC_N1, PC_N2, PC_FN, PC_LNG, PC_B1 = 0, 16, 64, 72, 80, 88, 92
NPR = 3072
CS_ID, CS_TMF, CS_TMB, CS_XF, CS_XB = 0, 128, 256, 384, 386
CS_LS, CS_PAD, CS_TOK, CS_IOTA = 388, 516, 517, 533
NCST = 533 + 2048


def _consts():
    s = np.arange(128)[:, None]
    t = np.arange(128)[None, :]
    cst = np.zeros((128, NCST), np.float32)
    cst[:, CS_ID:CS_ID + 128] = np.eye(128, dtype=np.float32)
    cst[:, CS_TMF:CS_TMF + 128] = (s <= t).astype(np.float32) - (s <= 63).astype(np.float32)
    cst[:, CS_TMB:CS_TMB + 128] = (s >= t).astype(np.float32) - (s >= 64).astype(np.float32)
    cst[:, CS_XF] = (s[:, 0] >= 64)
    cst[:, CS_XF + 1] = (s[:, 0] <= 63)
    cst[:, CS_XB] = (s[:, 0] <= 63)
    cst[:, CS_XB + 1] = (s[:, 0] >= 64)
    cst[:, CS_LS:CS_LS + 128] = (s < t).astype(np.float32)
    cst[:, CS_IOTA:CS_IOTA + 2048] = np.arange(2048, dtype=np.float32)[None, :]
    cst[:, CS_PAD] = 2048 + np.arange(128)
    cst[:, CS_TOK:CS_TOK + 16] = np.arange(128)[:, None] + 128 * np.arange(16)[None, :]
    mf = np.tile((s <= t).astype(np.float32), (1, 4))
    mb = np.tile((s >= t).astype(np.float32), (1, 4))
    masks = np.concatenate([mf, mb], axis=1)
    sel = np.zeros((32, 32, 128), np.float32)
    for e in range(32):
        sel[e, e, :] = 1.0
    return cst, masks, sel.reshape(32, 4096)


def col_layout(v):
    return np.ascontiguousarray(v.reshape(-1, 128).T)


def build_program(stage=99, dbg_cols=0):
    nc = bass.Bass("TRN2", target_bir_lowering=False)

    def D(name, shape, kind="ExternalInput"):
        return nc.dram_tensor(name, list(shape), F32, kind=kind).ap()

    xT_d = D("xT", [1024, 2048])
    ctxT_d = D("ctxT", [1024, 256])
    pcol_d = D("pcol", [128, NPC])
    prow_d = D("prow", [1, NPR])
    wmod_d = D("w_mod", [1024, 6144])
    win_d = D("w_in", [1024, 3584])
    wout_d = D("w_out", [1024, 1024])
    rw_d = D("router_w", [1024, 32])
    rb_d = D("router_b", [32, 1])
    w1_d = D("w1", [32, 1024, 2048]) if stage >= 2 else None
    w2_d = D("w2", [32, 1024, 1024]) if stage >= 2 else None
    b2_d = D("b2", [32, 1024])
    wsT_d = D("sgu_wT", [128, 512])
    cst_d = D("cst", [128, NCST])
    mask_d = D("masks", [128, 1024])
    sel_d = D("sel", [32, 4096])
    out_d = D("outT", [1024, 2048], kind="ExternalOutput")
    dbg_d = D("dbg", [128, dbg_cols], kind="ExternalOutput") if dbg_cols else None

    with ExitStack() as st:
        P = Prog(nc, st)
        dbg_state = {"off": 0}
        Rdbg = P.region("dbgout", dma=True) if dbg_cols else None
        if dbg_cols:
            dbg_state["dtmp"] = P.tile("dbg_bounce", [128, 512], F32)

        def dump(tl, ap, ncols):
            o = dbg_state["off"]
            P.dma("sp", k_dma(dbg_d[:, o:o + ncols], ap), Rdbg, reads=[tl], writes=[Rdbg])
            dbg_state["off"] = o + ncols

        xT = P.tile("xT", [128, 8, 2048], F32)
        RX = [P.region(f"x{c}") for c in range(16)]
        Rxload = P.region("xload", dma=True)
        pc = P.tile("pc", [128, NPC], F32, dma=True)
        cst = P.tile("cst", [128, CS_IOTA], F32, dma=True)
        identb = P.tile("identb", [128, 128], BF16, dma=True)
        onesb = P.tile("onesb", [128, 128], BF16)
        modc = P.tile("modc", [128, 48, 2], F32)
        mc = P.tile("mc", [128, 8, 8], F32)
        dummy = {e: P.tile(f"dummy_{e}", [128, 8], F32) for e in ("act", "dve", "pool")}
        dummy["sp"] = P.tile("dummy_sp", [128, 8], F32, dma=True)
        dummy["src"] = cst_d[:, 0:8]
        PSB = [P.psum(f"psb{i}", [128, 512], F32) for i in range(8)]
        ps_state = {"i": 0}

        def bank():
            b = PSB[ps_state["i"] % 8]
            ps_state["i"] += 1
            return b

        ident = cst.t[:, CS_ID:CS_ID + 128]

        P.dma("sp", k_dma(pc.t[:], pcol_d), pc)
        P.dma("sp", k_dma(cst.t[:], cst_d[:, 0:CS_IOTA]), cst)
        P.dma("pool", k_dma(identb.t[:], cst_d[:, CS_ID:CS_ID + 128]), identb)
        P.op("pool", k_memset(onesb.t[:], 1.0), writes=[onesb])
        for k in range(8):
            P.dma("sp", k_dma(xT.t[:, k, :], xT_d[k * 128:(k + 1) * 128, :]), Rxload,
                  writes=[Rxload] + RX)

        with ExitStack() as sa:
            cS = P.tile("cS", [128, 16], F32, stack=sa)
            wm = [P.tile(f"wm{i}", [128, 8, 768], F32, dma=True, stack=sa) for i in range(2)]
            P.op("act", k_act(cS.t[:], pc.t[:, PC_C:PC_C + 16], AF.Silu), reads=[pc], writes=[cS])
            psA = bank()
            psA_v = psA.t[:, 0:96].rearrange("p (j c) -> p j c", c=2)
            wmod_v = wmod_d.rearrange("(k p) c -> p k c", p=128)
            for blk in range(8):
                w = wm[blk % 2]
                P.dma("sp", k_dma(w.t[:], wmod_v[:, :, blk * 768:(blk + 1) * 768]), w)
                for jj in range(6):
                    j = blk * 6 + jj
                    P.op("pe", k_mm([(psA_v[:, j, :], w.t[:, k, jj * 128:(jj + 1) * 128],
                                      cS.t[:, 2 * k:2 * k + 2], k == 0, k == 7) for k in range(8)]),
                         reads=[w, cS], writes=[psA])
            P.op("dve", k_tt(modc.t[:], psA_v,
                             pc.t[:, PC_BMOD:PC_BMOD + 48].unsqueeze(2).to_broadcast([128, 48, 2]), ALU.add),
                 reads=[psA, pc], writes=[modc])
            n1 = pc.t[:, PC_N1:PC_N1 + 8]
            n2 = pc.t[:, PC_N2:PC_N2 + 8]
            P.op("dve", k_stt(mc.t[:, 0, :], modc.t[:, 8:16, 0], 1.0, n1, ALU.add, ALU.mult), reads=[modc, pc], writes=[mc])
            P.op("dve", k_cp(mc.t[:, 1, :], modc.t[:, 0:8, 0]), reads=[modc], writes=[mc])
            P.op("dve", k_stt(mc.t[:, 2, :], modc.t[:, 8:16, 1], 1.0, n1, ALU.add, ALU.mult), reads=[modc, pc], writes=[mc])
            P.op("dve", k_cp(mc.t[:, 3, :], modc.t[:, 0:8, 1]), reads=[modc], writes=[mc])
            P.op("dve", k_cp(mc.t[:, 4, :], modc.t[:, 16:24, 0]), reads=[modc], writes=[mc])
            P.op("dve", k_stt(mc.t[:, 5, :], modc.t[:, 32:40, 0], 1.0, n2, ALU.add, ALU.mult), reads=[modc, pc], writes=[mc])
            P.op("dve", k_cp(mc.t[:, 6, :], modc.t[:, 24:32, 0]), reads=[modc], writes=[mc])
            P.op("dve", k_cp(mc.t[:, 7, :], modc.t[:, 40:48, 0]), reads=[modc], writes=[mc])
            P.barrier(dummy)
        if stage == 0:
            dump(mc, mc.t[:].rearrange("p a b -> p (a b)"), 64)

        def dump_any(regs, ap2d, ncols, stack):
            if not dbg_cols:
                return
            if "dtmp" not in dbg_state:
                dbg_state["dtmp"] = P.tile("dbg_bounce", [128, 512], F32, stack=stack)
            tmp = dbg_state["dtmp"]
            for o in range(0, ncols, 512):
                n = min(512, ncols - o)
                P.op("dve", k_cp(tmp.t[:, 0:n], ap2d[:, o:o + n]), reads=regs, writes=[tmp])
                dump(tmp, tmp.t[:, 0:n], n)

        def bc8(col_ap, n):
            return col_ap.unsqueeze(2).to_broadcast([128, 8, n])

        def make_h(W, src_ap, src_regs, A_ap, sh_ap, hT_tl, hT_ap, n=128):
            P.op("act", k_act(W["sq"].t[:, :, 0:n], src_ap, AF.Square), reads=src_regs, writes=[W["sq"]])
            b = bank()
            P.op("pe", k_mm([(b.t[:, 0:n], onesb.t[:], W["sq"].t[:, k, 0:n], k == 0, k == 7) for k in range(8)]),
                 reads=[W["sq"], onesb], writes=[b])
            P.op("act", k_act(W["rs"].t[:, 0:n], b.t[:, 0:n], AF.Sqrt, scale=1.0 / 1024.0, bias=EPS),
                 reads=[b], writes=[W["rs"]])
            P.op("dve", k_rcp(W["rs"].t[:, 0:n], W["rs"].t[:, 0:n]), reads=[W["rs"]], writes=[W["rs"]])
            P.op("dve", k_tt(W["t1"].t[:, :, 0:n], src_ap, W["rs"].t[:, 0:n].unsqueeze(1).to_broadcast([128, 8, n]), ALU.mult),
                 reads=src_regs + [W["rs"]], writes=[W["t1"]])
            P.op("pool", k_tt(W["t1"].t[:, :, 0:n], W["t1"].t[:, :, 0:n], bc8(A_ap, n), ALU.mult),
                 reads=[W["t1"], mc], writes=[W["t1"]])
            P.op("dve", k_tt(hT_ap, W["t1"].t[:, :, 0:n], bc8(sh_ap, n), ALU.add),
                 reads=[W["t1"], mc], writes=[hT_tl])

        if stage >= 1:
            build_sublayer1(nc, P, st, locals())
        if stage >= 2:
            build_moe(nc, P, st, locals())
        else:
            for k in range(8):
                P.dma("sp", k_dma(out_d[k * 128:(k + 1) * 128, :], xT.t[:, k, :]), P.region(f"o{k}", dma=True), reads=RX)
        P.emit()
    return nc


def build_sublayer1(nc, P, st, env):
    xT, RX, pc, cst, identb, onesb, mc, dummy = (env[k] for k in ("xT", "RX", "pc", "cst", "identb", "onesb", "mc", "dummy"))
    bank, make_h, dump, stage = env["bank"], env["make_h"], env["dump"], env["stage"]
    ctxT_d, prow_d, win_d, wout_d, wsT_d, mask_d = (env[k] for k in ("ctxT_d", "prow_d", "win_d", "wout_d", "wsT_d", "mask_d"))
    ident = cst.t[:, CS_ID:CS_ID + 128]
    win_v = win_d.rearrange("(k p) c -> p k c", p=128)
    wout_v = wout_d.rearrange("(k p) c -> p k c", p=128)

    with ExitStack() as s1, ExitStack() as sfb:
        def T(name, shape, dt, dma=False, stack=None):
            return P.tile(name, shape, dt, dma=dma, stack=stack or s1)

        def TF(name, shape, dt, dma=False):
            return P.tile(name, shape, dt, dma=dma, stack=sfb)

        hTctx = T("hTctx", [128, 8, 256], BF16)
        win = T("win", [128, 8, 2560], BF16, dma=True)
        ybT = T("ybT", [128, 4, 2048], BF16)
        RYB = [P.region(f"yb{c}") for c in range(16)]
        W = {
            "sq": T("w_sq", [128, 8, 128], BF16),
            "rs": T("w_rs", [128, 128], F32),
            "t1": T("w_t1", [128, 8, 128], F32),
        }
        hT = T("hT", [128, 8, 128], BF16)
        Sf = TF("Sf", [128, 16, 512], BF16)
        RSf = [P.region(f"Sf{c}") for c in range(16)]
        masks = TF("masks", [128, 1024], BF16, dma=True)
        LB = {d: TF(f"LB{d}", [128, 512], F32, dma=True) for d in "fb"}
        OML = {d: TF(f"OML{d}", [128, 512], F32, dma=True) for d in "fb"}
        HN = TF("HN", [128, 512], F32, dma=True)
        sig = TF("sig", [128, 512], F32)
        ff = TF("ff", [128, 512], F32)
        logf = {d: TF(f"logf{d}", [128, 512], F32) for d in "fb"}
        kk = TF("kk", [128, 512], F32)
        einv = TF("einv", [128, 512], F32)
        Kt = {d: TF(f"Kt{d}", [128, 512], BF16) for d in "fb"}
        V = TF("V", [128, 512], BF16)
        Aprev = TF("Aprev", [128, 4], F32)
        dcol = TF("dcol", [128, 4], F32)
        Sp = TF("Sp", [128, 512], F32)
        Sbf = TF("Sbf", [128, 512], BF16)
        M = TF("M", [128, 512], F32)

        P.dma("pool", k_dma(masks.t[:], mask_d), masks)
        P.dma("sp", k_dma(HN.t[:], prow_d[:, 2048:2560].partition_broadcast(128)), HN)
        for i, d in enumerate("fb"):
            P.dma("sp", k_dma(LB[d].t[:], prow_d[:, (2 * i) * 512:(2 * i + 1) * 512].partition_broadcast(128)), LB[d])
            P.dma("sp", k_dma(OML[d].t[:], prow_d[:, (2 * i + 1) * 512:(2 * i + 2) * 512].partition_broadcast(128)), OML[d])
            P.op("dve", k_tt(OML[d].t[:], LB[d].t[:], OML[d].t[:], ALU.subtract), reads=[LB[d], OML[d]], writes=[OML[d]])
            P.op("act", k_act(LB[d].t[:], OML[d].t[:], AF.Sigmoid), reads=[OML[d]], writes=[LB[d]])
            P.op("dve", k_ts(OML[d].t[:], LB[d].t[:], -1.0, ALU.mult, 1.0, ALU.add), reads=[LB[d]], writes=[OML[d]])
        with ExitStack() as s0:
            ctxT = P.tile("ctxT", [128, 8, 256], F32, dma=True, stack=s0)
            for k in range(8):
                P.dma("sp", k_dma(ctxT.t[:, k, :], ctxT_d[k * 128:(k + 1) * 128, :]), ctxT)
            for hh in range(2):
                make_h(W, ctxT.t[:, :, hh * 128:(hh + 1) * 128], [ctxT], mc.t[:, 2, :], mc.t[:, 3, :], hTctx,
                       hTctx.t[:, :, hh * 128:(hh + 1) * 128], n=128)
            P.barrier(dummy)

        if env["dbg_cols"] and stage < 2:
            env["dump_any"]([hTctx], hTctx.t[:].rearrange("p a b -> p (a b)"), 2048, s1)
            env["dump_any"]([LB["f"]], LB["f"].t[:], 512, s1)
            env["dump_any"]([LB["b"]], LB["b"].t[:], 512, s1)

        def load_win(groups):
            for slot, g in enumerate(groups):
                P.dma("pool", k_dma(win.t[:, :, slot * 512:(slot + 1) * 512], win_v[:, :, g * 512:(g + 1) * 512]), win)

        def get_hT(kind, c):
            if kind == "ctx":
                return hTctx, hTctx.t[:, :, c * 128:(c + 1) * 128]
            make_h(W, xT.t[:, :, c * 128:(c + 1) * 128], [RX[c]], mc.t[:, 0, :], mc.t[:, 1, :], hT, hT.t[:], n=128)
            return hT, hT.t[:]

        def proj_tok(h_tl, h_ap, slot):
            b = bank()
            P.op("pe", k_mm([(b.t[:, 0:512], h_ap[:, k, :], win.t[:, k, slot * 512:(slot + 1) * 512], k == 0, k == 7)
                             for k in range(8)]), reads=[h_tl, win], writes=[b])
            return b

        def proj_ch(h_tl, h_ap, slot):
            b = bank()
            lst = []
            for h in range(4):
                for k in range(8):
                    lst.append((b.t[:, h * 128:(h + 1) * 128], win.t[:, k, slot * 512 + h * 128: slot * 512 + (h + 1) * 128],
                                h_ap[:, k, :], k == 0, k == 7))
            P.op("pe", k_mm(lst), reads=[h_tl, win], writes=[b])
            return b

        def decay_tok(zb_bank, d):
            tm = cst.t[:, (CS_TMF if d == "f" else CS_TMB):(CS_TMF if d == "f" else CS_TMB) + 128]
            P.op("act", k_act(sig.t[:], zb_bank.t[:, 0:512], AF.Sigmoid), reads=[zb_bank], writes=[sig])
            P.op("dve", k_tt(ff.t[:], sig.t[:], OML[d].t[:], ALU.mult), reads=[sig, OML[d]], writes=[ff])
            P.op("pool", k_tt(ff.t[:], ff.t[:], LB[d].t[:], ALU.add), reads=[ff, LB[d]], writes=[ff])
            P.op("act", k_act(logf[d].t[:], ff.t[:], AF.Ln), reads=[ff], writes=[logf[d]])
            P.op("pool", k_ts(kk.t[:], ff.t[:], -1.0, ALU.mult, 1.0, ALU.add), reads=[ff], writes=[kk])
            b = bank()
            P.op("pe", k_mm([(b.t[:, 0:512], tm, logf[d].t[:], True, True)]), reads=[cst, logf[d]], writes=[b])
            P.op("act", k_act(einv.t[:], b.t[:, 0:512], AF.Exp, scale=-1.0), reads=[b], writes=[einv])
            P.op("dve", k_tt(Kt[d].t[:], kk.t[:], einv.t[:], ALU.mult), reads=[kk, einv], writes=[Kt[d]])

        def state_step(d, p, store_c, need_kv):
            xo = CS_XF if d == "f" else CS_XB
            b = bank()
            cm = b.t[:, 0:8].rearrange("p (h c) -> p h c", c=2)
            P.op("pe", k_mm([(cm[:, h, :], logf[d].t[:, h * 128:(h + 1) * 128], cst.t[:, xo:xo + 2], True, True)
                             for h in range(4)]), reads=[logf[d], cst], writes=[b])
            if p > 0:
                P.op("dve", k_tt(dcol.t[:], cm[:, :, 1], Aprev.t[:], ALU.add), reads=[b, Aprev], writes=[dcol])
                P.op("act", k_act(dcol.t[:], dcol.t[:], AF.Exp), reads=[dcol], writes=[dcol])
                P.op("dve", k_tt(Sp.t[:].rearrange("p (h v) -> p h v", h=4), M.t[:].rearrange("p (h v) -> p h v", h=4),
                                 dcol.t[:].unsqueeze(2).to_broadcast([128, 4, 128]), ALU.mult),
                     reads=[M, dcol], writes=[Sp])
                if store_c is not None:
                    if d == "f":
                        P.op("act", k_acp(Sf.t[:, store_c, :], Sp.t[:]), reads=[Sp], writes=[RSf[store_c]])
                    else:
                        P.op("act", k_acp(Sbf.t[:], Sp.t[:]), reads=[Sp], writes=[Sbf])
            P.op("dve", k_cp(Aprev.t[:], cm[:, :, 0]), reads=[b], writes=[Aprev])
            if need_kv:
                b2 = bank()
                P.op("pe", k_mm([(b2.t[:, h * 128:(h + 1) * 128], Kt[d].t[:, h * 128:(h + 1) * 128],
                                  V.t[:, h * 128:(h + 1) * 128], True, True) for h in range(4)]),
                     reads=[Kt[d], V], writes=[b2])
                if p > 0:
                    P.op("dve", k_tt(M.t[:], b2.t[:, 0:512], Sp.t[:], ALU.add), reads=[b2, Sp], writes=[M])
                else:
                    P.op("dve", k_cp(M.t[:], b2.t[:, 0:512]), reads=[b2], writes=[M])

        load_win([G_ZF, G_I])
        order_f = [("ctx", 0), ("ctx", 1)] + [("lat", c) for c in range(16)]
        for p, (kind, c) in enumerate(order_f):
            h_tl, h_ap = get_hT(kind, c)
            zb_ = proj_tok(h_tl, h_ap, 0)
            decay_tok(zb_, "f")
            need_kv = p < 17
            if need_kv:
                ib_ = proj_tok(h_tl, h_ap, 1)
                P.op("act", k_acp(V.t[:], ib_.t[:, 0:512]), reads=[ib_], writes=[V])
            state_step("f", p, c if kind == "lat" else None, need_kv)

        if env["dbg_cols"] and stage < 2:
            env["dump_any"](RSf, Sf.t[:].rearrange("p a b -> p (a b)"), 8192, s1)

        if stage >= 1.2:
            sweep_b(nc, P, sfb, locals(), env)
        P.barrier(dummy)
        sfb.close()
        if stage >= 1.3:
            with ExitStack() as sc:
                sweep_c(nc, P, sc, locals(), env)
                P.barrier(dummy)


def sweep_b(nc, P, s1, L, env):
    xT, RX, pc, cst, identb, mc = (env[k] for k in ("xT", "RX", "pc", "cst", "identb", "mc"))
    bank, dump, stage = env["bank"], env["dump"], env["stage"]
    win, Sf, RSf, ybT, RYB, masks, LB, OML, HN = (L[k] for k in ("win", "Sf", "RSf", "ybT", "RYB", "masks", "LB", "OML", "HN"))
    logf, Kt, V, Sbf = (L[k] for k in ("logf", "Kt", "V", "Sbf"))

    def T(name, shape, dt, dma=False):
        return P.tile(name, shape, dt, dma=dma, stack=s1)
    load_win, get_hT, proj_tok, proj_ch, decay_tok, state_step = (L[k] for k in ("load_win", "get_hT", "proj_tok", "proj_ch", "decay_tok", "state_step"))

    ET = T("ET", [128, 512], F32)
    QtT = {d: T(f"QtT{d}", [128, 512], BF16) for d in "fb"}
    KtT = {d: T(f"KtT{d}", [128, 512], BF16) for d in "fb"}
    scT = {d: T(f"scT{d}", [128, 512], BF16) for d in "fb"}
    osq, on, sg, gs = L["sig"], L["ff"], L["einv"], L["kk"]
    ss4 = T("ss4", [128, 4], F32)
    yb = T("yb", [128, 512], BF16)

    for d in "fb":
        P.op("pool", k_memset(scT[d].t[:], 0.0), writes=[scT[d]])
    load_win([G_ZB, G_I, G_ZF, G_Q, G_G])
    order_b = [("ctx", 1), ("ctx", 0)] + [("lat", c) for c in range(15, -1, -1)]
    for p, (kind, c) in enumerate(order_b):
        h_tl, h_ap = get_hT(kind, c)
        zbb = proj_tok(h_tl, h_ap, 0)
        decay_tok(zbb, "b")
        ib_ = proj_tok(h_tl, h_ap, 1)
        P.op("act", k_acp(V.t[:], ib_.t[:, 0:512]), reads=[ib_], writes=[V])
        state_step("b", p, c if kind == "lat" else None, p < 17)
        if kind != "lat":
            continue
        zfb = proj_tok(h_tl, h_ap, 2)
        decay_tok(zfb, "f")
        qb = proj_ch(h_tl, h_ap, 3)
        for d in "fb":
            tm = cst.t[:, (CS_TMF if d == "f" else CS_TMB):(CS_TMF if d == "f" else CS_TMB) + 128]
            b = bank()
            P.op("pe", k_mm([(b.t[:, h * 128:(h + 1) * 128], logf[d].t[:, h * 128:(h + 1) * 128], tm, True, True)
                             for h in range(4)]), reads=[logf[d], cst], writes=[b])
            P.op("act", k_act(ET.t[:], b.t[:, 0:512], AF.Exp), reads=[b], writes=[ET])
            P.op("dve", k_tt(QtT[d].t[:], qb.t[:, 0:512], ET.t[:], ALU.mult), reads=[qb, ET], writes=[QtT[d]])
            bt = bank()
            btv = bt.t[:].bitcast(BF16)
            P.op("pe", k_tr([(btv[:, h * 128:(h + 1) * 128], Kt[d].t[:, h * 128:(h + 1) * 128], identb.t[:]) for h in range(4)]),
                 reads=[Kt[d], identb], writes=[bt])
            P.op("act", k_acp(KtT[d].t[:], btv[:, 0:512]), reads=[bt], writes=[KtT[d]])
            bs_ = bank()
            P.op("pe", k_mm([(bs_.t[:, h * 128:(h + 1) * 128], KtT[d].t[:, h * 128:(h + 1) * 128],
                              QtT[d].t[:, h * 128:(h + 1) * 128], True, True) for h in range(4)]),
                 reads=[KtT[d], QtT[d]], writes=[bs_])
            mo = 0 if d == "f" else 512
            v3 = lambda ap: ap.rearrange("p (h t) -> p h t", h=4)
            pa, pb = (slice(0, 64), slice(64, 128)) if d == "f" else (slice(64, 128), slice(0, 64))
            cb = slice(64, 128) if d == "f" else slice(0, 64)
            P.op("dve", k_tt(scT[d].t[pa, :], bs_.t[pa, 0:512], masks.t[pa, mo:mo + 512], ALU.mult),
                 reads=[bs_, masks], writes=[scT[d]])
            P.op("dve", k_tt(v3(scT[d].t[pb, :])[:, :, cb], v3(bs_.t[pb, 0:512])[:, :, cb],
                             v3(masks.t[pb, mo:mo + 512])[:, :, cb], ALU.mult),
                 reads=[bs_, masks, scT[d]], writes=[scT[d]])
        bo = bank()
        lst = []
        for h in range(4):
            hs = slice(h * 128, (h + 1) * 128)
            lst.append((bo.t[:, hs], scT["f"].t[:, hs], V.t[:, hs], True, False))
            lst.append((bo.t[:, hs], scT["b"].t[:, hs], V.t[:, hs], False, False))
            lst.append((bo.t[:, hs], QtT["f"].t[:, hs], Sf.t[:, c, hs], False, False))
            lst.append((bo.t[:, hs], QtT["b"].t[:, hs], Sbf.t[:, hs], False, True))
        P.op("pe", k_mm(lst), reads=[scT["f"], scT["b"], V, QtT["f"], QtT["b"], RSf[c], Sbf], writes=[bo])
        P.op("act", k_act(osq.t[:], bo.t[:, 0:512], AF.Square), reads=[bo], writes=[osq])
        P.op("dve", k_red(ss4.t[:], osq.t[:].rearrange("p (h v) -> p h v", h=4)), reads=[osq], writes=[ss4])
        P.op("act", k_act(ss4.t[:], ss4.t[:], AF.Sqrt, scale=1.0 / 128.0, bias=EPS), reads=[ss4], writes=[ss4])
        P.op("dve", k_rcp(ss4.t[:], ss4.t[:]), reads=[ss4], writes=[ss4])
        P.op("dve", k_tt(on.t[:].rearrange("p (h v) -> p h v", h=4), bo.t[:, 0:512].rearrange("p (h v) -> p h v", h=4),
                         ss4.t[:].unsqueeze(2).to_broadcast([128, 4, 128]), ALU.mult), reads=[bo, ss4], writes=[on])
        P.op("pool", k_tt(on.t[:], on.t[:], HN.t[:], ALU.mult), reads=[on, HN], writes=[on])
        gb = proj_tok(h_tl, h_ap, 4)
        P.op("act", k_act(sg.t[:], gb.t[:, 0:512], AF.Sigmoid), reads=[gb], writes=[sg])
        P.op("dve", k_tt(gs.t[:], gb.t[:, 0:512], sg.t[:], ALU.mult), reads=[gb, sg], writes=[gs])
        P.op("dve", k_tt(yb.t[:], on.t[:], gs.t[:], ALU.mult), reads=[on, gs], writes=[yb])
        by = bank()
        byv = by.t[:].bitcast(BF16)
        P.op("pe", k_tr([(byv[:, h * 128:(h + 1) * 128], yb.t[:, h * 128:(h + 1) * 128], identb.t[:]) for h in range(4)]),
             reads=[yb, identb], writes=[by])
        P.op("act", k_acp(ybT.t[:, :, c * 128:(c + 1) * 128], byv[:, 0:512].rearrange("p (h t) -> p h t", h=4)),
             reads=[by], writes=[RYB[c]])

    if env["dbg_cols"] and stage < 2:
        env["dump_any"](RYB, ybT.t[:].rearrange("p a b -> p (a b)"), 8192, s1)


def sweep_c(nc, P, s1, L, env):
    xT, RX, pc, cst, identb, mc = (env[k] for k in ("xT", "RX", "pc", "cst", "identb", "mc"))
    bank, dump, stage = env["bank"], env["dump"], env["stage"]
    wout_v = L["wout_v"]
    win, ybT, RYB = (L[k] for k in ("win", "ybT", "RYB"))
    load_win, get_hT, proj_tok, proj_ch = (L[k] for k in ("load_win", "get_hT", "proj_tok", "proj_ch"))
    prow_d, wsT_d = env["prow_d"], env["wsT_d"]

    def T(name, shape, dt, dma=False):
        return P.tile(name, shape, dt, dma=dma, stack=s1)

    BS = T("BS", [128, 512], F32, dma=True)
    WsT = T("WsT", [128, 512], BF16, dma=True)
    P.dma("sp", k_dma(BS.t[:], prow_d[:, 2560:3072].partition_broadcast(128)), BS)
    P.dma("pool", k_dma(WsT.t[:], wsT_d), WsT)

    wout = T("wout", [128, 8, 1024], BF16, dma=True)
    P.dma("pool", k_dma(wout.t[:, :, 0:512], wout_v[:, :, 0:512]), wout)
    P.dma("pool", k_dma(wout.t[:, :, 512:1024], wout_v[:, :, 512:1024]), wout)
    gu = T("gu", [128, 512], F32)
    gv = T("gv", [128, 512], F32)
    cen = T("cen", [128, 512], F32)
    sqc = T("sqc", [128, 512], F32)
    st4 = T("st4", [128, 4], F32)
    vn = T("vn", [128, 512], BF16)
    za = T("za", [128, 512], F32)
    yaT = T("yaT", [128, 4, 128], BF16)
    xtmp = T("xtmp", [128, 8, 128], F32)
    lng = pc.t[:, PC_LNG:PC_LNG + 4]

    load_win([G_U, G_V])
    for c in range(16):
        h_tl, h_ap = get_hT("lat", c)
        ub = proj_ch(h_tl, h_ap, 0)
        P.op("act", k_act(gu.t[:], ub.t[:, 0:512], AF.Gelu_apprx_tanh), reads=[ub], writes=[gu])
        vb = proj_tok(h_tl, h_ap, 1)
        P.op("act", k_act(gv.t[:], vb.t[:, 0:512], AF.Gelu_apprx_tanh), reads=[vb], writes=[gv])
        v3 = lambda t: t.t[:].rearrange("p (h d) -> p h d", h=4)
        b4 = lambda t: t.t[:].unsqueeze(2).to_broadcast([128, 4, 128])
        P.op("dve", k_red(st4.t[:], v3(gv)), reads=[gv], writes=[st4])
        P.op("dve", k_ts(st4.t[:], st4.t[:], 1.0 / 128.0, ALU.mult), reads=[st4], writes=[st4])
        P.op("dve", k_tt(v3(cen), v3(gv), b4(st4), ALU.subtract), reads=[gv, st4], writes=[cen])
        P.op("act", k_act(sqc.t[:], cen.t[:], AF.Square), reads=[cen], writes=[sqc])
        P.op("dve", k_red(st4.t[:], v3(sqc)), reads=[sqc], writes=[st4])
        P.op("act", k_act(st4.t[:], st4.t[:], AF.Sqrt, scale=1.0 / 128.0, bias=EPS), reads=[st4], writes=[st4])
        P.op("dve", k_rcp(st4.t[:], st4.t[:]), reads=[st4], writes=[st4])
        P.op("dve", k_tt(v3(vn), v3(cen), b4(st4), ALU.mult), reads=[cen, st4], writes=[vn])
        bz = bank()
        P.op("pe", k_mm([(bz.t[:, h * 128:(h + 1) * 128], vn.t[:, h * 128:(h + 1) * 128], WsT.t[:, h * 128:(h + 1) * 128], True, True)
                         for h in range(4)]), reads=[vn, WsT], writes=[bz])
        P.op("dve", k_tt(v3(za), bz.t[:, 0:512].rearrange("p (h d) -> p h d", h=4),
                         lng.unsqueeze(2).to_broadcast([128, 4, 128]), ALU.mult), reads=[bz, pc], writes=[za])
        P.op("pool", k_tt(za.t[:], za.t[:], BS.t[:], ALU.add), reads=[za, BS], writes=[za])
        P.op("dve", k_tt(yaT.t[:].rearrange("p h t -> p (h t)"), za.t[:], gu.t[:], ALU.mult), reads=[za, gu], writes=[yaT])
        bw = [bank(), bank()]
        for half in range(2):
            lst = []
            for jj in range(4):
                j = half * 4 + jj
                for k in range(8):
                    rhs = yaT.t[:, k, :] if k < 4 else ybT.t[:, k - 4, c * 128:(c + 1) * 128]
                    lst.append((bw[half].t[:, jj * 128:(jj + 1) * 128], wout.t[:, k, j * 128:(j + 1) * 128], rhs, k == 0, k == 7))
            P.op("pe", k_mm(lst), reads=[yaT, RYB[c], wout], writes=[bw[half]])
            P.op("dve", k_tt(xtmp.t[:, half * 4:(half + 1) * 4, :], bw[half].t[:, 0:512].rearrange("p (j t) -> p j t", j=4),
                             mc.t[:, 4, half * 4:(half + 1) * 4].unsqueeze(2).to_broadcast([128, 4, 128]), ALU.mult),
                 reads=[bw[half], mc], writes=[xtmp])
        P.op("pool", k_tt(xT.t[:, :, c * 128:(c + 1) * 128], xT.t[:, :, c * 128:(c + 1) * 128], xtmp.t[:], ALU.add),
             reads=[xtmp, RX[c]], writes=[RX[c]])


CAP = 256
NBLK = CAP // 128
NROUND = 2048 // CAP
NSINGLE = 3
NROWS = 2048 + 128


def build_moe(nc, P, st, env):
    xT, RX, pc, cst, identb, onesb, mc, dummy = (env[k] for k in ("xT", "RX", "pc", "cst", "identb", "onesb", "mc", "dummy"))
    bank, dump, stage = env["bank"], env["dump"], env["stage"]
    rw_d, rb_d, w1_d, w2_d, b2_d, sel_d, out_d, cst_d = (env[k] for k in ("rw_d", "rb_d", "w1_d", "w2_d", "b2_d", "sel_d", "out_d", "cst_d"))
    ident = cst.t[:, CS_ID:CS_ID + 128]
    RXG = lambda g: RX[g * 4:(g + 1) * 4]
    U32 = mybir.dt.uint32
    I32 = mybir.dt.int32

    h2rows_d = nc.dram_tensor("h2rows", [NROWS, 1024], BF16).ap()
    moe_d = nc.dram_tensor("moe_acc", [NROWS, 1024], F32).ap()
    h2T_d = nc.dram_tensor("h2T_scr", [1024, 2048], BF16).ap()
    xscr_d = nc.dram_tensor("x_scr", [1024, 2048], F32).ap()
    Rxscr = P.region("x_scr", dma=True)
    Rxload = env["Rxload"]
    Rh2rows = P.region("h2rows", dma=True)
    Rmoe = P.region("moe_acc", dma=True)
    Rmoe0 = P.region("moe_zero", dma=True)
    Rh2Td = P.region("h2T_scr", dma=True)

    with ExitStack() as s2:
        def T(name, shape, dt, dma=False, stack=None):
            return P.tile(name, shape, dt, dma=dma, stack=stack or s2)

        posm = T("posm", [128, 16, 32], F32)
        Rm = T("Rm", [128, 16, 32, 3], F32)
        b2sb = T("b2sb", [32, 1024], F32, dma=True)
        flags = T("flags", [128, NROUND, 32], I32)
        lsb = T("lsb", [128, 128], BF16, dma=True)
        P.dma("sp", k_dma(b2sb.t[:], b2_d), b2sb)
        P.dma("pool", k_dma(lsb.t[:], cst_d[:, CS_LS:CS_LS + 128]), lsb)

        with ExitStack() as s3:
            h2T = T("h2T", [128, 8, 2048], BF16, stack=s3)
            RH2 = [P.region(f"h2_{g}") for g in range(4)]
            rw = T("rw", [128, 8, 32], F32, dma=True, stack=s3)
            rb = T("rb", [32, 1], F32, dma=True, stack=s3)
            sq = T("m_sq", [128, 8, 512], BF16, stack=s3)
            rs = T("m_rs", [128, 512], F32, stack=s3)
            h2f = T("h2f", [128, 8, 512], F32, stack=s3)
            lgT = T("lgT", [32, 2048], F32, stack=s3)
            cwTf = T("cwTf", [32, 2048], F32, stack=s3)
            lt = T("lt", [128, 32], F32, stack=s3)
            m8 = T("m8", [128, 8], F32, stack=s3)
            negm = T("negm", [128, 1], F32, stack=s3)
            ex = T("ex", [128, 32], F32, stack=s3)
            den = T("den", [128, 1], F32, stack=s3)
            cwa = T("cwa", [128, 16, 32], F32, stack=s3)
            mska = T("mska", [128, 16, 32], BF16, stack=s3)
            hrow = T("hrow", [128, 1024], BF16, stack=s3)
            zrow = T("zrow", [128, 1024], F32, stack=s3)
            P.dma("sp", k_dma(rw.t[:], rw_d.rearrange("(k p) e -> p k e", p=128)), rw)
            P.dma("sp", k_dma(rb.t[:], rb_d), rb)
            P.op("pool", k_memset(zrow.t[:], 0.0), writes=[zrow])
            for i in range(NROWS // 128):
                P.dma("sp", k_dma(moe_d[i * 128:(i + 1) * 128, :], zrow.t[:]), Rmoe0, reads=[zrow])
            P.op("pool", k_memset(hrow.t[:], 0.0), writes=[hrow])
            P.dma("sp", k_dma(h2rows_d[2048:2176, :], hrow.t[:]), Rh2rows, reads=[hrow])
            for g in range(4):
                gs_ = slice(g * 512, (g + 1) * 512)
                P.op("act", k_act(sq.t[:], xT.t[:, :, gs_], AF.Square), reads=RXG(g), writes=[sq])
                b = bank()
                P.op("pe", k_mm([(b.t[:, 0:512], onesb.t[:], sq.t[:, k, :], k == 0, k == 7) for k in range(8)]),
                     reads=[sq, onesb], writes=[b])
                P.op("act", k_act(rs.t[:], b.t[:, 0:512], AF.Sqrt, scale=1.0 / 1024.0, bias=EPS), reads=[b], writes=[rs])
                P.op("dve", k_rcp(rs.t[:], rs.t[:]), reads=[rs], writes=[rs])
                P.op("dve", k_tt(h2f.t[:], xT.t[:, :, gs_], rs.t[:].unsqueeze(1).to_broadcast([128, 8, 512]), ALU.mult),
                     reads=RXG(g) + [rs], writes=[h2f])
                P.op("pool", k_tt(h2f.t[:], h2f.t[:], mc.t[:, 5, :].unsqueeze(2).to_broadcast([128, 8, 512]), ALU.mult),
                     reads=[h2f, mc], writes=[h2f])
                P.op("dve", k_tt(h2f.t[:], h2f.t[:], mc.t[:, 6, :].unsqueeze(2).to_broadcast([128, 8, 512]), ALU.add),
                     reads=[h2f, mc], writes=[h2f])
                P.op("act", k_acp(h2T.t[:, :, gs_], h2f.t[:]), reads=[h2f], writes=[RH2[g]])
                b = bank()
                P.op("pe", k_mm([(b.t[0:32, 0:512], rw.t[:, k, :], h2f.t[:, k, :], k == 0, k == 7) for k in range(8)]),
                     reads=[rw, h2f], writes=[b])
                P.op("dve", k_ts(lgT.t[:, gs_], b.t[0:32, 0:512], rb.t[:, 0:1], ALU.add), reads=[b, rb], writes=[lgT])
            for k in range(8):
                P.dma("sp", k_dma(h2T_d[k * 128:(k + 1) * 128, :], h2T.t[:, k, :]), Rh2Td, reads=RH2)
            for tix in range(16):
                ts_ = slice(tix * 128, (tix + 1) * 128)
                b = bank()
                P.op("pe", k_tr([(b.t[:, 0:32], lgT.t[:, ts_], cst.t[0:32, CS_ID:CS_ID + 32])]), reads=[lgT, cst], writes=[b])
                P.op("dve", k_cp(lt.t[:], b.t[:, 0:32]), reads=[b], writes=[lt])
                P.op("dve", lambda e: e.max(out=m8.t[:], in_=lt.t[:]), reads=[lt], writes=[m8])
                P.op("dve", k_ts(negm.t[:], m8.t[:, 0:1], -1.0, ALU.mult), reads=[m8], writes=[negm])
                P.op("dve", k_ts(mska.t[:, tix, :], lt.t[:], m8.t[:, 3:4], ALU.is_ge), reads=[lt, m8], writes=[mska])
                P.op("act", k_act(ex.t[:], lt.t[:], AF.Exp, bias=negm.t[:, 0:1], scale=1.0), reads=[lt, negm], writes=[ex])
                P.op("dve", k_tt(ex.t[:], ex.t[:], mska.t[:, tix, :], ALU.mult), reads=[ex, mska], writes=[ex])
                P.op("dve", k_red(den.t[:], ex.t[:]), reads=[ex], writes=[den])
                P.op("dve", k_rcp(den.t[:], den.t[:]), reads=[den], writes=[den])
                P.op("dve", k_ts(cwa.t[:, tix, :], ex.t[:], den.t[:, 0:1], ALU.mult), reads=[ex, den], writes=[cwa])
                b = bank()
                P.op("pe", k_tr([(b.t[0:32, 0:128], cwa.t[:, tix, :], ident)]), reads=[cwa, cst], writes=[b])
                P.op("act", k_acp(cwTf.t[:, ts_], b.t[0:32, 0:128]), reads=[b], writes=[cwTf])
                b = bank()
                bv = b.t[:].bitcast(BF16)
                P.op("pe", k_tr([(bv[:, k * 128:(k + 1) * 128], h2T.t[:, k, ts_], identb.t[:]) for k in range(8)]),
                     reads=RH2 + [identb], writes=[b])
                P.op("act", k_acp(hrow.t[:], bv[:, 0:1024]), reads=[b], writes=[hrow])
                P.dma("sp", k_dma(h2rows_d[ts_, :], hrow.t[:]), Rh2rows, reads=[hrow])
            bp = bank()
            bpv = bp.t[:, 0:512].rearrange("p (i e) -> p i e", e=32)
            lst = []
            for i in range(16):
                for i2 in range(i + 1):
                    lst.append((bpv[:, i, :], (lsb.t[:] if i2 == i else onesb.t[:]), mska.t[:, i2, :], i2 == 0, i2 == i))
            P.op("pe", k_mm(lst), reads=[mska, lsb, onesb], writes=[bp])
            bc = bank()
            P.op("pe", k_mm([(bc.t[:, 0:32], onesb.t[:], mska.t[:, i, :], i == 0, i == 15) for i in range(16)]),
                 reads=[mska, onesb], writes=[bc])
            P.op("dve", k_stt(posm.t[:], bpv, 1.0, mska.t[:], ALU.add, ALU.mult), reads=[bp, mska], writes=[posm])
            P.op("dve", k_ts(posm.t[:], posm.t[:], -1.0, ALU.add), reads=[posm], writes=[posm])
            for rd in range(NROUND):
                P.op("dve", k_ts(flags.t[:, rd, :], bc.t[:, 0:32], float(rd * CAP) + 0.5, ALU.is_gt), reads=[bc], writes=[flags])
            P.op("pool", k_memset(Rm.t[:], 1.0), writes=[Rm])
            P.op("dve", k_cp(Rm.t[:, :, :, 2], cwa.t[:]), reads=[cwa, Rm], writes=[Rm])
            P.op("dve", k_cp(Rm.t[:, :, :, 0], cst.t[:, CS_TOK:CS_TOK + 16].unsqueeze(2).to_broadcast([128, 16, 32])),
                 reads=[cst, Rm], writes=[Rm])
            for j in range(8):
                for g in range(4):
                    gs_ = slice(g * 512, (g + 1) * 512)
                    b = bank()
                    P.op("pe", k_mm([(b.t[:, 0:512], b2sb.t[:, j * 128:(j + 1) * 128], cwTf.t[:, gs_], True, True)]),
                         reads=[b2sb, cwTf], writes=[b])
                    P.op("dve", k_stt(xT.t[:, j, gs_], b.t[:, 0:512], mc.t[:, 7, j:j + 1], xT.t[:, j, gs_], ALU.mult, ALU.add),
                         reads=[b, mc] + RXG(g), writes=RXG(g))
            P.barrier(dummy)

        s4 = ExitStack()
        TE = lambda name, shape, dt, dma=False: P.tile(name, shape, dt, dma=dma, stack=s4)
        b1u1 = TE("b1u1", [128, 32, 8], F32)
        iota_t = TE("iota", [128, 2048], F32, dma=True)
        P.dma("sp", k_dma(iota_t.t[:], cst_d[:, CS_IOTA:CS_IOTA + 2048]), iota_t)
        stg = [TE(f"stg{i}", [128, 2048], F32, dma=True) for i in range(4)]
        for k in range(8):
            P.dma("sp", k_dma(xscr_d[k * 128:(k + 1) * 128, :], xT.t[:, k, :]), Rxscr, reads=RX)
        P.barrier(dummy)
        xflat = xT.t[:].rearrange("p k t -> p (k t)")
        w1e = [xflat[:, b_ * 8192:(b_ + 1) * 8192].bitcast(BF16).rearrange("p (k c) -> p k c", k=8) for b_ in range(2)]
        RW1 = [[P.region(f"w1e{b_}_{s_}") for s_ in range(8)] for b_ in range(2)]
        w2e = [TE(f"w2e{b_}", [128, 8, 1024], BF16) for b_ in range(2)]
        RW2 = [[P.region(f"w2e{b_}_{h}") for h in range(4)] for b_ in range(2)]
        Pm = [TE(f"Pm{i}", [128, CAP], F32) for i in range(4)]
        idxr = TE("idxr", [3, CAP], F32)
        idxf = TE("idxf", [128, NBLK, 3], F32)
        tmpi = TE("tmpi", [128, NBLK], F32)
        idxu = [TE(f"idxu{i}", [128, NBLK], U32) for i in range(2)]
        cws = [TE(f"cws{i}", [128, NBLK], F32) for i in range(2)]
        Xg = [TE(f"Xg{i}", [128, 1024], BF16, dma=True) for i in range(3)]
        XT = [TE(f"XT{i}", [128, 8, CAP], BF16) for i in range(2)]
        actT = TE("actT", [128, 8, CAP], BF16)
        RACT = [P.region(f"act{j}") for j in range(8)]
        ga = [TE(f"ga{i}", [128, CAP], F32) for i in range(4)]
        uu = [TE(f"uu{i}", [128, CAP], F32) for i in range(4)]
        Ysb = [TE(f"Ysb{i}", [128, 1024], F32) for i in range(NBLK)]
        w1_v = w1_d.rearrange("e (k p) c -> e p k c", p=128)
        w2_v = w2_d.rearrange("e (k p) c -> e p k c", p=128)
        b1v = pc.t[:, PC_B1:PC_B1 + 512].rearrange("p (e j) -> p e j", j=16)
        P.op("dve", k_ts(b1u1.t[:], b1v[:, :, 8:16], 1.0, ALU.add), reads=[pc], writes=[b1u1])
        padrow = cst.t[:, CS_PAD:CS_PAD + 1]
        cnt = {"stg": 0, "pm": 0, "x": 0, "wk": 0, "rd": 0, "cast": 0}
        cast_engs = ("act", "dve")

        def load_cast(src_ap, stg_view, dst_reg, dst_ap):
            sg_ = stg[cnt["stg"] % 4]
            cnt["stg"] += 1
            eng = cast_engs[cnt["cast"] % 2]
            cnt["cast"] += 1
            sv = stg_view(sg_.t)
            P.dma("sp", k_dma(sv, src_ap), sg_)
            if eng == "act":
                P.op("act", k_act(dst_ap, sv, AF.Copy), reads=[sg_], writes=[dst_reg])
            else:
                P.op(eng, k_cp(dst_ap, sv), reads=[sg_], writes=[dst_reg])

        def weight_pieces(e):
            wb = e % 2
            lst = []
            for k in range(8):
                lst.append(lambda k=k: load_cast(w1_d[e, k * 128:(k + 1) * 128, :], (lambda t: t[:]), RW1[wb][k], w1e[wb][:, k, :]))
            for q_ in range(4):
                lst.append(lambda q_=q_: load_cast(w2_v[e, :, 2 * q_:2 * q_ + 2, :], (lambda t: t[:].rearrange("p (k c) -> p k c", k=2)),
                                                   RW2[wb][q_], w2e[wb].t[:, 2 * q_:2 * q_ + 2, :]))
            return lst

        for f_ in weight_pieces(0):
            f_()
        for e in range(32):
            wb = e % 2
            pending = weight_pieces(e + 1) if e + 1 < 32 else []
            for rd in range(NROUND):
                fcol = rd * 32 + e
                if 0 < rd <= NSINGLE:
                    P.cond_begin(flags, flags.t[:].rearrange("p r e -> p (r e)")[0:1, fcol:fcol + 1])
                iu = idxu[cnt["rd"] % 2]
                cw_ = cws[cnt["rd"] % 2]
                xt = XT[cnt["rd"] % 2]
                cnt["rd"] += 1
                iota = iota_t.t[:, rd * CAP:(rd + 1) * CAP]
                bi = bank()
                for i in range(16):
                    pm = Pm[cnt["pm"] % 4]
                    cnt["pm"] += 1
                    P.op("dve", k_ts(pm.t[:], iota, posm.t[:, i, e:e + 1], ALU.is_equal), reads=[iota_t, posm], writes=[pm])
                    P.op("pe", k_mm([(bi.t[0:3, 0:CAP], Rm.t[:, i, e, :], pm.t[:], i == 0, i == 15)]), reads=[pm, Rm], writes=[bi])
                P.op("act", k_acp(idxr.t[:], bi.t[0:3, 0:CAP]), reads=[bi], writes=[idxr])
                bi2 = bank()
                biv = bi2.t[:, 0:NBLK * 3].rearrange("p (j c) -> p j c", c=3)
                P.op("pe", k_tr([(biv[:, j, :], idxr.t[0:3, j * 128:(j + 1) * 128], cst.t[0:3, CS_ID:CS_ID + 3]) for j in range(NBLK)]),
                     reads=[idxr, cst], writes=[bi2])
                P.op("dve", k_cp(idxf.t[:], biv), reads=[bi2], writes=[idxf])
                P.op("dve", k_ts(tmpi.t[:], idxf.t[:, :, 1], -1.0, ALU.mult, 1.0, ALU.add), reads=[idxf], writes=[tmpi])
                P.op("dve", k_stt(iu.t[:], tmpi.t[:], padrow, idxf.t[:, :, 0], ALU.mult, ALU.add), reads=[tmpi, idxf, cst], writes=[iu])
                P.op("dve", k_ts(cw_.t[:], idxf.t[:, :, 2], 1.0 / 1.702, ALU.mult), reads=[idxf], writes=[cw_])
                for j in range(NBLK):
                    xg = Xg[cnt["x"] % 3]
                    cnt["x"] += 1
                    P.dma("pool", (lambda en, xg=xg, iu=iu, j=j: en.indirect_dma_start(
                        out=xg.t[:], out_offset=None, in_=h2rows_d,
                        in_offset=bass.IndirectOffsetOnAxis(ap=iu.t[:, j:j + 1], axis=0))), xg, reads=[iu, Rh2rows])
                    b = bank()
                    bv = b.t[:].bitcast(BF16)
                    P.op("pe", k_tr([(bv[:, k * 128:(k + 1) * 128], xg.t[:, k * 128:(k + 1) * 128], identb.t[:]) for k in range(8)]),
                         reads=[xg, identb], writes=[b])
                    P.op("act", k_acp(xt.t[:, :, j * 128:(j + 1) * 128], bv[:, 0:1024].rearrange("p (k r) -> p k r", k=8)),
                         reads=[b], writes=[xt])
                for j in range(8):
                    bg = bank()
                    bu = bank()
                    P.op("pe", k_mm([(bg.t[:, 0:CAP], w1e[wb][:, k, j * 128:(j + 1) * 128], xt.t[:, k, :], k == 0, k == 7)
                                     for k in range(8)]), reads=RW1[wb] + [xt], writes=[bg])
                    P.op("pe", k_mm([(bu.t[:, 0:CAP], w1e[wb][:, k, 1024 + j * 128:1024 + (j + 1) * 128], xt.t[:, k, :], k == 0, k == 7)
                                     for k in range(8)]), reads=RW1[wb] + [xt], writes=[bu])
                    g_, u_ = ga[cnt["wk"] % 4], uu[cnt["wk"] % 4]
                    cnt["wk"] += 1
                    P.op("dve", k_ts(g_.t[:], bg.t[:, 0:CAP], b1v[:, e, j:j + 1], ALU.add, 7.0, ALU.min), reads=[bg, pc], writes=[g_])
                    P.op("act", k_act(g_.t[:], g_.t[:], AF.Silu, scale=1.702), reads=[g_], writes=[g_])
                    P.op("dve", k_ts(u_.t[:], bu.t[:, 0:CAP], b1u1.t[:, e, j:j + 1], ALU.add, 8.0, ALU.min), reads=[bu, b1u1], writes=[u_])
                    P.op("dve", k_stt(actT.t[:, j, :], u_.t[:], -6.0, g_.t[:], ALU.max, ALU.mult), reads=[u_, g_], writes=[RACT[j]])
                    if rd == 0 and pending:
                        pending.pop(0)()
                for half in range(2):
                    for j in range(NBLK):
                        b = bank()
                        P.op("pe", k_mm([(b.t[:, 0:512], actT.t[:, k, j * 128:(j + 1) * 128], w2e[wb].t[:, k, half * 512:(half + 1) * 512], k == 0, k == 7)
                                         for k in range(8)]), reads=RW2[wb] + RACT, writes=[b])
                        P.op("act", k_act(Ysb[j].t[:, half * 512:(half + 1) * 512], b.t[:, 0:512], AF.Copy, scale=cw_.t[:, j:j + 1]),
                             reads=[b, cw_], writes=[Ysb[j]])
                        if rd == 0 and pending:
                            pending.pop(0)()
                for j in range(NBLK):
                    P.dma("pool", (lambda en, j=j, iu=iu: en.indirect_dma_start(
                        out=moe_d, out_offset=bass.IndirectOffsetOnAxis(ap=iu.t[:, j:j + 1], axis=0),
                        in_=Ysb[j].t[:], in_offset=None, compute_op=ALU.add)), Rmoe, reads=[Ysb[j], iu, Rmoe0], serialize=True)
                if rd == NROUND - 1:
                    for _ in range(NSINGLE):
                        P.cond_end()
                elif rd == 0:
                    while pending:
                        pending.pop(0)()
            while pending:
                pending.pop(0)()
        P.barrier(dummy)
        s4.close()
        for k in range(8):
            P.dma("sp", k_dma(xT.t[:, k, :], xscr_d[k * 128:(k + 1) * 128, :]), Rxload, reads=[Rxscr], writes=[Rxload] + RX)

        s5 = ExitStack()
        Mt = [P.tile(f"Mt{i}", [128, 1024], F32, dma=True, stack=s5) for i in range(2)]
        xtmp = P.tile("c_xtmp", [128, 8, 128], F32, stack=s5)
        for tix in range(16):
            ts_ = slice(tix * 128, (tix + 1) * 128)
            mt = Mt[tix % 2]
            P.dma("sp", k_dma(mt.t[:], moe_d[ts_, :]), mt, reads=[Rmoe])
            if env["dbg_cols"]:
                dump(mt, mt.t[:], 1024)
            for half in range(2):
                b = bank()
                P.op("pe", k_tr([(b.t[:, kk_ * 128:(kk_ + 1) * 128], mt.t[:, (half * 4 + kk_) * 128:(half * 4 + kk_ + 1) * 128], ident)
                                 for kk_ in range(4)]), reads=[mt, cst], writes=[b])
                P.op("dve", k_tt(xtmp.t[:, half * 4:(half + 1) * 4, :], b.t[:, 0:512].rearrange("p (j t) -> p j t", j=4),
                                 mc.t[:, 7, half * 4:(half + 1) * 4].unsqueeze(2).to_broadcast([128, 4, 128]), ALU.mult),
                     reads=[b, mc], writes=[xtmp])
            P.op("pool", k_tt(xT.t[:, :, ts_], xT.t[:, :, ts_], xtmp.t[:], ALU.add), reads=[xtmp, RX[tix]], writes=[RX[tix]])
        P.barrier(dummy)
        s5.close()

        sqf = T("f_sq", [128, 8, 512], BF16)
        rsf = T("f_rs", [128, 512], F32)
        of = T("of", [128, 8, 512], F32)
        Rout = P.region("out", dma=True)
        fn = pc.t[:, PC_FN:PC_FN + 8]
        out_v = out_d.rearrange("(k p) t -> p k t", p=128)
        for g in range(4):
            gs_ = slice(g * 512, (g + 1) * 512)
            P.op("act", k_act(sqf.t[:], xT.t[:, :, gs_], AF.Square), reads=RXG(g), writes=[sqf])
            b = bank()
            P.op("pe", k_mm([(b.t[:, 0:512], onesb.t[:], sqf.t[:, k, :], k == 0, k == 7) for k in range(8)]),
                 reads=[sqf, onesb], writes=[b])
            P.op("act", k_act(rsf.t[:], b.t[:, 0:512], AF.Sqrt, scale=1.0 / 1024.0, bias=EPS), reads=[b], writes=[rsf])
            P.op("dve", k_rcp(rsf.t[:], rsf.t[:]), reads=[rsf], writes=[rsf])
            P.op("dve", k_tt(of.t[:], xT.t[:, :, gs_], rsf.t[:].unsqueeze(1).to_broadcast([128, 8, 512]), ALU.mult),
                 reads=RXG(g) + [rsf], writes=[of])
            P.op("pool", k_tt(of.t[:], of.t[:], fn.unsqueeze(2).to_broadcast([128, 8, 512]), ALU.mult), reads=[of, pc], writes=[of])
            P.dma("sp", k_dma(out_v[:, :, gs_], of.t[:]), Rout, reads=[of], writes=[Rout])


_CACHE = {}


def make_in_maps(x, c, ctx, c_ctx, w_mod, b_mod, norm1, w_in, sgu_ln, sgu_w, sgu_b, lb_fwd, lb_bwd,
                 hgrn_norm, w_out, norm2, router_w, router_b, w1, b1, w2, b2, final_norm):
    f = lambda a: np.ascontiguousarray(np.asarray(a, dtype=np.float32))
    cst, masks, sel = _consts()
    pcol = np.zeros((NCORES, 128, NPC), np.float32)
    cc = col_layout(f(c_ctx))
    for b in range(NCORES):
        cb = col_layout(f(c[b]))
        pcol[b, :, PC_C:PC_C + 16:2] = cb
        pcol[b, :, PC_C + 1:PC_C + 16:2] = cc
    pcol[:, :, PC_BMOD:PC_BMOD + 48] = col_layout(f(b_mod[0]))
    pcol[:, :, PC_N1:PC_N1 + 8] = col_layout(f(norm1[0]))
    pcol[:, :, PC_N2:PC_N2 + 8] = col_layout(f(norm2[0]))
    pcol[:, :, PC_FN:PC_FN + 8] = col_layout(f(final_norm))
    pcol[:, :, PC_LNG:PC_LNG + 4] = f(sgu_ln[0]).T
    b1c = f(b1[0]).reshape(32, 16, 128).transpose(2, 0, 1).reshape(128, 512)
    pcol[:, :, PC_B1:PC_B1 + 512] = b1c
    prow = np.concatenate([f(lb_fwd[0]), f(lb_fwd[1]), f(lb_bwd[0]), f(lb_bwd[1]), f(hgrn_norm[0]),
                           f(sgu_b[0]).reshape(-1)])[None, :]
    wsT = np.ascontiguousarray(f(sgu_w[0]).transpose(2, 0, 1).reshape(128, 512))
    shared = {
        "prow": f(prow), "w_mod": f(w_mod[0]), "w_in": f(w_in[0]), "w_out": f(w_out[0]),
        "router_w": f(router_w[0]), "router_b": f(router_b[0]).reshape(32, 1), "w1": f(w1[0]), "w2": f(w2[0]),
        "b2": f(b2[0]), "sgu_wT": wsT, "cst": cst, "masks": masks, "sel": sel,
    }
    maps = []
    for b in range(NCORES):
        m = dict(shared)
        m["xT"] = np.ascontiguousarray(f(x[b]).T)
        m["ctxT"] = np.ascontiguousarray(f(ctx[b]).T)
        m["pcol"] = pcol[b]
        maps.append(m)
    return maps


def kernel(**inputs):
    nc = build_program()
    maps = make_in_maps(**inputs)
    res = run_bass_kernel_spmd(nc, maps, core_ids=list(range(NCORES)))
    out = np.stack([np.ascontiguousarray(res.results[b]["outT"].T) for b in range(NCORES)], axis=0)
    return out.astype(np.float32)
```

```python
import numpy as np
from contextlib import ExitStack
import concourse.bass as bass
import concourse.mybir as mybir
from concourse.bass_utils import run_bass_kernel_spmd

F32 = mybir.dt.float32
BF16 = mybir.dt.bfloat16
AF = mybir.ActivationFunctionType
ALU = mybir.AluOpType
AX = mybir.AxisListType

EPOCH = 8192
NCORES = 8
EPS = 1e-6


class Region:
    __slots__ = ("name", "writers", "readers", "chan", "dma_count")

    def __init__(self, name, chan=None):
        self.name = name
        self.writers = []
        self.readers = []
        self.chan = chan
        self.dma_count = 0


class Op:
    __slots__ = ("eng", "fn", "deps", "idx", "is_dma", "token", "name", "kind", "flag_ap", "ext", "cid", "chain")

    def __init__(self, eng, fn, name=""):
        self.eng = eng
        self.fn = fn
        self.deps = []
        self.idx = None
        self.is_dma = False
        self.token = None
        self.name = name
        self.kind = "op"
        self.flag_ap = None
        self.ext = None
        self.cid = None
        self.chain = False


class Tl:
    __slots__ = ("t", "r")

    def __init__(self, t, r):
        self.t = t
        self.r = r


class Prog:
    ENGS = ("pe", "act", "dve", "pool", "sp")

    def __init__(self, nc, stack):
        self.nc = nc
        self.stack = stack
        self.ops = {e: [] for e in self.ENGS}
        self.count = {e: 0 for e in self.ENGS}
        self.sems = {e: [] for e in self.ENGS}
        self.n_chan = 0
        self.final_tokens = {}
        self.last_dma = {}

    def sem(self, name):
        return self.stack.enter_context(self.nc.semaphore(name))

    def region(self, name, dma=False):
        ch = None
        if dma:
            self.n_chan += 1
            ch = self.sem(f"c{self.n_chan}_{name}")
        return Region(name, ch)

    def tile(self, name, shape, dtype, dma=False, stack=None):
        st = stack or self.stack
        t = st.enter_context(self.nc.sbuf_tensor("sb_" + name, list(shape), dtype))
        return Tl(t, self.region(name, dma))

    def psum(self, name, shape, dtype=F32):
        t = self.stack.enter_context(self.nc.psum_tensor(name, list(shape), dtype))
        return Tl(t, self.region(name))

    def _add(self, eng, fn, reads, writes, name, dma_region=None):
        op = Op(eng, fn, name)
        is_dma = dma_region is not None
        deps = []
        for r in reads:
            deps.extend(r.writers)
        for w in writes:
            deps.extend(w.readers)
            if is_dma and not w.readers and w.writers and all(x.is_dma for x in w.writers):
                pass
            else:
                deps.extend(w.writers)
        seen = set()
        for d in deps:
            if id(d) in seen:
                continue
            seen.add(id(d))
            if d.eng == "pe" and eng == "pe" and not d.is_dma and not is_dma:
                continue
            op.deps.append(d)
        if is_dma:
            op.is_dma = True
            dma_region.dma_count += 16
            op.token = (dma_region.chan, dma_region.dma_count)
            self.final_tokens[id(dma_region.chan)] = op.token
            self.last_dma[id(dma_region.chan)] = op
        else:
            self.count[eng] += 1
            op.idx = self.count[eng]
        for r in reads:
            r.readers.append(op)
        for w in writes:
            if is_dma and not w.readers and w.writers and all(x.is_dma for x in w.writers):
                w.writers.append(op)
            else:
                w.writers = [op]
            w.readers = []
        self.ops[eng].append(op)
        return op

    def op(self, eng, fn, reads=(), writes=(), name=""):
        return self._add(eng, fn, [x.r if isinstance(x, Tl) else x for x in reads],
                         [x.r if isinstance(x, Tl) else x for x in writes], name)

    def dma(self, eng, fn, dst, reads=(), writes=None, name="", serialize=False):
        dst_r = dst.r if isinstance(dst, Tl) else dst
        w = [dst_r] if writes is None else [x.r if isinstance(x, Tl) else x for x in writes]
        rd = [x.r if isinstance(x, Tl) else x for x in reads]
        if serialize:
            rd = rd + [dst_r]
        op = self._add(eng, fn, rd, w, name, dma_region=dst_r)
        op.chain = serialize
        return op

    def barrier(self, dummy):
        lasts = []
        for e in ("pe", "act", "dve", "pool"):
            for o in reversed(self.ops[e]):
                if not o.is_dma and o.kind == "op":
                    lasts.append(o)
                    break
        dmas = list(self.last_dma.values())
        new_ops = []
        for e in ("act", "dve", "pool"):
            if e == "act":
                fn = (lambda en: en.memzero(dummy["act"].t[:]))
            else:
                fn = (lambda en, e=e: en.memset(dummy[e].t[:], 0.0))
            new_ops.append(self.op(e, fn, writes=[dummy[e]]))
        new_ops.append(self.dma("sp", lambda en: en.dma_start(out=dummy["sp"].t[:], in_=dummy["src"]), dummy["sp"]))
        for op in new_ops:
            for d in lasts + dmas:
                if d is not op and all(d is not x for x in op.deps):
                    op.deps.append(d)

    def cond_begin(self, flag_tl, flag_ap):
        if not hasattr(self, "_cstack"):
            self._cstack = []
        self._cstack.append({e: len(self.ops[e]) for e in self.ENGS})
        for e in self.ENGS:
            m = Op(e, None, "cbegin")
            m.kind = "cbegin"
            m.flag_ap = flag_ap
            m.deps = list(flag_tl.r.writers)
            self.ops[e].append(m)

    def cond_end(self):
        cstart = self._cstack.pop()
        body = set()
        for e in self.ENGS:
            for o in self.ops[e][cstart[e] + 1:]:
                body.add(id(o))
        ext_ops = []
        for e in self.ENGS:
            for o in self.ops[e][cstart[e] + 1:]:
                for d in o.deps:
                    if id(d) not in body:
                        only = e if (o.chain and d.chain and d.is_dma and o.is_dma and d.token[0] is o.token[0]) else None
                        ext_ops.append((d, only))
        for e in self.ENGS:
            m = Op(e, None, "cend")
            m.kind = "cend"
            m.ext = ext_ops
            self.ops[e].append(m)

    def _tok(self, d):
        if d.is_dma:
            return d.token
        e = d.eng
        ep = (d.idx - 1) // EPOCH
        return (self.sems[e][ep], (d.idx - 1) % EPOCH + 1)

    def emit(self):
        nc = self.nc
        for e in self.ENGS:
            n_ep = (self.count[e] + EPOCH - 1) // EPOCH
            for i in range(n_ep):
                self.sems[e].append(self.sem(f"s_{e}{i}"))
        prog = self

        def run(e, engine):
            waited = {}

            def emit_op(op):
                need = {}
                for d in op.deps:
                    sem, val = prog._tok(d)
                    k = id(sem)
                    if need.get(k, (sem, 0))[1] < val:
                        need[k] = (sem, val)
                for k, (sem, val) in need.items():
                    if waited.get(k, 0) >= val:
                        continue
                    waited[k] = val
                    engine.wait_ge(sem, val)
                if op.kind != "op":
                    return
                ins = op.fn(engine)
                if op.is_dma:
                    ins.then_inc(op.token[0], 16)
                else:
                    ep = (op.idx - 1) // EPOCH
                    ins.then_inc(prog.sems[e][ep], 1)

            ops = prog.ops[e]

            def emit_range(lo, hi):
                i = lo
                while i < hi:
                    op = ops[i]
                    if op.kind == "cbegin":
                        depth = 1
                        j = i + 1
                        while True:
                            if ops[j].kind == "cbegin":
                                depth += 1
                            elif ops[j].kind == "cend":
                                depth -= 1
                                if depth == 0:
                                    break
                            j += 1
                        real = [b for b in ops[i + 1:j] if b.kind == "op"]
                        if real:
                            emit_op(op)
                            incs = {}
                            for b in real:
                                if b.is_dma:
                                    sem, n = b.token[0], 16
                                else:
                                    sem, n = prog.sems[e][(b.idx - 1) // EPOCH], 1
                                k = id(sem)
                                incs[k] = (sem, incs.get(k, (sem, 0))[1] + n)
                            with engine.register() as freg:
                                engine.reg_load(freg, op.flag_ap)
                                saved = dict(waited)
                                with engine.If_eq(freg, 1):
                                    emit_range(i + 1, j)
                                waited.clear()
                                waited.update(saved)
                                with engine.Else():
                                    ext = {}
                                    for d_, only_ in ops[j].ext:
                                        if only_ is not None and only_ != e:
                                            continue
                                        sem_, v_ = prog._tok(d_)
                                        if ext.get(id(sem_), (sem_, 0))[1] < v_:
                                            ext[id(sem_)] = (sem_, v_)
                                    pre = None
                                    for o in reversed(ops[:i]):
                                        if o.kind == "op" and not o.is_dma:
                                            pre = o
                                            break
                                    if pre is not None:
                                        sem, v = prog._tok(pre)
                                        if ext.get(id(sem), (sem, 0))[1] < v:
                                            ext[id(sem)] = (sem, v)
                                    for k, (sem, v) in ext.items():
                                        if waited.get(k, 0) >= v:
                                            continue
                                        engine.wait_ge(sem, v)
                                    for sem, n in incs.values():
                                        engine.sem_inc(sem, n)
                        i = j + 1
                        continue
                    if op.kind == "cend":
                        i += 1
                        continue
                    emit_op(op)
                    i += 1

            emit_range(0, len(ops))
            if e == "sp":
                for sem, val in prog.final_tokens.values():
                    engine.wait_ge(sem, val)
                for ce in ("pe", "act", "dve", "pool"):
                    c = prog.count[ce]
                    if c:
                        ep = (c - 1) // EPOCH
                        engine.wait_ge(prog.sems[ce][ep], (c - 1) % EPOCH + 1)

        with nc.Block() as block:
            @block.tensor
            def _(eng):
                run("pe", eng)

            @block.scalar
            def _(eng):
                run("act", eng)

            @block.vector
            def _(eng):
                run("dve", eng)

            @block.gpsimd
            def _(eng):
                run("pool", eng)

            @block.sync
            def _(eng):
                run("sp", eng)


def k_mm(lst):
    def f(e):
        ins = None
        for (o, l, r, s, t) in lst:
            ins = e.matmul(o, lhsT=l, rhs=r, start=s, stop=t)
        return ins
    return f


def k_tr(lst):
    def f(e):
        ins = None
        for (o, i, idn) in lst:
            ins = e.transpose(out=o, in_=i, identity=idn)
        return ins
    return f


def k_act(out, in_, func, **kw):
    return lambda e: e.activation(out=out, in_=in_, func=func, **kw)


def k_tt(out, a, b, op):
    return lambda e: e.tensor_tensor(out=out, in0=a, in1=b, op=op)


def k_ts(out, a, s1, op0, s2=None, op1=None):
    if op1 is None:
        return lambda e: e.tensor_scalar(out=out, in0=a, scalar1=s1, scalar2=None, op0=op0)
    return lambda e: e.tensor_scalar(out=out, in0=a, scalar1=s1, scalar2=s2, op0=op0, op1=op1)


def k_stt(out, a, s, b, op0, op1):
    return lambda e: e.scalar_tensor_tensor(out=out, in0=a, scalar=s, in1=b, op0=op0, op1=op1)


def k_cp(out, in_):
    return lambda e: e.tensor_copy(out=out, in_=in_)


def k_acp(out, in_):
    return lambda e: e.copy(out=out, in_=in_)


def k_red(out, in_):
    return lambda e: e.reduce_sum(out=out, in_=in_, axis=AX.X)


def k_rcp(out, in_):
    return lambda e: e.reciprocal(out=out, in_=in_)


def k_dma(out, in_):
    return lambda e: e.dma_start(out=out, in_=in_)


def k_memset(ap, v):
    return lambda e: e.memset(ap, v)


G_U, G_V, G_Q, G_ZF, G_ZB, G_I, G_G = range(7)
NPC = 16 + 48 + 24 + 4 + 512
PC_C, PC_BMOD, PC_N1, PC_N2, PC_FN, PC_LNG, PC_B1 = 0, 16, 64, 72, 80, 88, 92
NPR = 3072
CS_ID, CS_TMF, CS_TMB, CS_XF, CS_XB = 0, 128, 256, 384, 386
CS_LS, CS_PAD, CS_TOK, CS_IOTA = 388, 516, 517, 533
NCST = 533 + 2048


def _consts():
    s = np.arange(128)[:, None]
    t = np.arange(128)[None, :]
    cst = np.zeros((128, NCST), np.float32)
    cst[:, CS_ID:CS_ID + 128] = np.eye(128, dtype=np.float32)
    cst[:, CS_TMF:CS_TMF + 128] = (s <= t).astype(np.float32) - (s <= 63).astype(np.float32)
    cst[:, CS_TMB:CS_TMB + 128] = (s >= t).astype(np.float32) - (s >= 64).astype(np.float32)
    cst[:, CS_XF] = (s[:, 0] >= 64)
    cst[:, CS_XF + 1] = (s[:, 0] <= 63)
    cst[:, CS_XB] = (s[:, 0] <= 63)
    cst[:, CS_XB + 1] = (s[:, 0] >= 64)
    cst[:, CS_LS:CS_LS + 128] = (s < t).astype(np.float32)
    cst[:, CS_IOTA:CS_IOTA + 2048] = np.arange(2048, dtype=np.float32)[None, :]
    cst[:, CS_PAD] = 2048 + np.arange(128)
    cst[:, CS_TOK:CS_TOK + 16] = np.arange(128)[:, None] + 128 * np.arange(16)[None, :]
    mf = np.tile((s <= t).astype(np.float32), (1, 4))
    mb = np.tile((s >= t).astype(np.float32), (1, 4))
    masks = np.concatenate([mf, mb], axis=1)
    sel = np.zeros((32, 32, 128), np.float32)
    for e in range(32):
        sel[e, e, :] = 1.0
    return cst, masks, sel.reshape(32, 4096)


def col_layout(v):
    return np.ascontiguousarray(v.reshape(-1, 128).T)


def build_program(stage=99, dbg_cols=0):
    nc = bass.Bass("TRN2", target_bir_lowering=False)

    def D(name, shape, kind="ExternalInput"):
        return nc.dram_tensor(name, list(shape), F32, kind=kind).ap()

    xT_d = D("xT", [1024, 2048])
    ctxT_d = D("ctxT", [1024, 256])
    pcol_d = D("pcol", [128, NPC])
    prow_d = D("prow", [1, NPR])
    wmod_d = D("w_mod", [1024, 6144])
    win_d = D("w_in", [1024, 3584])
    wout_d = D("w_out", [1024, 1024])
    rw_d = D("router_w", [1024, 32])
    rb_d = D("router_b", [32, 1])
    w1_d = D("w1", [32, 1024, 2048]) if stage >= 2 else None
    w2_d = D("w2", [32, 1024, 1024]) if stage >= 2 else None
    b2_d = D("b2", [32, 1024])
    wsT_d = D("sgu_wT", [128, 512])
    cst_d = D("cst", [128, NCST])
    mask_d = D("masks", [128, 1024])
    sel_d = D("sel", [32, 4096])
    out_d = D("outT", [1024, 2048], kind="ExternalOutput")
    dbg_d = D("dbg", [128, dbg_cols], kind="ExternalOutput") if dbg_cols else None

    with ExitStack() as st:
        P = Prog(nc, st)
        dbg_state = {"off": 0}
        Rdbg = P.region("dbgout", dma=True) if dbg_cols else None
        if dbg_cols:
            dbg_state["dtmp"] = P.tile("dbg_bounce", [128, 512], F32)

        def dump(tl, ap, ncols):
            o = dbg_state["off"]
            P.dma("sp", k_dma(dbg_d[:, o:o + ncols], ap), Rdbg, reads=[tl], writes=[Rdbg])
            dbg_state["off"] = o + ncols

        xT = P.tile("xT", [128, 8, 2048], F32)
        RX = [P.region(f"x{c}") for c in range(16)]
        Rxload = P.region("xload", dma=True)
        pc = P.tile("pc", [128, NPC], F32, dma=True)
        cst = P.tile("cst", [128, CS_IOTA], F32, dma=True)
        identb = P.tile("identb", [128, 128], BF16, dma=True)
        onesb = P.tile("onesb", [128, 128], BF16)
        modc = P.tile("modc", [128, 48, 2], F32)
        mc = P.tile("mc", [128, 8, 8], F32)
        dummy = {e: P.tile(f"dummy_{e}", [128, 8], F32) for e in ("act", "dve", "pool")}
        dummy["sp"] = P.tile("dummy_sp", [128, 8], F32, dma=True)
        dummy["src"] = cst_d[:, 0:8]
        PSB = [P.psum(f"psb{i}", [128, 512], F32) for i in range(8)]
        ps_state = {"i": 0}

        def bank():
            b = PSB[ps_state["i"] % 8]
            ps_state["i"] += 1
            return b

        ident = cst.t[:, CS_ID:CS_ID + 128]

        P.dma("sp", k_dma(pc.t[:], pcol_d), pc)
        P.dma("sp", k_dma(cst.t[:], cst_d[:, 0:CS_IOTA]), cst)
        P.dma("pool", k_dma(identb.t[:], cst_d[:, CS_ID:CS_ID + 128]), identb)
        P.op("pool", k_memset(onesb.t[:], 1.0), writes=[onesb])
        for k in range(8):
            P.dma("sp", k_dma(xT.t[:, k, :], xT_d[k * 128:(k + 1) * 128, :]), Rxload,
                  writes=[Rxload] + RX)

        with ExitStack() as sa:
            cS = P.tile("cS", [128, 16], F32, stack=sa)
            wm = [P.tile(f"wm{i}", [128, 8, 768], F32, dma=True, stack=sa) for i in range(2)]
            P.op("act", k_act(cS.t[:], pc.t[:, PC_C:PC_C + 16], AF.Silu), reads=[pc], writes=[cS])
            psA = bank()
            psA_v = psA.t[:, 0:96].rearrange("p (j c) -> p j c", c=2)
            wmod_v = wmod_d.rearrange("(k p) c -> p k c", p=128)
            for blk in range(8):
                w = wm[blk % 2]
                P.dma("sp", k_dma(w.t[:], wmod_v[:, :, blk * 768:(blk + 1) * 768]), w)
                for jj in range(6):
                    j = blk * 6 + jj
                    P.op("pe", k_mm([(psA_v[:, j, :], w.t[:, k, jj * 128:(jj + 1) * 128],
                                      cS.t[:, 2 * k:2 * k + 2], k == 0, k == 7) for k in range(8)]),
                         reads=[w, cS], writes=[psA])
            P.op("dve", k_tt(modc.t[:], psA_v,
                             pc.t[:, PC_BMOD:PC_BMOD + 48].unsqueeze(2).to_broadcast([128, 48, 2]), ALU.add),
                 reads=[psA, pc], writes=[modc])
            n1 = pc.t[:, PC_N1:PC_N1 + 8]
            n2 = pc.t[:, PC_N2:PC_N2 + 8]
            P.op("dve", k_stt(mc.t[:, 0, :], modc.t[:, 8:16, 0], 1.0, n1, ALU.add, ALU.mult), reads=[modc, pc], writes=[mc])
            P.op("dve", k_cp(mc.t[:, 1, :], modc.t[:, 0:8, 0]), reads=[modc], writes=[mc])
            P.op("dve", k_stt(mc.t[:, 2, :], modc.t[:, 8:16, 1], 1.0, n1, ALU.add, ALU.mult), reads=[modc, pc], writes=[mc])
            P.op("dve", k_cp(mc.t[:, 3, :], modc.t[:, 0:8, 1]), reads=[modc], writes=[mc])
            P.op("dve", k_cp(mc.t[:, 4, :], modc.t[:, 16:24, 0]), reads=[modc], writes=[mc])
            P.op("dve", k_stt(mc.t[:, 5, :], modc.t[:, 32:40, 0], 1.0, n2, ALU.add, ALU.mult), reads=[modc, pc], writes=[mc])
            P.op("dve", k_cp(mc.t[:, 6, :], modc.t[:, 24:32, 0]), reads=[modc], writes=[mc])
            P.op("dve", k_cp(mc.t[:, 7, :], modc.t[:, 40:48, 0]), reads=[modc], writes=[mc])
            P.barrier(dummy)
        if stage == 0:
            dump(mc, mc.t[:].rearrange("p a b -> p (a b)"), 64)

        def dump_any(regs, ap2d, ncols, stack):
            if not dbg_cols:
                return
            if "dtmp" not in dbg_state:
                dbg_state["dtmp"] = P.tile("dbg_bounce", [128, 512], F32, stack=stack)
            tmp = dbg_state["dtmp"]
            for o in range(0, ncols, 512):
                n = min(512, ncols - o)
                P.op("dve", k_cp(tmp.t[:, 0:n], ap2d[:, o:o + n]), reads=regs, writes=[tmp])
                dump(tmp, tmp.t[:, 0:n], n)

        def bc8(col_ap, n):
            return col_ap.unsqueeze(2).to_broadcast([128, 8, n])

        def make_h(W, src_ap, src_regs, A_ap, sh_ap, hT_tl, hT_ap, n=128):
            P.op("act", k_act(W["sq"].t[:, :, 0:n], src_ap, AF.Square), reads=src_regs, writes=[W["sq"]])
            b = bank()
            P.op("pe", k_mm([(b.t[:, 0:n], onesb.t[:], W["sq"].t[:, k, 0:n], k == 0, k == 7) for k in range(8)]),
                 reads=[W["sq"], onesb], writes=[b])
            P.op("act", k_act(W["rs"].t[:, 0:n], b.t[:, 0:n], AF.Sqrt, scale=1.0 / 1024.0, bias=EPS),
                 reads=[b], writes=[W["rs"]])
            P.op("dve", k_rcp(W["rs"].t[:, 0:n], W["rs"].t[:, 0:n]), reads=[W["rs"]], writes=[W["rs"]])
            P.op("dve", k_tt(W["t1"].t[:, :, 0:n], src_ap, W["rs"].t[:, 0:n].unsqueeze(1).to_broadcast([128, 8, n]), ALU.mult),
                 reads=src_regs + [W["rs"]], writes=[W["t1"]])
            P.op("pool", k_tt(W["t1"].t[:, :, 0:n], W["t1"].t[:, :, 0:n], bc8(A_ap, n), ALU.mult),
                 reads=[W["t1"], mc], writes=[W["t1"]])
            P.op("dve", k_tt(hT_ap, W["t1"].t[:, :, 0:n], bc8(sh_ap, n), ALU.add),
                 reads=[W["t1"], mc], writes=[hT_tl])

        if stage >= 1:
            build_sublayer1(nc, P, st, locals())
        if stage >= 2:
            build_moe(nc, P, st, locals())
        else:
            for k in range(8):
                P.dma("sp", k_dma(out_d[k * 128:(k + 1) * 128, :], xT.t[:, k, :]), P.region(f"o{k}", dma=True), reads=RX)
        P.emit()
    return nc


def build_sublayer1(nc, P, st, env):
    xT, RX, pc, cst, identb, onesb, mc, dummy = (env[k] for k in ("xT", "RX", "pc", "cst", "identb", "onesb", "mc", "dummy"))
    bank, make_h, dump, stage = env["bank"], env["make_h"], env["dump"], env["stage"]
    ctxT_d, prow_d, win_d, wout_d, wsT_d, mask_d = (env[k] for k in ("ctxT_d", "prow_d", "win_d", "wout_d", "wsT_d", "mask_d"))
    ident = cst.t[:, CS_ID:CS_ID + 128]
    win_v = win_d.rearrange("(k p) c -> p k c", p=128)
    wout_v = wout_d.rearrange("(k p) c -> p k c", p=128)

    with ExitStack() as s1, ExitStack() as sfb:
        def T(name, shape, dt, dma=False, stack=None):
            return P.tile(name, shape, dt, dma=dma, stack=stack or s1)

        def TF(name, shape, dt, dma=False):
            return P.tile(name, shape, dt, dma=dma, stack=sfb)

        hTctx = T("hTctx", [128, 8, 256], BF16)
        win = T("win", [128, 8, 2560], BF16, dma=True)
        ybT = T("ybT", [128, 4, 2048], BF16)
        RYB = [P.region(f"yb{c}") for c in range(16)]
        W = {
            "sq": T("w_sq", [128, 8, 128], BF16),
            "rs": T("w_rs", [128, 128], F32),
            "t1": T("w_t1", [128, 8, 128], F32),
        }
        hT = T("hT", [128, 8, 128], BF16)
        Sf = TF("Sf", [128, 16, 512], BF16)
        RSf = [P.region(f"Sf{c}") for c in range(16)]
        masks = TF("masks", [128, 1024], BF16, dma=True)
        LB = {d: TF(f"LB{d}", [128, 512], F32, dma=True) for d in "fb"}
        OML = {d: TF(f"OML{d}", [128, 512], F32, dma=True) for d in "fb"}
        HN = TF("HN", [128, 512], F32, dma=True)
        sig = TF("sig", [128, 512], F32)
        ff = TF("ff", [128, 512], F32)
        logf = {d: TF(f"logf{d}", [128, 512], F32) for d in "fb"}
        kk = TF("kk", [128, 512], F32)
        einv = TF("einv", [128, 512], F32)
        Kt = {d: TF(f"Kt{d}", [128, 512], BF16) for d in "fb"}
        V = TF("V", [128, 512], BF16)
        Aprev = TF("Aprev", [128, 4], F32)
        dcol = TF("dcol", [128, 4], F32)
        Sp = TF("Sp", [128, 512], F32)
        Sbf = TF("Sbf", [128, 512], BF16)
        M = TF("M", [128, 512], F32)

        P.dma("pool", k_dma(masks.t[:], mask_d), masks)
        P.dma("sp", k_dma(HN.t[:], prow_d[:, 2048:2560].partition_broadcast(128)), HN)
        for i, d in enumerate("fb"):
            P.dma("sp", k_dma(LB[d].t[:], prow_d[:, (2 * i) * 512:(2 * i + 1) * 512].partition_broadcast(128)), LB[d])
            P.dma("sp", k_dma(OML[d].t[:], prow_d[:, (2 * i + 1) * 512:(2 * i + 2) * 512].partition_broadcast(128)), OML[d])
            P.op("dve", k_tt(OML[d].t[:], LB[d].t[:], OML[d].t[:], ALU.subtract), reads=[LB[d], OML[d]], writes=[OML[d]])
            P.op("act", k_act(LB[d].t[:], OML[d].t[:], AF.Sigmoid), reads=[OML[d]], writes=[LB[d]])
            P.op("dve", k_ts(OML[d].t[:], LB[d].t[:], -1.0, ALU.mult, 1.0, ALU.add), reads=[LB[d]], writes=[OML[d]])
        with ExitStack() as s0:
            ctxT = P.tile("ctxT", [128, 8, 256], F32, dma=True, stack=s0)
            for k in range(8):
                P.dma("sp", k_dma(ctxT.t[:, k, :], ctxT_d[k * 128:(k + 1) * 128, :]), ctxT)
            for hh in range(2):
                make_h(W, ctxT.t[:, :, hh * 128:(hh + 1) * 128], [ctxT], mc.t[:, 2, :], mc.t[:, 3, :], hTctx,
                       hTctx.t[:, :, hh * 128:(hh + 1) * 128], n=128)
            P.barrier(dummy)

        if env["dbg_cols"] and stage < 2:
            env["dump_any"]([hTctx], hTctx.t[:].rearrange("p a b -> p (a b)"), 2048, s1)
            env["dump_any"]([LB["f"]], LB["f"].t[:], 512, s1)
            env["dump_any"]([LB["b"]], LB["b"].t[:], 512, s1)

        def load_win(groups):
            for slot, g in enumerate(groups):
                P.dma("pool", k_dma(win.t[:, :, slot * 512:(slot + 1) * 512], win_v[:, :, g * 512:(g + 1) * 512]), win)

        def get_hT(kind, c):
            if kind == "ctx":
                return hTctx, hTctx.t[:, :, c * 128:(c + 1) * 128]
            make_h(W, xT.t[:, :, c * 128:(c + 1) * 128], [RX[c]], mc.t[:, 0, :], mc.t[:, 1, :], hT, hT.t[:], n=128)
            return hT, hT.t[:]

        def proj_tok(h_tl, h_ap, slot):
            b = bank()
            P.op("pe", k_mm([(b.t[:, 0:512], h_ap[:, k, :], win.t[:, k, slot * 512:(slot + 1) * 512], k == 0, k == 7)
                             for k in range(8)]), reads=[h_tl, win], writes=[b])
            return b

        def proj_ch(h_tl, h_ap, slot):
            b = bank()
            lst = []
            for h in range(4):
                for k in range(8):
                    lst.append((b.t[:, h * 128:(h + 1) * 128], win.t[:, k, slot * 512 + h * 128: slot * 512 + (h + 1) * 128],
                                h_ap[:, k, :], k == 0, k == 7))
            P.op("pe", k_mm(lst), reads=[h_tl, win], writes=[b])
            return b

        def decay_tok(zb_bank, d):
            tm = cst.t[:, (CS_TMF if d == "f" else CS_TMB):(CS_TMF if d == "f" else CS_TMB) + 128]
            P.op("act", k_act(sig.t[:], zb_bank.t[:, 0:512], AF.Sigmoid), reads=[zb_bank], writes=[sig])
            P.op("dve", k_tt(ff.t[:], sig.t[:], OML[d].t[:], ALU.mult), reads=[sig, OML[d]], writes=[ff])
            P.op("pool", k_tt(ff.t[:], ff.t[:], LB[d].t[:], ALU.add), reads=[ff, LB[d]], writes=[ff])
            P.op("act", k_act(logf[d].t[:], ff.t[:], AF.Ln), reads=[ff], writes=[logf[d]])
            P.op("pool", k_ts(kk.t[:], ff.t[:], -1.0, ALU.mult, 1.0, ALU.add), reads=[ff], writes=[kk])
            b = bank()
            P.op("pe", k_mm([(b.t[:, 0:512], tm, logf[d].t[:], True, True)]), reads=[cst, logf[d]], writes=[b])
            P.op("act", k_act(einv.t[:], b.t[:, 0:512], AF.Exp, scale=-1.0), reads=[b], writes=[einv])
            P.op("dve", k_tt(Kt[d].t[:], kk.t[:], einv.t[:], ALU.mult), reads=[kk, einv], writes=[Kt[d]])

        def state_step(d, p, store_c, need_kv):
            xo = CS_XF if d == "f" else CS_XB
            b = bank()
            cm = b.t[:, 0:8].rearrange("p (h c) -> p h c", c=2)
            P.op("pe", k_mm([(cm[:, h, :], logf[d].t[:, h * 128:(h + 1) * 128], cst.t[:, xo:xo + 2], True, True)
                             for h in range(4)]), reads=[logf[d], cst], writes=[b])
            if p > 0:
                P.op("dve", k_tt(dcol.t[:], cm[:, :, 1], Aprev.t[:], ALU.add), reads=[b, Aprev], writes=[dcol])
                P.op("act", k_act(dcol.t[:], dcol.t[:], AF.Exp), reads=[dcol], writes=[dcol])
                P.op("dve", k_tt(Sp.t[:].rearrange("p (h v) -> p h v", h=4), M.t[:].rearrange("p (h v) -> p h v", h=4),
                                 dcol.t[:].unsqueeze(2).to_broadcast([128, 4, 128]), ALU.mult),
                     reads=[M, dcol], writes=[Sp])
                if store_c is not None:
                    if d == "f":
                        P.op("act", k_acp(Sf.t[:, store_c, :], Sp.t[:]), reads=[Sp], writes=[RSf[store_c]])
                    else:
                        P.op("act", k_acp(Sbf.t[:], Sp.t[:]), reads=[Sp], writes=[Sbf])
            P.op("dve", k_cp(Aprev.t[:], cm[:, :, 0]), reads=[b], writes=[Aprev])
            if need_kv:
                b2 = bank()
                P.op("pe", k_mm([(b2.t[:, h * 128:(h + 1) * 128], Kt[d].t[:, h * 128:(h + 1) * 128],
                                  V.t[:, h * 128:(h + 1) * 128], True, True) for h in range(4)]),
                     reads=[Kt[d], V], writes=[b2])
                if p > 0:
                    P.op("dve", k_tt(M.t[:], b2.t[:, 0:512], Sp.t[:], ALU.add), reads=[b2, Sp], writes=[M])
                else:
                    P.op("dve", k_cp(M.t[:], b2.t[:, 0:512]), reads=[b2], writes=[M])

        load_win([G_ZF, G_I])
        order_f = [("ctx", 0), ("ctx", 1)] + [("lat", c) for c in range(16)]
        for p, (kind, c) in enumerate(order_f):
            h_tl, h_ap = get_hT(kind, c)
            zb_ = proj_tok(h_tl, h_ap, 0)
            decay_tok(zb_, "f")
            need_kv = p < 17
            if need_kv:
                ib_ = proj_tok(h_tl, h_ap, 1)
                P.op("act", k_acp(V.t[:], ib_.t[:, 0:512]), reads=[ib_], writes=[V])
            state_step("f", p, c if kind == "lat" else None, need_kv)

        if env["dbg_cols"] and stage < 2:
            env["dump_any"](RSf, Sf.t[:].rearrange("p a b -> p (a b)"), 8192, s1)

        if stage >= 1.2:
            sweep_b(nc, P, sfb, locals(), env)
        P.barrier(dummy)
        sfb.close()
        if stage >= 1.3:
            with ExitStack() as sc:
                sweep_c(nc, P, sc, locals(), env)
                P.barrier(dummy)


def sweep_b(nc, P, s1, L, env):
    xT, RX, pc, cst, identb, mc = (env[k] for k in ("xT", "RX", "pc", "cst", "identb", "mc"))
    bank, dump, stage = env["bank"], env["dump"], env["stage"]
    win, Sf, RSf, ybT, RYB, masks, LB, OML, HN = (L[k] for k in ("win", "Sf", "RSf", "ybT", "RYB", "masks", "LB", "OML", "HN"))
    logf, Kt, V, Sbf = (L[k] for k in ("logf", "Kt", "V", "Sbf"))

    def T(name, shape, dt, dma=False):
        return P.tile(name, shape, dt, dma=dma, stack=s1)
    load_win, get_hT, proj_tok, proj_ch, decay_tok, state_step = (L[k] for k in ("load_win", "get_hT", "proj_tok", "proj_ch", "decay_tok", "state_step"))

    ET = T("ET", [128, 512], F32)
    QtT = {d: T(f"QtT{d}", [128, 512], BF16) for d in "fb"}
    KtT = {d: T(f"KtT{d}", [128, 512], BF16) for d in "fb"}
    scT = {d: T(f"scT{d}", [128, 512], BF16) for d in "fb"}
    osq, on, sg, gs = L["sig"], L["ff"], L["einv"], L["kk"]
    ss4 = T("ss4", [128, 4], F32)
    yb = T("yb", [128, 512], BF16)

    for d in "fb":
        P.op("pool", k_memset(scT[d].t[:], 0.0), writes=[scT[d]])
    load_win([G_ZB, G_I, G_ZF, G_Q, G_G])
    order_b = [("ctx", 1), ("ctx", 0)] + [("lat", c) for c in range(15, -1, -1)]
    for p, (kind, c) in enumerate(order_b):
        h_tl, h_ap = get_hT(kind, c)
        zbb = proj_tok(h_tl, h_ap, 0)
        decay_tok(zbb, "b")
        ib_ = proj_tok(h_tl, h_ap, 1)
        P.op("act", k_acp(V.t[:], ib_.t[:, 0:512]), reads=[ib_], writes=[V])
        state_step("b", p, c if kind == "lat" else None, p < 17)
        if kind != "lat":
            continue
        zfb = proj_tok(h_tl, h_ap, 2)
        decay_tok(zfb, "f")
        qb = proj_ch(h_tl, h_ap, 3)
        for d in "fb":
            tm = cst.t[:, (CS_TMF if d == "f" else CS_TMB):(CS_TMF if d == "f" else CS_TMB) + 128]
            b = bank()
            P.op("pe", k_mm([(b.t[:, h * 128:(h + 1) * 128], logf[d].t[:, h * 128:(h + 1) * 128], tm, True, True)
                             for h in range(4)]), reads=[logf[d], cst], writes=[b])
            P.op("act", k_act(ET.t[:], b.t[:, 0:512], AF.Exp), reads=[b], writes=[ET])
            P.op("dve", k_tt(QtT[d].t[:], qb.t[:, 0:512], ET.t[:], ALU.mult), reads=[qb, ET], writes=[QtT[d]])
            bt = bank()
            btv = bt.t[:].bitcast(BF16)
            P.op("pe", k_tr([(btv[:, h * 128:(h + 1) * 128], Kt[d].t[:, h * 128:(h + 1) * 128], identb.t[:]) for h in range(4)]),
                 reads=[Kt[d], identb], writes=[bt])
            P.op("act", k_acp(KtT[d].t[:], btv[:, 0:512]), reads=[bt], writes=[KtT[d]])
            bs_ = bank()
            P.op("pe", k_mm([(bs_.t[:, h * 128:(h + 1) * 128], KtT[d].t[:, h * 128:(h + 1) * 128],
                              QtT[d].t[:, h * 128:(h + 1) * 128], True, True) for h in range(4)]),
                 reads=[KtT[d], QtT[d]], writes=[bs_])
            mo = 0 if d == "f" else 512
            v3 = lambda ap: ap.rearrange("p (h t) -> p h t", h=4)
            pa, pb = (slice(0, 64), slice(64, 128)) if d == "f" else (slice(64, 128), slice(0, 64))
            cb = slice(64, 128) if d == "f" else slice(0, 64)
            P.op("dve", k_tt(scT[d].t[pa, :], bs_.t[pa, 0:512], masks.t[pa, mo:mo + 512], ALU.mult),
                 reads=[bs_, masks], writes=[scT[d]])
            P.op("dve", k_tt(v3(scT[d].t[pb, :])[:, :, cb], v3(bs_.t[pb, 0:512])[:, :, cb],
                             v3(masks.t[pb, mo:mo + 512])[:, :, cb], ALU.mult),
                 reads=[bs_, masks, scT[d]], writes=[scT[d]])
        bo = bank()
        lst = []
        for h in range(4):
            hs = slice(h * 128, (h + 1) * 128)
            lst.append((bo.t[:, hs], scT["f"].t[:, hs], V.t[:, hs], True, False))
            lst.append((bo.t[:, hs], scT["b"].t[:, hs], V.t[:, hs], False, False))
            lst.append((bo.t[:, hs], QtT["f"].t[:, hs], Sf.t[:, c, hs], False, False))
            lst.append((bo.t[:, hs], QtT["b"].t[:, hs], Sbf.t[:, hs], False, True))
        P.op("pe", k_mm(lst), reads=[scT["f"], scT["b"], V, QtT["f"], QtT["b"], RSf[c], Sbf], writes=[bo])
        P.op("act", k_act(osq.t[:], bo.t[:, 0:512], AF.Square), reads=[bo], writes=[osq])
        P.op("dve", k_red(ss4.t[:], osq.t[:].rearrange("p (h v) -> p h v", h=4)), reads=[osq], writes=[ss4])
        P.op("act", k_act(ss4.t[:], ss4.t[:], AF.Sqrt, scale=1.0 / 128.0, bias=EPS), reads=[ss4], writes=[ss4])
        P.op("dve", k_rcp(ss4.t[:], ss4.t[:]), reads=[ss4], writes=[ss4])
        P.op("dve", k_tt(on.t[:].rearrange("p (h v) -> p h v", h=4), bo.t[:, 0:512].rearrange("p (h v) -> p h v", h=4),
                         ss4.t[:].unsqueeze(2).to_broadcast([128, 4, 128]), ALU.mult), reads=[bo, ss4], writes=[on])
        P.op("pool", k_tt(on.t[:], on.t[:], HN.t[:], ALU.mult), reads=[on, HN], writes=[on])
        gb = proj_tok(h_tl, h_ap, 4)
        P.op("act", k_act(sg.t[:], gb.t[:, 0:512], AF.Sigmoid), reads=[gb], writes=[sg])
        P.op("dve", k_tt(gs.t[:], gb.t[:, 0:512], sg.t[:], ALU.mult), reads=[gb, sg], writes=[gs])
        P.op("dve", k_tt(yb.t[:], on.t[:], gs.t[:], ALU.mult), reads=[on, gs], writes=[yb])
        by = bank()
        byv = by.t[:].bitcast(BF16)
        P.op("pe", k_tr([(byv[:, h * 128:(h + 1) * 128], yb.t[:, h * 128:(h + 1) * 128], identb.t[:]) for h in range(4)]),
             reads=[yb, identb], writes=[by])
        P.op("act", k_acp(ybT.t[:, :, c * 128:(c + 1) * 128], byv[:, 0:512].rearrange("p (h t) -> p h t", h=4)),
             reads=[by], writes=[RYB[c]])

    if env["dbg_cols"] and stage < 2:
        env["dump_any"](RYB, ybT.t[:].rearrange("p a b -> p (a b)"), 8192, s1)


def sweep_c(nc, P, s1, L, env):
    xT, RX, pc, cst, identb, mc = (env[k] for k in ("xT", "RX", "pc", "cst", "identb", "mc"))
    bank, dump, stage = env["bank"], env["dump"], env["stage"]
    wout_v = L["wout_v"]
    win, ybT, RYB = (L[k] for k in ("win", "ybT", "RYB"))
    load_win, get_hT, proj_tok, proj_ch = (L[k] for k in ("load_win", "get_hT", "proj_tok", "proj_ch"))
    prow_d, wsT_d = env["prow_d"], env["wsT_d"]

    def T(name, shape, dt, dma=False):
        return P.tile(name, shape, dt, dma=dma, stack=s1)

    BS = T("BS", [128, 512], F32, dma=True)
    WsT = T("WsT", [128, 512], BF16, dma=True)
    P.dma("sp", k_dma(BS.t[:], prow_d[:, 2560:3072].partition_broadcast(128)), BS)
    P.dma("pool", k_dma(WsT.t[:], wsT_d), WsT)

    wout = T("wout", [128, 8, 1024], BF16, dma=True)
    P.dma("pool", k_dma(wout.t[:, :, 0:512], wout_v[:, :, 0:512]), wout)
    P.dma("pool", k_dma(wout.t[:, :, 512:1024], wout_v[:, :, 512:1024]), wout)
    gu = T("gu", [128, 512], F32)
    gv = T("gv", [128, 512], F32)
    cen = T("cen", [128, 512], F32)
    sqc = T("sqc", [128, 512], F32)
    st4 = T("st4", [128, 4], F32)
    vn = T("vn", [128, 512], BF16)
    za = T("za", [128, 512], F32)
    yaT = T("yaT", [128, 4, 128], BF16)
    xtmp = T("xtmp", [128, 8, 128], F32)
    lng = pc.t[:, PC_LNG:PC_LNG + 4]

    load_win([G_U, G_V])
    for c in range(16):
        h_tl, h_ap = get_hT("lat", c)
        ub = proj_ch(h_tl, h_ap, 0)
        P.op("act", k_act(gu.t[:], ub.t[:, 0:512], AF.Gelu_apprx_tanh), reads=[ub], writes=[gu])
        vb = proj_tok(h_tl, h_ap, 1)
        P.op("act", k_act(gv.t[:], vb.t[:, 0:512], AF.Gelu_apprx_tanh), reads=[vb], writes=[gv])
        v3 = lambda t: t.t[:].rearrange("p (h d) -> p h d", h=4)
        b4 = lambda t: t.t[:].unsqueeze(2).to_broadcast([128, 4, 128])
        P.op("dve", k_red(st4.t[:], v3(gv)), reads=[gv], writes=[st4])
        P.op("dve", k_ts(st4.t[:], st4.t[:], 1.0 / 128.0, ALU.mult), reads=[st4], writes=[st4])
        P.op("dve", k_tt(v3(cen), v3(gv), b4(st4), ALU.subtract), reads=[gv, st4], writes=[cen])
        P.op("act", k_act(sqc.t[:], cen.t[:], AF.Square), reads=[cen], writes=[sqc])
        P.op("dve", k_red(st4.t[:], v3(sqc)), reads=[sqc], writes=[st4])
        P.op("act", k_act(st4.t[:], st4.t[:], AF.Sqrt, scale=1.0 / 128.0, bias=EPS), reads=[st4], writes=[st4])
        P.op("dve", k_rcp(st4.t[:], st4.t[:]), reads=[st4], writes=[st4])
        P.op("dve", k_tt(v3(vn), v3(cen), b4(st4), ALU.mult), reads=[cen, st4], writes=[vn])
        bz = bank()
        P.op("pe", k_mm([(bz.t[:, h * 128:(h + 1) * 128], vn.t[:, h * 128:(h + 1) * 128], WsT.t[:, h * 128:(h + 1) * 128], True, True)
                         for h in range(4)]), reads=[vn, WsT], writes=[bz])
        P.op("dve", k_tt(v3(za), bz.t[:, 0:512].rearrange("p (h d) -> p h d", h=4),
                         lng.unsqueeze(2).to_broadcast([128, 4, 128]), ALU.mult), reads=[bz, pc], writes=[za])
        P.op("pool", k_tt(za.t[:], za.t[:], BS.t[:], ALU.add), reads=[za, BS], writes=[za])
        P.op("dve", k_tt(yaT.t[:].rearrange("p h t -> p (h t)"), za.t[:], gu.t[:], ALU.mult), reads=[za, gu], writes=[yaT])
        bw = [bank(), bank()]
        for half in range(2):
            lst = []
            for jj in range(4):
                j = half * 4 + jj
                for k in range(8):
                    rhs = yaT.t[:, k, :] if k < 4 else ybT.t[:, k - 4, c * 128:(c + 1) * 128]
                    lst.append((bw[half].t[:, jj * 128:(jj + 1) * 128], wout.t[:, k, j * 128:(j + 1) * 128], rhs, k == 0, k == 7))
            P.op("pe", k_mm(lst), reads=[yaT, RYB[c], wout], writes=[bw[half]])
            P.op("dve", k_tt(xtmp.t[:, half * 4:(half + 1) * 4, :], bw[half].t[:, 0:512].rearrange("p (j t) -> p j t", j=4),
                             mc.t[:, 4, half * 4:(half + 1) * 4].unsqueeze(2).to_broadcast([128, 4, 128]), ALU.mult),
                 reads=[bw[half], mc], writes=[xtmp])
        P.op("pool", k_tt(xT.t[:, :, c * 128:(c + 1) * 128], xT.t[:, :, c * 128:(c + 1) * 128], xtmp.t[:], ALU.add),
             reads=[xtmp, RX[c]], writes=[RX[c]])


CAP = 256
NBLK = CAP // 128
NROUND = 2048 // CAP
NSINGLE = 3
NROWS = 2048 + 128


def build_moe(nc, P, st, env):
    xT, RX, pc, cst, identb, onesb, mc, dummy = (env[k] for k in ("xT", "RX", "pc", "cst", "identb", "onesb", "mc", "dummy"))
    bank, dump, stage = env["bank"], env["dump"], env["stage"]
    rw_d, rb_d, w1_d, w2_d, b2_d, sel_d, out_d, cst_d = (env[k] for k in ("rw_d", "rb_d", "w1_d", "w2_d", "b2_d", "sel_d", "out_d", "cst_d"))
    ident = cst.t[:, CS_ID:CS_ID + 128]
    RXG = lambda g: RX[g * 4:(g + 1) * 4]
    U32 = mybir.dt.uint32
    I32 = mybir.dt.int32

    h2rows_d = nc.dram_tensor("h2rows", [NROWS, 1024], BF16).ap()
    moe_d = nc.dram_tensor("moe_acc", [NROWS, 1024], F32).ap()
    h2T_d = nc.dram_tensor("h2T_scr", [1024, 2048], BF16).ap()
    xscr_d = nc.dram_tensor("x_scr", [1024, 2048], F32).ap()
    Rxscr = P.region("x_scr", dma=True)
    Rxload = env["Rxload"]
    Rh2rows = P.region("h2rows", dma=True)
    Rmoe = P.region("moe_acc", dma=True)
    Rmoe0 = P.region("moe_zero", dma=True)
    Rh2Td = P.region("h2T_scr", dma=True)

    with ExitStack() as s2:
        def T(name, shape, dt, dma=False, stack=None):
            return P.tile(name, shape, dt, dma=dma, stack=stack or s2)

        posm = T("posm", [128, 16, 32], F32)
        Rm = T("Rm", [128, 16, 32, 3], F32)
        b2sb = T("b2sb", [32, 1024], F32, dma=True)
        flags = T("flags", [128, NROUND, 32], I32)
        lsb = T("lsb", [128, 128], BF16, dma=True)
        P.dma("sp", k_dma(b2sb.t[:], b2_d), b2sb)
        P.dma("pool", k_dma(lsb.t[:], cst_d[:, CS_LS:CS_LS + 128]), lsb)

        with ExitStack() as s3:
            h2T = T("h2T", [128, 8, 2048], BF16, stack=s3)
            RH2 = [P.region(f"h2_{g}") for g in range(4)]
            rw = T("rw", [128, 8, 32], F32, dma=True, stack=s3)
            rb = T("rb", [32, 1], F32, dma=True, stack=s3)
            sq = T("m_sq", [128, 8, 512], BF16, stack=s3)
            rs = T("m_rs", [128, 512], F32, stack=s3)
            h2f = T("h2f", [128, 8, 512], F32, stack=s3)
            lgT = T("lgT", [32, 2048], F32, stack=s3)
            cwTf = T("cwTf", [32, 2048], F32, stack=s3)
            lt = T("lt", [128, 32], F32, stack=s3)
            m8 = T("m8", [128, 8], F32, stack=s3)
            negm = T("negm", [128, 1], F32, stack=s3)
            ex = T("ex", [128, 32], F32, stack=s3)
            den = T("den", [128, 1], F32, stack=s3)
            cwa = T("cwa", [128, 16, 32], F32, stack=s3)
            mska = T("mska", [128, 16, 32], BF16, stack=s3)
            hrow = T("hrow", [128, 1024], BF16, stack=s3)
            zrow = T("zrow", [128, 1024], F32, stack=s3)
            P.dma("sp", k_dma(rw.t[:], rw_d.rearrange("(k p) e -> p k e", p=128)), rw)
            P.dma("sp", k_dma(rb.t[:], rb_d), rb)
            P.op("pool", k_memset(zrow.t[:], 0.0), writes=[zrow])
            for i in range(NROWS // 128):
                P.dma("sp", k_dma(moe_d[i * 128:(i + 1) * 128, :], zrow.t[:]), Rmoe0, reads=[zrow])
            P.op("pool", k_memset(hrow.t[:], 0.0), writes=[hrow])
            P.dma("sp", k_dma(h2rows_d[2048:2176, :], hrow.t[:]), Rh2rows, reads=[hrow])
            for g in range(4):
                gs_ = slice(g * 512, (g + 1) * 512)
                P.op("act", k_act(sq.t[:], xT.t[:, :, gs_], AF.Square), reads=RXG(g), writes=[sq])
                b = bank()
                P.op("pe", k_mm([(b.t[:, 0:512], onesb.t[:], sq.t[:, k, :], k == 0, k == 7) for k in range(8)]),
                     reads=[sq, onesb], writes=[b])
                P.op("act", k_act(rs.t[:], b.t[:, 0:512], AF.Sqrt, scale=1.0 / 1024.0, bias=EPS), reads=[b], writes=[rs])
                P.op("dve", k_rcp(rs.t[:], rs.t[:]), reads=[rs], writes=[rs])
                P.op("dve", k_tt(h2f.t[:], xT.t[:, :, gs_], rs.t[:].unsqueeze(1).to_broadcast([128, 8, 512]), ALU.mult),
                     reads=RXG(g) + [rs], writes=[h2f])
                P.op("pool", k_tt(h2f.t[:], h2f.t[:], mc.t[:, 5, :].unsqueeze(2).to_broadcast([128, 8, 512]), ALU.mult),
                     reads=[h2f, mc], writes=[h2f])
                P.op("dve", k_tt(h2f.t[:], h2f.t[:], mc.t[:, 6, :].unsqueeze(2).to_broadcast([128, 8, 512]), ALU.add),
                     reads=[h2f, mc], writes=[h2f])
                P.op("act", k_acp(h2T.t[:, :, gs_], h2f.t[:]), reads=[h2f], writes=[RH2[g]])
                b = bank()
                P.op("pe", k_mm([(b.t[0:32, 0:512], rw.t[:, k, :], h2f.t[:, k, :], k == 0, k == 7) for k in range(8)]),
                     reads=[rw, h2f], writes=[b])
                P.op("dve", k_ts(lgT.t[:, gs_], b.t[0:32, 0:512], rb.t[:, 0:1], ALU.add), reads=[b, rb], writes=[lgT])
            for k in range(8):
                P.dma("sp", k_dma(h2T_d[k * 128:(k + 1) * 128, :], h2T.t[:, k, :]), Rh2Td, reads=RH2)
            for tix in range(16):
                ts_ = slice(tix * 128, (tix + 1) * 128)
                b = bank()
                P.op("pe", k_tr([(b.t[:, 0:32], lgT.t[:, ts_], cst.t[0:32, CS_ID:CS_ID + 32])]), reads=[lgT, cst], writes=[b])
                P.op("dve", k_cp(lt.t[:], b.t[:, 0:32]), reads=[b], writes=[lt])
                P.op("dve", lambda e: e.max(out=m8.t[:], in_=lt.t[:]), reads=[lt], writes=[m8])
                P.op("dve", k_ts(negm.t[:], m8.t[:, 0:1], -1.0, ALU.mult), reads=[m8], writes=[negm])
                P.op("dve", k_ts(mska.t[:, tix, :], lt.t[:], m8.t[:, 3:4], ALU.is_ge), reads=[lt, m8], writes=[mska])
                P.op("act", k_act(ex.t[:], lt.t[:], AF.Exp, bias=negm.t[:, 0:1], scale=1.0), reads=[lt, negm], writes=[ex])
                P.op("dve", k_tt(ex.t[:], ex.t[:], mska.t[:, tix, :], ALU.mult), reads=[ex, mska], writes=[ex])
                P.op("dve", k_red(den.t[:], ex.t[:]), reads=[ex], writes=[den])
                P.op("dve", k_rcp(den.t[:], den.t[:]), reads=[den], writes=[den])
                P.op("dve", k_ts(cwa.t[:, tix, :], ex.t[:], den.t[:, 0:1], ALU.mult), reads=[ex, den], writes=[cwa])
                b = bank()
                P.op("pe", k_tr([(b.t[0:32, 0:128], cwa.t[:, tix, :], ident)]), reads=[cwa, cst], writes=[b])
                P.op("act", k_acp(cwTf.t[:, ts_], b.t[0:32, 0:128]), reads=[b], writes=[cwTf])
                b = bank()
                bv = b.t[:].bitcast(BF16)
                P.op("pe", k_tr([(bv[:, k * 128:(k + 1) * 128], h2T.t[:, k, ts_], identb.t[:]) for k in range(8)]),
                     reads=RH2 + [identb], writes=[b])
                P.op("act", k_acp(hrow.t[:], bv[:, 0:1024]), reads=[b], writes=[hrow])
                P.dma("sp", k_dma(h2rows_d[ts_, :], hrow.t[:]), Rh2rows, reads=[hrow])
            bp = bank()
            bpv = bp.t[:, 0:512].rearrange("p (i e) -> p i e", e=32)
            lst = []
            for i in range(16):
                for i2 in range(i + 1):
                    lst.append((bpv[:, i, :], (lsb.t[:] if i2 == i else onesb.t[:]), mska.t[:, i2, :], i2 == 0, i2 == i))
            P.op("pe", k_mm(lst), reads=[mska, lsb, onesb], writes=[bp])
            bc = bank()
            P.op("pe", k_mm([(bc.t[:, 0:32], onesb.t[:], mska.t[:, i, :], i == 0, i == 15) for i in range(16)]),
                 reads=[mska, onesb], writes=[bc])
            P.op("dve", k_stt(posm.t[:], bpv, 1.0, mska.t[:], ALU.add, ALU.mult), reads=[bp, mska], writes=[posm])
            P.op("dve", k_ts(posm.t[:], posm.t[:], -1.0, ALU.add), reads=[posm], writes=[posm])
            for rd in range(NROUND):
                P.op("dve", k_ts(flags.t[:, rd, :], bc.t[:, 0:32], float(rd * CAP) + 0.5, ALU.is_gt), reads=[bc], writes=[flags])
            P.op("pool", k_memset(Rm.t[:], 1.0), writes=[Rm])
            P.op("dve", k_cp(Rm.t[:, :, :, 2], cwa.t[:]), reads=[cwa, Rm], writes=[Rm])
            P.op("dve", k_cp(Rm.t[:, :, :, 0], cst.t[:, CS_TOK:CS_TOK + 16].unsqueeze(2).to_broadcast([128, 16, 32])),
                 reads=[cst, Rm], writes=[Rm])
            for j in range(8):
                for g in range(4):
                    gs_ = slice(g * 512, (g + 1) * 512)
                    b = bank()
                    P.op("pe", k_mm([(b.t[:, 0:512], b2sb.t[:, j * 128:(j + 1) * 128], cwTf.t[:, gs_], True, True)]),
                         reads=[b2sb, cwTf], writes=[b])
                    P.op("dve", k_stt(xT.t[:, j, gs_], b.t[:, 0:512], mc.t[:, 7, j:j + 1], xT.t[:, j, gs_], ALU.mult, ALU.add),
                         reads=[b, mc] + RXG(g), writes=RXG(g))
            P.barrier(dummy)

        s4 = ExitStack()
        TE = lambda name, shape, dt, dma=False: P.tile(name, shape, dt, dma=dma, stack=s4)
        b1u1 = TE("b1u1", [128, 32, 8], F32)
        iota_t = TE("iota", [128, 2048], F32, dma=True)
        P.dma("sp", k_dma(iota_t.t[:], cst_d[:, CS_IOTA:CS_IOTA + 2048]), iota_t)
        stg = [TE(f"stg{i}", [128, 2048], F32, dma=True) for i in range(5)]
        for k in range(8):
            P.dma("sp", k_dma(xscr_d[k * 128:(k + 1) * 128, :], xT.t[:, k, :]), Rxscr, reads=RX)
        P.barrier(dummy)
        xflat = xT.t[:].rearrange("p k t -> p (k t)")
        w1e = [xflat[:, b_ * 8192:(b_ + 1) * 8192].bitcast(BF16).rearrange("p (k c) -> p k c", k=8) for b_ in range(2)]
        RW1 = [[P.region(f"w1e{b_}_{s_}") for s_ in range(8)] for b_ in range(2)]
        w2e = [TE(f"w2e{b_}", [128, 8, 1024], BF16) for b_ in range(2)]
        RW2 = [[P.region(f"w2e{b_}_{h}") for h in range(4)] for b_ in range(2)]
        Pm = [TE(f"Pm{i}", [128, CAP], F32) for i in range(4)]
        idxr = TE("idxr", [3, CAP], F32)
        idxf = TE("idxf", [128, NBLK, 3], F32)
        tmpi = TE("tmpi", [128, NBLK], F32)
        idxu = [TE(f"idxu{i}", [128, NBLK], U32) for i in range(2)]
        cws = [TE(f"cws{i}", [128, NBLK], F32) for i in range(2)]
        Xg = [TE(f"Xg{i}", [128, 1024], BF16, dma=True) for i in range(3)]
        XT = [TE(f"XT{i}", [128, 8, CAP], BF16) for i in range(2)]
        actT = TE("actT", [128, 8, CAP], BF16)
        RACT = [P.region(f"act{j}") for j in range(8)]
        ga = [TE(f"ga{i}", [128, CAP], F32) for i in range(4)]
        uu = [TE(f"uu{i}", [128, CAP], F32) for i in range(4)]
        Ysb = [TE(f"Ysb{i}", [128, 1024], F32) for i in range(NBLK)]
        w1_v = w1_d.rearrange("e (k p) c -> e p k c", p=128)
        w2_v = w2_d.rearrange("e (k p) c -> e p k c", p=128)
        b1v = pc.t[:, PC_B1:PC_B1 + 512].rearrange("p (e j) -> p e j", j=16)
        P.op("dve", k_ts(b1u1.t[:], b1v[:, :, 8:16], 1.0, ALU.add), reads=[pc], writes=[b1u1])
        padrow = cst.t[:, CS_PAD:CS_PAD + 1]
        cnt = {"stg": 0, "pm": 0, "x": 0, "wk": 0, "rd": 0, "cast": 0}
        cast_engs = ("act", "dve")

        def load_cast(src_ap, stg_view, dst_reg, dst_ap):
            sg_ = stg[cnt["stg"] % 5]
            cnt["stg"] += 1
            eng = cast_engs[cnt["cast"] % 2]
            cnt["cast"] += 1
            sv = stg_view(sg_.t)
            P.dma("sp", k_dma(sv, src_ap), sg_)
            if eng == "act":
                P.op("act", k_act(dst_ap, sv, AF.Copy), reads=[sg_], writes=[dst_reg])
            else:
                P.op(eng, k_cp(dst_ap, sv), reads=[sg_], writes=[dst_reg])

        def weight_pieces(e):
            wb = e % 2
            lst = []
            for k in range(8):
                lst.append(lambda k=k: load_cast(w1_d[e, k * 128:(k + 1) * 128, :], (lambda t: t[:]), RW1[wb][k], w1e[wb][:, k, :]))
            for q_ in range(4):
                lst.append(lambda q_=q_: load_cast(w2_v[e, :, 2 * q_:2 * q_ + 2, :], (lambda t: t[:].rearrange("p (k c) -> p k c", k=2)),
                                                   RW2[wb][q_], w2e[wb].t[:, 2 * q_:2 * q_ + 2, :]))
            return lst

        for f_ in weight_pieces(0):
            f_()
        for e in range(32):
            wb = e % 2
            pending = weight_pieces(e + 1) if e + 1 < 32 else []
            for rd in range(NROUND):
                fcol = rd * 32 + e
                if 0 < rd <= NSINGLE:
                    P.cond_begin(flags, flags.t[:].rearrange("p r e -> p (r e)")[0:1, fcol:fcol + 1])
                iu = idxu[cnt["rd"] % 2]
                cw_ = cws[cnt["rd"] % 2]
                xt = XT[cnt["rd"] % 2]
                cnt["rd"] += 1
                iota = iota_t.t[:, rd * CAP:(rd + 1) * CAP]
                bi = bank()
                for i in range(16):
                    pm = Pm[cnt["pm"] % 4]
                    cnt["pm"] += 1
                    P.op("dve", k_ts(pm.t[:], iota, posm.t[:, i, e:e + 1], ALU.is_equal), reads=[iota_t, posm], writes=[pm])
                    P.op("pe", k_mm([(bi.t[0:3, 0:CAP], Rm.t[:, i, e, :], pm.t[:], i == 0, i == 15)]), reads=[pm, Rm], writes=[bi])
                P.op("act", k_acp(idxr.t[:], bi.t[0:3, 0:CAP]), reads=[bi], writes=[idxr])
                bi2 = bank()
                biv = bi2.t[:, 0:NBLK * 3].rearrange("p (j c) -> p j c", c=3)
                P.op("pe", k_tr([(biv[:, j, :], idxr.t[0:3, j * 128:(j + 1) * 128], cst.t[0:3, CS_ID:CS_ID + 3]) for j in range(NBLK)]),
                     reads=[idxr, cst], writes=[bi2])
                P.op("dve", k_cp(idxf.t[:], biv), reads=[bi2], writes=[idxf])
                P.op("dve", k_ts(tmpi.t[:], idxf.t[:, :, 1], -1.0, ALU.mult, 1.0, ALU.add), reads=[idxf], writes=[tmpi])
                P.op("dve", k_stt(iu.t[:], tmpi.t[:], padrow, idxf.t[:, :, 0], ALU.mult, ALU.add), reads=[tmpi, idxf, cst], writes=[iu])
                P.op("dve", k_ts(cw_.t[:], idxf.t[:, :, 2], 1.0 / 1.702, ALU.mult), reads=[idxf], writes=[cw_])
                for j in range(NBLK):
                    xg = Xg[cnt["x"] % 3]
                    cnt["x"] += 1
                    P.dma("pool", (lambda en, xg=xg, iu=iu, j=j: en.indirect_dma_start(
                        out=xg.t[:], out_offset=None, in_=h2rows_d,
                        in_offset=bass.IndirectOffsetOnAxis(ap=iu.t[:, j:j + 1], axis=0))), xg, reads=[iu, Rh2rows])
                    b = bank()
                    bv = b.t[:].bitcast(BF16)
                    P.op("pe", k_tr([(bv[:, k * 128:(k + 1) * 128], xg.t[:, k * 128:(k + 1) * 128], identb.t[:]) for k in range(8)]),
                         reads=[xg, identb], writes=[b])
                    P.op("act", k_acp(xt.t[:, :, j * 128:(j + 1) * 128], bv[:, 0:1024].rearrange("p (k r) -> p k r", k=8)),
                         reads=[b], writes=[xt])
                for j in range(8):
                    bg = bank()
                    bu = bank()
                    P.op("pe", k_mm([(bg.t[:, 0:CAP], w1e[wb][:, k, j * 128:(j + 1) * 128], xt.t[:, k, :], k == 0, k == 7)
                                     for k in range(8)]), reads=RW1[wb] + [xt], writes=[bg])
                    P.op("pe", k_mm([(bu.t[:, 0:CAP], w1e[wb][:, k, 1024 + j * 128:1024 + (j + 1) * 128], xt.t[:, k, :], k == 0, k == 7)
                                     for k in range(8)]), reads=RW1[wb] + [xt], writes=[bu])
                    g_, u_ = ga[cnt["wk"] % 4], uu[cnt["wk"] % 4]
                    cnt["wk"] += 1
                    P.op("dve", k_ts(g_.t[:], bg.t[:, 0:CAP], b1v[:, e, j:j + 1], ALU.add, 7.0, ALU.min), reads=[bg, pc], writes=[g_])
                    P.op("act", k_act(g_.t[:], g_.t[:], AF.Silu, scale=1.702), reads=[g_], writes=[g_])
                    P.op("dve", k_ts(u_.t[:], bu.t[:, 0:CAP], b1u1.t[:, e, j:j + 1], ALU.add, 8.0, ALU.min), reads=[bu, b1u1], writes=[u_])
                    P.op("dve", k_stt(actT.t[:, j, :], u_.t[:], -6.0, g_.t[:], ALU.max, ALU.mult), reads=[u_, g_], writes=[RACT[j]])
                    if rd == 0 and pending:
                        pending.pop(0)()
                for half in range(2):
                    for j in range(NBLK):
                        b = bank()
                        P.op("pe", k_mm([(b.t[:, 0:512], actT.t[:, k, j * 128:(j + 1) * 128], w2e[wb].t[:, k, half * 512:(half + 1) * 512], k == 0, k == 7)
                                         for k in range(8)]), reads=RW2[wb] + RACT, writes=[b])
                        P.op("act", k_act(Ysb[j].t[:, half * 512:(half + 1) * 512], b.t[:, 0:512], AF.Copy, scale=cw_.t[:, j:j + 1]),
                             reads=[b, cw_], writes=[Ysb[j]])
                        if rd == 0 and pending:
                            pending.pop(0)()
                for j in range(NBLK):
                    P.dma("pool", (lambda en, j=j, iu=iu: en.indirect_dma_start(
                        out=moe_d, out_offset=bass.IndirectOffsetOnAxis(ap=iu.t[:, j:j + 1], axis=0),
                        in_=Ysb[j].t[:], in_offset=None, compute_op=ALU.add)), Rmoe, reads=[Ysb[j], iu, Rmoe0], serialize=True)
                if rd == NROUND - 1:
                    for _ in range(NSINGLE):
                        P.cond_end()
                elif rd == 0:
                    while pending:
                        pending.pop(0)()
            while pending:
                pending.pop(0)()
        P.barrier(dummy)
        s4.close()
        for k in range(8):
            P.dma("sp", k_dma(xT.t[:, k, :], xscr_d[k * 128:(k + 1) * 128, :]), Rxload, reads=[Rxscr], writes=[Rxload] + RX)

        s5 = ExitStack()
        Mt = [P.tile(f"Mt{i}", [128, 1024], F32, dma=True, stack=s5) for i in range(2)]
        xtmp = P.tile("c_xtmp", [128, 8, 128], F32, stack=s5)
        for tix in range(16):
            ts_ = slice(tix * 128, (tix + 1) * 128)
            mt = Mt[tix % 2]
            P.dma("sp", k_dma(mt.t[:], moe_d[ts_, :]), mt, reads=[Rmoe])
            if env["dbg_cols"]:
                dump(mt, mt.t[:], 1024)
            for half in range(2):
                b = bank()
                P.op("pe", k_tr([(b.t[:, kk_ * 128:(kk_ + 1) * 128], mt.t[:, (half * 4 + kk_) * 128:(half * 4 + kk_ + 1) * 128], ident)
                                 for kk_ in range(4)]), reads=[mt, cst], writes=[b])
                P.op("dve", k_tt(xtmp.t[:, half * 4:(half + 1) * 4, :], b.t[:, 0:512].rearrange("p (j t) -> p j t", j=4),
                                 mc.t[:, 7, half * 4:(half + 1) * 4].unsqueeze(2).to_broadcast([128, 4, 128]), ALU.mult),
                     reads=[b, mc], writes=[xtmp])
            P.op("pool", k_tt(xT.t[:, :, ts_], xT.t[:, :, ts_], xtmp.t[:], ALU.add), reads=[xtmp, RX[tix]], writes=[RX[tix]])
        P.barrier(dummy)
        s5.close()

        sqf = T("f_sq", [128, 8, 512], BF16)
        rsf = T("f_rs", [128, 512], F32)
        of = T("of", [128, 8, 512], F32)
        Rout = P.region("out", dma=True)
        fn = pc.t[:, PC_FN:PC_FN + 8]
        out_v = out_d.rearrange("(k p) t -> p k t", p=128)
        for g in range(4):
            gs_ = slice(g * 512, (g + 1) * 512)
            P.op("act", k_act(sqf.t[:], xT.t[:, :, gs_], AF.Square), reads=RXG(g), writes=[sqf])
            b = bank()
            P.op("pe", k_mm([(b.t[:, 0:512], onesb.t[:], sqf.t[:, k, :], k == 0, k == 7) for k in range(8)]),
                 reads=[sqf, onesb], writes=[b])
            P.op("act", k_act(rsf.t[:], b.t[:, 0:512], AF.Sqrt, scale=1.0 / 1024.0, bias=EPS), reads=[b], writes=[rsf])
            P.op("dve", k_rcp(rsf.t[:], rsf.t[:]), reads=[rsf], writes=[rsf])
            P.op("dve", k_tt(of.t[:], xT.t[:, :, gs_], rsf.t[:].unsqueeze(1).to_broadcast([128, 8, 512]), ALU.mult),
                 reads=RXG(g) + [rsf], writes=[of])
            P.op("pool", k_tt(of.t[:], of.t[:], fn.unsqueeze(2).to_broadcast([128, 8, 512]), ALU.mult), reads=[of, pc], writes=[of])
            P.dma("sp", k_dma(out_v[:, :, gs_], of.t[:]), Rout, reads=[of], writes=[Rout])


_CACHE = {}


def make_in_maps(x, c, ctx, c_ctx, w_mod, b_mod, norm1, w_in, sgu_ln, sgu_w, sgu_b, lb_fwd, lb_bwd,
                 hgrn_norm, w_out, norm2, router_w, router_b, w1, b1, w2, b2, final_norm):
    f = lambda a: np.ascontiguousarray(np.asarray(a, dtype=np.float32))
    cst, masks, sel = _consts()
    pcol = np.zeros((NCORES, 128, NPC), np.float32)
    cc = col_layout(f(c_ctx))
    for b in range(NCORES):
        cb = col_layout(f(c[b]))
        pcol[b, :, PC_C:PC_C + 16:2] = cb
        pcol[b, :, PC_C + 1:PC_C + 16:2] = cc
    pcol[:, :, PC_BMOD:PC_BMOD + 48] = col_layout(f(b_mod[0]))
    pcol[:, :, PC_N1:PC_N1 + 8] = col_layout(f(norm1[0]))
    pcol[:, :, PC_N2:PC_N2 + 8] = col_layout(f(norm2[0]))
    pcol[:, :, PC_FN:PC_FN + 8] = col_layout(f(final_norm))
    pcol[:, :, PC_LNG:PC_LNG + 4] = f(sgu_ln[0]).T
    b1c = f(b1[0]).reshape(32, 16, 128).transpose(2, 0, 1).reshape(128, 512)
    pcol[:, :, PC_B1:PC_B1 + 512] = b1c
    prow = np.concatenate([f(lb_fwd[0]), f(lb_fwd[1]), f(lb_bwd[0]), f(lb_bwd[1]), f(hgrn_norm[0]),
                           f(sgu_b[0]).reshape(-1)])[None, :]
    wsT = np.ascontiguousarray(f(sgu_w[0]).transpose(2, 0, 1).reshape(128, 512))
    shared = {
        "prow": f(prow), "w_mod": f(w_mod[0]), "w_in": f(w_in[0]), "w_out": f(w_out[0]),
        "router_w": f(router_w[0]), "router_b": f(router_b[0]).reshape(32, 1), "w1": f(w1[0]), "w2": f(w2[0]),
        "b2": f(b2[0]), "sgu_wT": wsT, "cst": cst, "masks": masks, "sel": sel,
    }
    maps = []
    for b in range(NCORES):
        m = dict(shared)
        m["xT"] = np.ascontiguousarray(f(x[b]).T)
        m["ctxT"] = np.ascontiguousarray(f(ctx[b]).T)
        m["pcol"] = pcol[b]
        maps.append(m)
    return maps


def kernel(**inputs):
    nc = build_program()
    maps = make_in_maps(**inputs)
    res = run_bass_kernel_spmd(nc, maps, core_ids=list(range(NCORES)))
    out = np.stack([np.ascontiguousarray(res.results[b]["outT"].T) for b in range(NCORES)], axis=0)
    return out.astype(np.float32)
```

```python
import numpy as np
from contextlib import ExitStack
import concourse.bass as bass
import concourse.mybir as mybir
from concourse.bass_utils import run_bass_kernel_spmd

F32 = mybir.dt.float32
BF16 = mybir.dt.bfloat16
AF = mybir.ActivationFunctionType
ALU = mybir.AluOpType
AX = mybir.AxisListType

EPOCH = 8192
NCORES = 8
EPS = 1e-6


class Region:
    __slots__ = ("name", "writers", "readers", "chan", "dma_count")

    def __init__(self, name, chan=None):
        self.name = name
        self.writers = []
        self.readers = []
        self.chan = chan
        self.dma_count = 0


class Op:
    __slots__ = ("eng", "fn", "deps", "idx", "is_dma", "token", "name", "kind", "flag_ap", "ext", "cid", "chain")

    def __init__(self, eng, fn, name=""):
        self.eng = eng
        self.fn = fn
        self.deps = []
        self.idx = None
        self.is_dma = False
        self.token = None
        self.name = name
        self.kind = "op"
        self.flag_ap = None
        self.ext = None
        self.cid = None
        self.chain = False


class Tl:
    __slots__ = ("t", "r")

    def __init__(self, t, r):
        self.t = t
        self.r = r


class Prog:
    ENGS = ("pe", "act", "dve", "pool", "sp")

    def __init__(self, nc, stack):
        self.nc = nc
        self.stack = stack
        self.ops = {e: [] for e in self.ENGS}
        self.count = {e: 0 for e in self.ENGS}
        self.sems = {e: [] for e in self.ENGS}
        self.n_chan = 0
        self.final_tokens = {}
        self.last_dma = {}

    def sem(self, name):
        return self.stack.enter_context(self.nc.semaphore(name))

    def region(self, name, dma=False):
        ch = None
        if dma:
            self.n_chan += 1
            ch = self.sem(f"c{self.n_chan}_{name}")
        return Region(name, ch)

    def tile(self, name, shape, dtype, dma=False, stack=None):
        st = stack or self.stack
        t = st.enter_context(self.nc.sbuf_tensor("sb_" + name, list(shape), dtype))
        return Tl(t, self.region(name, dma))

    def psum(self, name, shape, dtype=F32):
        t = self.stack.enter_context(self.nc.psum_tensor(name, list(shape), dtype))
        return Tl(t, self.region(name))

    def _add(self, eng, fn, reads, writes, name, dma_region=None):
        op = Op(eng, fn, name)
        is_dma = dma_region is not None
        deps = []
        for r in reads:
            deps.extend(r.writers)
        for w in writes:
            deps.extend(w.readers)
            if is_dma and not w.readers and w.writers and all(x.is_dma for x in w.writers):
                pass
            else:
                deps.extend(w.writers)
        seen = set()
        for d in deps:
            if id(d) in seen:
                continue
            seen.add(id(d))
            if d.eng == "pe" and eng == "pe" and not d.is_dma and not is_dma:
                continue
            op.deps.append(d)
        if is_dma:
            op.is_dma = True
            dma_region.dma_count += 16
            op.token = (dma_region.chan, dma_region.dma_count)
            self.final_tokens[id(dma_region.chan)] = op.token
            self.last_dma[id(dma_region.chan)] = op
        else:
            self.count[eng] += 1
            op.idx = self.count[eng]
        for r in reads:
            r.readers.append(op)
        for w in writes:
            if is_dma and not w.readers and w.writers and all(x.is_dma for x in w.writers):
                w.writers.append(op)
            else:
                w.writers = [op]
            w.readers = []
        self.ops[eng].append(op)
        return op

    def op(self, eng, fn, reads=(), writes=(), name=""):
        return self._add(eng, fn, [x.r if isinstance(x, Tl) else x for x in reads],
                         [x.r if isinstance(x, Tl) else x for x in writes], name)

    def dma(self, eng, fn, dst, reads=(), writes=None, name="", serialize=False):
        dst_r = dst.r if isinstance(dst, Tl) else dst
        w = [dst_r] if writes is None else [x.r if isinstance(x, Tl) else x for x in writes]
        rd = [x.r if isinstance(x, Tl) else x for x in reads]
        if serialize:
            rd = rd + [dst_r]
        op = self._add(eng, fn, rd, w, name, dma_region=dst_r)
        op.chain = serialize
        return op

    def barrier(self, dummy):
        lasts = []
        for e in ("pe", "act", "dve", "pool"):
            for o in reversed(self.ops[e]):
                if not o.is_dma and o.kind == "op":
                    lasts.append(o)
                    break
        dmas = list(self.last_dma.values())
        new_ops = []
        for e in ("act", "dve", "pool"):
            if e == "act":
                fn = (lambda en: en.memzero(dummy["act"].t[:]))
            else:
                fn = (lambda en, e=e: en.memset(dummy[e].t[:], 0.0))
            new_ops.append(self.op(e, fn, writes=[dummy[e]]))
        new_ops.append(self.dma("sp", lambda en: en.dma_start(out=dummy["sp"].t[:], in_=dummy["src"]), dummy["sp"]))
        for op in new_ops:
            for d in lasts + dmas:
                if d is not op and all(d is not x for x in op.deps):
                    op.deps.append(d)

    def cond_begin(self, flag_tl, flag_ap):
        if not hasattr(self, "_cstack"):
            self._cstack = []
        self._cstack.append({e: len(self.ops[e]) for e in self.ENGS})
        for e in self.ENGS:
            m = Op(e, None, "cbegin")
            m.kind = "cbegin"
            m.flag_ap = flag_ap
            m.deps = list(flag_tl.r.writers)
            self.ops[e].append(m)

    def cond_end(self):
        cstart = self._cstack.pop()
        body = set()
        for e in self.ENGS:
            for o in self.ops[e][cstart[e] + 1:]:
                body.add(id(o))
        ext_ops = []
        for e in self.ENGS:
            for o in self.ops[e][cstart[e] + 1:]:
                for d in o.deps:
                    if id(d) not in body:
                        only = e if (o.chain and d.chain and d.is_dma and o.is_dma and d.token[0] is o.token[0]) else None
                        ext_ops.append((d, only))
        for e in self.ENGS:
            m = Op(e, None, "cend")
            m.kind = "cend"
            m.ext = ext_ops
            self.ops[e].append(m)

    def _tok(self, d):
        if d.is_dma:
            return d.token
        e = d.eng
        ep = (d.idx - 1) // EPOCH
        return (self.sems[e][ep], (d.idx - 1) % EPOCH + 1)

    def emit(self):
        nc = self.nc
        for e in self.ENGS:
            n_ep = (self.count[e] + EPOCH - 1) // EPOCH
            for i in range(n_ep):
                self.sems[e].append(self.sem(f"s_{e}{i}"))
        prog = self

        def run(e, engine):
            waited = {}

            def emit_op(op):
                need = {}
                for d in op.deps:
                    sem, val = prog._tok(d)
                    k = id(sem)
                    if need.get(k, (sem, 0))[1] < val:
                        need[k] = (sem, val)
                for k, (sem, val) in need.items():
                    if waited.get(k, 0) >= val:
                        continue
                    waited[k] = val
                    engine.wait_ge(sem, val)
                if op.kind != "op":
                    return
                ins = op.fn(engine)
                if op.is_dma:
                    ins.then_inc(op.token[0], 16)
                else:
                    ep = (op.idx - 1) // EPOCH
                    ins.then_inc(prog.sems[e][ep], 1)

            ops = prog.ops[e]

            def emit_range(lo, hi):
                i = lo
                while i < hi:
                    op = ops[i]
                    if op.kind == "cbegin":
                        depth = 1
                        j = i + 1
                        while True:
                            if ops[j].kind == "cbegin":
                                depth += 1
                            elif ops[j].kind == "cend":
                                depth -= 1
                                if depth == 0:
                                    break
                            j += 1
                        real = [b for b in ops[i + 1:j] if b.kind == "op"]
                        if real:
                            emit_op(op)
                            incs = {}
                            for b in real:
                                if b.is_dma:
                                    sem, n = b.token[0], 16
                                else:
                                    sem, n = prog.sems[e][(b.idx - 1) // EPOCH], 1
                                k = id(sem)
                                incs[k] = (sem, incs.get(k, (sem, 0))[1] + n)
                            with engine.register() as freg:
                                engine.reg_load(freg, op.flag_ap)
                                saved = dict(waited)
                                with engine.If_eq(freg, 1):
                                    emit_range(i + 1, j)
                                waited.clear()
                                waited.update(saved)
                                with engine.Else():
                                    ext = {}
                                    for d_, only_ in ops[j].ext:
                                        if only_ is not None and only_ != e:
                                            continue
                                        sem_, v_ = prog._tok(d_)
                                        if ext.get(id(sem_), (sem_, 0))[1] < v_:
                                            ext[id(sem_)] = (sem_, v_)
                                    pre = None
                                    for o in reversed(ops[:i]):
                                        if o.kind == "op" and not o.is_dma:
                                            pre = o
                                            break
                                    if pre is not None:
                                        sem, v = prog._tok(pre)
                                        if ext.get(id(sem), (sem, 0))[1] < v:
                                            ext[id(sem)] = (sem, v)
                                    for k, (sem, v) in ext.items():
                                        if waited.get(k, 0) >= v:
                                            continue
                                        engine.wait_ge(sem, v)
                                    for sem, n in incs.values():
                                        engine.sem_inc(sem, n)
                        i = j + 1
                        continue
                    if op.kind == "cend":
                        i += 1
                        continue
                    emit_op(op)
                    i += 1

            emit_range(0, len(ops))
            if e == "sp":
                for sem, val in prog.final_tokens.values():
                    engine.wait_ge(sem, val)
                for ce in ("pe", "act", "dve", "pool"):
                    c = prog.count[ce]
                    if c:
                        ep = (c - 1) // EPOCH
                        engine.wait_ge(prog.sems[ce][ep], (c - 1) % EPOCH + 1)

        with nc.Block() as block:
            @block.tensor
            def _(eng):
                run("pe", eng)

            @block.scalar
            def _(eng):
                run("act", eng)

            @block.vector
            def _(eng):
                run("dve", eng)

            @block.gpsimd
            def _(eng):
                run("pool", eng)

            @block.sync
            def _(eng):
                run("sp", eng)


def k_mm(lst):
    def f(e):
        ins = None
        for (o, l, r, s, t) in lst:
            ins = e.matmul(o, lhsT=l, rhs=r, start=s, stop=t)
        return ins
    return f


def k_tr(lst):
    def f(e):
        ins = None
        for (o, i, idn) in lst:
            ins = e.transpose(out=o, in_=i, identity=idn)
        return ins
    return f


def k_act(out, in_, func, **kw):
    return lambda e: e.activation(out=out, in_=in_, func=func, **kw)


def k_tt(out, a, b, op):
    return lambda e: e.tensor_tensor(out=out, in0=a, in1=b, op=op)


def k_ts(out, a, s1, op0, s2=None, op1=None):
    if op1 is None:
        return lambda e: e.tensor_scalar(out=out, in0=a, scalar1=s1, scalar2=None, op0=op0)
    return lambda e: e.tensor_scalar(out=out, in0=a, scalar1=s1, scalar2=s2, op0=op0, op1=op1)


def k_stt(out, a, s, b, op0, op1):
    return lambda e: e.scalar_tensor_tensor(out=out, in0=a, scalar=s, in1=b, op0=op0, op1=op1)


def k_cp(out, in_):
    return lambda e: e.tensor_copy(out=out, in_=in_)


def k_acp(out, in_):
    return lambda e: e.copy(out=out, in_=in_)


def k_red(out, in_):
    return lambda e: e.reduce_sum(out=out, in_=in_, axis=AX.X)


def k_rcp(out, in_):
    return lambda e: e.reciprocal(out=out, in_=in_)


def k_dma(out, in_):
    return lambda e: e.dma_start(out=out, in_=in_)


def k_memset(ap, v):
    return lambda e: e.memset(ap, v)


G_U, G_V, G_Q, G_ZF, G_ZB, G_I, G_G = range(7)
NPC = 16 + 48 + 24 + 4 + 512
PC_C, PC_BMOD, PC_N1, PC_N2, PC_FN, PC_LNG, PC_B1 = 0, 16, 64, 72, 80, 88, 92
NPR = 3072
CS_ID, CS_TMF, CS_TMB, CS_XF, CS_XB = 0, 128, 256, 384, 386
CS_LS, CS_PAD, CS_TOK, CS_IOTA = 388, 516, 517, 533
NCST = 533 + 2048


def _consts():
    s = np.arange(128)[:, None]
    t = np.arange(128)[None, :]
    cst = np.zeros((128, NCST), np.float32)
    cst[:, CS_ID:CS_ID + 128] = np.eye(128, dtype=np.float32)
    cst[:, CS_TMF:CS_TMF + 128] = (s <= t).astype(np.float32) - (s <= 63).astype(np.float32)
    cst[:, CS_TMB:CS_TMB + 128] = (s >= t).astype(np.float32) - (s >= 64).astype(np.float32)
    cst[:, CS_XF] = (s[:, 0] >= 64)
    cst[:, CS_XF + 1] = (s[:, 0] <= 63)
    cst[:, CS_XB] = (s[:, 0] <= 63)
    cst[:, CS_XB + 1] = (s[:, 0] >= 64)
    cst[:, CS_LS:CS_LS + 128] = (s < t).astype(np.float32)
    cst[:, CS_IOTA:CS_IOTA + 2048] = np.arange(2048, dtype=np.float32)[None, :]
    cst[:, CS_PAD] = 2048 + np.arange(128)
    cst[:, CS_TOK:CS_TOK + 16] = np.arange(128)[:, None] + 128 * np.arange(16)[None, :]
    mf = np.tile((s <= t).astype(np.float32), (1, 4))
    mb = np.tile((s >= t).astype(np.float32), (1, 4))
    masks = np.concatenate([mf, mb], axis=1)
    sel = np.zeros((32, 32, 128), np.float32)
    for e in range(32):
        sel[e, e, :] = 1.0
    return cst, masks, sel.reshape(32, 4096)


def col_layout(v):
    return np.ascontiguousarray(v.reshape(-1, 128).T)


def build_program(stage=99, dbg_cols=0):
    nc = bass.Bass("TRN2", target_bir_lowering=False)

    def D(name, shape, kind="ExternalInput"):
        return nc.dram_tensor(name, list(shape), F32, kind=kind).ap()

    xT_d = D("xT", [1024, 2048])
    ctxT_d = D("ctxT", [1024, 256])
    pcol_d = D("pcol", [128, NPC])
    prow_d = D("prow", [1, NPR])
    wmod_d = D("w_mod", [1024, 6144])
    win_d = D("w_in", [1024, 3584])
    wout_d = D("w_out", [1024, 1024])
    rw_d = D("router_w", [1024, 32])
    rb_d = D("router_b", [32, 1])
    w1_d = D("w1", [32, 1024, 2048]) if stage >= 2 else None
    w2_d = D("w2", [32, 1024, 1024]) if stage >= 2 else None
    b2_d = D("b2", [32, 1024])
    wsT_d = D("sgu_wT", [128, 512])
    cst_d = D("cst", [128, NCST])
    mask_d = D("masks", [128, 1024])
    sel_d = D("sel", [32, 4096])
    out_d = D("outT", [1024, 2048], kind="ExternalOutput")
    dbg_d = D("dbg", [128, dbg_cols], kind="ExternalOutput") if dbg_cols else None

    with ExitStack() as st:
        P = Prog(nc, st)
        dbg_state = {"off": 0}
        Rdbg = P.region("dbgout", dma=True) if dbg_cols else None
        if dbg_cols:
            dbg_state["dtmp"] = P.tile("dbg_bounce", [128, 512], F32)

        def dump(tl, ap, ncols):
            o = dbg_state["off"]
            P.dma("sp", k_dma(dbg_d[:, o:o + ncols], ap), Rdbg, reads=[tl], writes=[Rdbg])
            dbg_state["off"] = o + ncols

        xT = P.tile("xT", [128, 8, 2048], F32)
        RX = [P.region(f"x{c}") for c in range(16)]
        Rxload = P.region("xload", dma=True)
        pc = P.tile("pc", [128, NPC], F32, dma=True)
        cst = P.tile("cst", [128, CS_IOTA], F32, dma=True)
        identb = P.tile("identb", [128, 128], BF16, dma=True)
        onesb = P.tile("onesb", [128, 128], BF16)
        modc = P.tile("modc", [128, 48, 2], F32)
        mc = P.tile("mc", [128, 8, 8], F32)
        dummy = {e: P.tile(f"dummy_{e}", [128, 8], F32) for e in ("act", "dve", "pool")}
        dummy["sp"] = P.tile("dummy_sp", [128, 8], F32, dma=True)
        dummy["src"] = cst_d[:, 0:8]
        PSB = [P.psum(f"psb{i}", [128, 512], F32) for i in range(8)]
        ps_state = {"i": 0}

        def bank():
            b = PSB[ps_state["i"] % 8]
            ps_state["i"] += 1
            return b

        ident = cst.t[:, CS_ID:CS_ID + 128]

        P.dma("sp", k_dma(pc.t[:], pcol_d), pc)
        P.dma("sp", k_dma(cst.t[:], cst_d[:, 0:CS_IOTA]), cst)
        P.dma("pool", k_dma(identb.t[:], cst_d[:, CS_ID:CS_ID + 128]), identb)
        P.op("pool", k_memset(onesb.t[:], 1.0), writes=[onesb])
        for k in range(8):
            P.dma("sp", k_dma(xT.t[:, k, :], xT_d[k * 128:(k + 1) * 128, :]), Rxload,
                  writes=[Rxload] + RX)

        with ExitStack() as sa:
            cS = P.tile("cS", [128, 16], F32, stack=sa)
            wm = [P.tile(f"wm{i}", [128, 8, 768], F32, dma=True, stack=sa) for i in range(2)]
            P.op("act", k_act(cS.t[:], pc.t[:, PC_C:PC_C + 16], AF.Silu), reads=[pc], writes=[cS])
            psA = bank()
            psA_v = psA.t[:, 0:96].rearrange("p (j c) -> p j c", c=2)
            wmod_v = wmod_d.rearrange("(k p) c -> p k c", p=128)
            for blk in range(8):
                w = wm[blk % 2]
                P.dma("sp", k_dma(w.t[:], wmod_v[:, :, blk * 768:(blk + 1) * 768]), w)
                for jj in range(6):
                    j = blk * 6 + jj
                    P.op("pe", k_mm([(psA_v[:, j, :], w.t[:, k, jj * 128:(jj + 1) * 128],
                                      cS.t[:, 2 * k:2 * k + 2], k == 0, k == 7) for k in range(8)]),
                         reads=[w, cS], writes=[psA])
            P.op("dve", k_tt(modc.t[:], psA_v,
                             pc.t[:, PC_BMOD:PC_BMOD + 48].unsqueeze(2).to_broadcast([128, 48, 2]), ALU.add),
                 reads=[psA, pc], writes=[modc])
            n1 = pc.t[:, PC_N1:PC_N1 + 8]
            n2 = pc.t[:, PC_N2:PC_N2 + 8]
            P.op("dve", k_stt(mc.t[:, 0, :], modc.t[:, 8:16, 0], 1.0, n1, ALU.add, ALU.mult), reads=[modc, pc], writes=[mc])
            P.op("dve", k_cp(mc.t[:, 1, :], modc.t[:, 0:8, 0]), reads=[modc], writes=[mc])
            P.op("dve", k_stt(mc.t[:, 2, :], modc.t[:, 8:16, 1], 1.0, n1, ALU.add, ALU.mult), reads=[modc, pc], writes=[mc])
            P.op("dve", k_cp(mc.t[:, 3, :], modc.t[:, 0:8, 1]), reads=[modc], writes=[mc])
            P.op("dve", k_cp(mc.t[:, 4, :], modc.t[:, 16:24, 0]), reads=[modc], writes=[mc])
            P.op("dve", k_stt(mc.t[:, 5, :], modc.t[:, 32:40, 0], 1.0, n2, ALU.add, ALU.mult), reads=[modc, pc], writes=[mc])
            P.op("dve", k_cp(mc.t[:, 6, :], modc.t[:, 24:32, 0]), reads=[modc], writes=[mc])
            P.op("dve", k_cp(mc.t[:, 7, :], modc.t[:, 40:48, 0]), reads=[modc], writes=[mc])
            P.barrier(dummy)
        if stage == 0:
            dump(mc, mc.t[:].rearrange("p a b -> p (a b)"), 64)

        def dump_any(regs, ap2d, ncols, stack):
            if not dbg_cols:
                return
            if "dtmp" not in dbg_state:
                dbg_state["dtmp"] = P.tile("dbg_bounce", [128, 512], F32, stack=stack)
            tmp = dbg_state["dtmp"]
            for o in range(0, ncols, 512):
                n = min(512, ncols - o)
                P.op("dve", k_cp(tmp.t[:, 0:n], ap2d[:, o:o + n]), reads=regs, writes=[tmp])
                dump(tmp, tmp.t[:, 0:n], n)

        def bc8(col_ap, n):
            return col_ap.unsqueeze(2).to_broadcast([128, 8, n])

        def make_h(W, src_ap, src_regs, A_ap, sh_ap, hT_tl, hT_ap, n=128):
            P.op("act", k_act(W["sq"].t[:, :, 0:n], src_ap, AF.Square), reads=src_regs, writes=[W["sq"]])
            b = bank()
            P.op("pe", k_mm([(b.t[:, 0:n], onesb.t[:], W["sq"].t[:, k, 0:n], k == 0, k == 7) for k in range(8)]),
                 reads=[W["sq"], onesb], writes=[b])
            P.op("act", k_act(W["rs"].t[:, 0:n], b.t[:, 0:n], AF.Sqrt, scale=1.0 / 1024.0, bias=EPS),
                 reads=[b], writes=[W["rs"]])
            P.op("dve", k_rcp(W["rs"].t[:, 0:n], W["rs"].t[:, 0:n]), reads=[W["rs"]], writes=[W["rs"]])
            P.op("dve", k_tt(W["t1"].t[:, :, 0:n], src_ap, W["rs"].t[:, 0:n].unsqueeze(1).to_broadcast([128, 8, n]), ALU.mult),
                 reads=src_regs + [W["rs"]], writes=[W["t1"]])
            P.op("pool", k_tt(W["t1"].t[:, :, 0:n], W["t1"].t[:, :, 0:n], bc8(A_ap, n), ALU.mult),
                 reads=[W["t1"], mc], writes=[W["t1"]])
            P.op("dve", k_tt(hT_ap, W["t1"].t[:, :, 0:n], bc8(sh_ap, n), ALU.add),
                 reads=[W["t1"], mc], writes=[hT_tl])

        if stage >= 1:
            build_sublayer1(nc, P, st, locals())
        if stage >= 2:
            build_moe(nc, P, st, locals())
        else:
            for k in range(8):
                P.dma("sp", k_dma(out_d[k * 128:(k + 1) * 128, :], xT.t[:, k, :]), P.region(f"o{k}", dma=True), reads=RX)
        P.emit()
    return nc


def build_sublayer1(nc, P, st, env):
    xT, RX, pc, cst, identb, onesb, mc, dummy = (env[k] for k in ("xT", "RX", "pc", "cst", "identb", "onesb", "mc", "dummy"))
    bank, make_h, dump, stage = env["bank"], env["make_h"], env["dump"], env["stage"]
    ctxT_d, prow_d, win_d, wout_d, wsT_d, mask_d = (env[k] for k in ("ctxT_d", "prow_d", "win_d", "wout_d", "wsT_d", "mask_d"))
    ident = cst.t[:, CS_ID:CS_ID + 128]
    win_v = win_d.rearrange("(k p) c -> p k c", p=128)
    wout_v = wout_d.rearrange("(k p) c -> p k c", p=128)

    with ExitStack() as s1, ExitStack() as sfb:
        def T(name, shape, dt, dma=False, stack=None):
            return P.tile(name, shape, dt, dma=dma, stack=stack or s1)

        def TF(name, shape, dt, dma=False):
            return P.tile(name, shape, dt, dma=dma, stack=sfb)

        hTctx = T("hTctx", [128, 8, 256], BF16)
        win = T("win", [128, 8, 2560], BF16, dma=True)
        ybT = T("ybT", [128, 4, 2048], BF16)
        RYB = [P.region(f"yb{c}") for c in range(16)]
        W = {
            "sq": T("w_sq", [128, 8, 128], BF16),
            "rs": T("w_rs", [128, 128], F32),
            "t1": T("w_t1", [128, 8, 128], F32),
        }
        hT = T("hT", [128, 8, 128], BF16)
        Sf = TF("Sf", [128, 16, 512], BF16)
        RSf = [P.region(f"Sf{c}") for c in range(16)]
        masks = TF("masks", [128, 1024], BF16, dma=True)
        LB = {d: TF(f"LB{d}", [128, 512], F32, dma=True) for d in "fb"}
        OML = {d: TF(f"OML{d}", [128, 512], F32, dma=True) for d in "fb"}
        HN = TF("HN", [128, 512], F32, dma=True)
        sig = TF("sig", [128, 512], F32)
        ff = TF("ff", [128, 512], F32)
        logf = {d: TF(f"logf{d}", [128, 512], F32) for d in "fb"}
        kk = TF("kk", [128, 512], F32)
        einv = TF("einv", [128, 512], F32)
        Kt = {d: TF(f"Kt{d}", [128, 512], BF16) for d in "fb"}
        V = TF("V", [128, 512], BF16)
        Aprev = TF("Aprev", [128, 4], F32)
        dcol = TF("dcol", [128, 4], F32)
        Sp = TF("Sp", [128, 512], F32)
        Sbf = TF("Sbf", [128, 512], BF16)
        M = TF("M", [128, 512], F32)

        P.dma("pool", k_dma(masks.t[:], mask_d), masks)
        P.dma("sp", k_dma(HN.t[:], prow_d[:, 2048:2560].partition_broadcast(128)), HN)
        for i, d in enumerate("fb"):
            P.dma("sp", k_dma(LB[d].t[:], prow_d[:, (2 * i) * 512:(2 * i + 1) * 512].partition_broadcast(128)), LB[d])
            P.dma("sp", k_dma(OML[d].t[:], prow_d[:, (2 * i + 1) * 512:(2 * i + 2) * 512].partition_broadcast(128)), OML[d])
            P.op("dve", k_tt(OML[d].t[:], LB[d].t[:], OML[d].t[:], ALU.subtract), reads=[LB[d], OML[d]], writes=[OML[d]])
            P.op("act", k_act(LB[d].t[:], OML[d].t[:], AF.Sigmoid), reads=[OML[d]], writes=[LB[d]])
            P.op("dve", k_ts(OML[d].t[:], LB[d].t[:], -1.0, ALU.mult, 1.0, ALU.add), reads=[LB[d]], writes=[OML[d]])
        with ExitStack() as s0:
            ctxT = P.tile("ctxT", [128, 8, 256], F32, dma=True, stack=s0)
            for k in range(8):
                P.dma("sp", k_dma(ctxT.t[:, k, :], ctxT_d[k * 128:(k + 1) * 128, :]), ctxT)
            for hh in range(2):
                make_h(W, ctxT.t[:, :, hh * 128:(hh + 1) * 128], [ctxT], mc.t[:, 2, :], mc.t[:, 3, :], hTctx,
                       hTctx.t[:, :, hh * 128:(hh + 1) * 128], n=128)
            P.barrier(dummy)

        if env["dbg_cols"] and stage < 2:
            env["dump_any"]([hTctx], hTctx.t[:].rearrange("p a b -> p (a b)"), 2048, s1)
            env["dump_any"]([LB["f"]], LB["f"].t[:], 512, s1)
            env["dump_any"]([LB["b"]], LB["b"].t[:], 512, s1)

        def load_win(groups):
            for slot, g in enumerate(groups):
                P.dma("pool", k_dma(win.t[:, :, slot * 512:(slot + 1) * 512], win_v[:, :, g * 512:(g + 1) * 512]), win)

        def get_hT(kind, c):
            if kind == "ctx":
                return hTctx, hTctx.t[:, :, c * 128:(c + 1) * 128]
            make_h(W, xT.t[:, :, c * 128:(c + 1) * 128], [RX[c]], mc.t[:, 0, :], mc.t[:, 1, :], hT, hT.t[:], n=128)
            return hT, hT.t[:]

        def proj_tok(h_tl, h_ap, slot):
            b = bank()
            P.op("pe", k_mm([(b.t[:, 0:512], h_ap[:, k, :], win.t[:, k, slot * 512:(slot + 1) * 512], k == 0, k == 7)
                             for k in range(8)]), reads=[h_tl, win], writes=[b])
            return b

        def proj_ch(h_tl, h_ap, slot):
            b = bank()
            lst = []
            for h in range(4):
                for k in range(8):
                    lst.append((b.t[:, h * 128:(h + 1) * 128], win.t[:, k, slot * 512 + h * 128: slot * 512 + (h + 1) * 128],
                                h_ap[:, k, :], k == 0, k == 7))
            P.op("pe", k_mm(lst), reads=[h_tl, win], writes=[b])
            return b

        def decay_tok(zb_bank, d):
            tm = cst.t[:, (CS_TMF if d == "f" else CS_TMB):(CS_TMF if d == "f" else CS_TMB) + 128]
            P.op("act", k_act(sig.t[:], zb_bank.t[:, 0:512], AF.Sigmoid), reads=[zb_bank], writes=[sig])
            P.op("dve", k_tt(ff.t[:], sig.t[:], OML[d].t[:], ALU.mult), reads=[sig, OML[d]], writes=[ff])
            P.op("pool", k_tt(ff.t[:], ff.t[:], LB[d].t[:], ALU.add), reads=[ff, LB[d]], writes=[ff])
            P.op("act", k_act(logf[d].t[:], ff.t[:], AF.Ln), reads=[ff], writes=[logf[d]])
            P.op("pool", k_ts(kk.t[:], ff.t[:], -1.0, ALU.mult, 1.0, ALU.add), reads=[ff], writes=[kk])
            b = bank()
            P.op("pe", k_mm([(b.t[:, 0:512], tm, logf[d].t[:], True, True)]), reads=[cst, logf[d]], writes=[b])
            P.op("act", k_act(einv.t[:], b.t[:, 0:512], AF.Exp, scale=-1.0), reads=[b], writes=[einv])
            P.op("dve", k_tt(Kt[d].t[:], kk.t[:], einv.t[:], ALU.mult), reads=[kk, einv], writes=[Kt[d]])

        def state_step(d, p, store_c, need_kv):
            xo = CS_XF if d == "f" else CS_XB
            b = bank()
            cm = b.t[:, 0:8].rearrange("p (h c) -> p h c", c=2)
            P.op("pe", k_mm([(cm[:, h, :], logf[d].t[:, h * 128:(h + 1) * 128], cst.t[:, xo:xo + 2], True, True)
                             for h in range(4)]), reads=[logf[d], cst], writes=[b])
            if p > 0:
                P.op("dve", k_tt(dcol.t[:], cm[:, :, 1], Aprev.t[:], ALU.add), reads=[b, Aprev], writes=[dcol])
                P.op("act", k_act(dcol.t[:], dcol.t[:], AF.Exp), reads=[dcol], writes=[dcol])
                P.op("dve", k_tt(Sp.t[:].rearrange("p (h v) -> p h v", h=4), M.t[:].rearrange("p (h v) -> p h v", h=4),
                                 dcol.t[:].unsqueeze(2).to_broadcast([128, 4, 128]), ALU.mult),
                     reads=[M, dcol], writes=[Sp])
                if store_c is not None:
                    if d == "f":
                        P.op("act", k_acp(Sf.t[:, store_c, :], Sp.t[:]), reads=[Sp], writes=[RSf[store_c]])
                    else:
                        P.op("act", k_acp(Sbf.t[:], Sp.t[:]), reads=[Sp], writes=[Sbf])
            P.op("dve", k_cp(Aprev.t[:], cm[:, :, 0]), reads=[b], writes=[Aprev])
            if need_kv:
                b2 = bank()
                P.op("pe", k_mm([(b2.t[:, h * 128:(h + 1) * 128], Kt[d].t[:, h * 128:(h + 1) * 128],
                                  V.t[:, h * 128:(h + 1) * 128], True, True) for h in range(4)]),
                     reads=[Kt[d], V], writes=[b2])
                if p > 0:
                    P.op("dve", k_tt(M.t[:], b2.t[:, 0:512], Sp.t[:], ALU.add), reads=[b2, Sp], writes=[M])
                else:
                    P.op("dve", k_cp(M.t[:], b2.t[:, 0:512]), reads=[b2], writes=[M])

        load_win([G_ZF, G_I])
        order_f = [("ctx", 0), ("ctx", 1)] + [("lat", c) for c in range(16)]
        for p, (kind, c) in enumerate(order_f):
            h_tl, h_ap = get_hT(kind, c)
            zb_ = proj_tok(h_tl, h_ap, 0)
            decay_tok(zb_, "f")
            need_kv = p < 17
            if need_kv:
                ib_ = proj_tok(h_tl, h_ap, 1)
                P.op("act", k_acp(V.t[:], ib_.t[:, 0:512]), reads=[ib_], writes=[V])
            state_step("f", p, c if kind == "lat" else None, need_kv)

        if env["dbg_cols"] and stage < 2:
            env["dump_any"](RSf, Sf.t[:].rearrange("p a b -> p (a b)"), 8192, s1)

        if stage >= 1.2:
            sweep_b(nc, P, sfb, locals(), env)
        P.barrier(dummy)
        sfb.close()
        if stage >= 1.3:
            with ExitStack() as sc:
                sweep_c(nc, P, sc, locals(), env)
                P.barrier(dummy)


def sweep_b(nc, P, s1, L, env):
    xT, RX, pc, cst, identb, mc = (env[k] for k in ("xT", "RX", "pc", "cst", "identb", "mc"))
    bank, dump, stage = env["bank"], env["dump"], env["stage"]
    win, Sf, RSf, ybT, RYB, masks, LB, OML, HN = (L[k] for k in ("win", "Sf", "RSf", "ybT", "RYB", "masks", "LB", "OML", "HN"))
    logf, Kt, V, Sbf = (L[k] for k in ("logf", "Kt", "V", "Sbf"))

    def T(name, shape, dt, dma=False):
        return P.tile(name, shape, dt, dma=dma, stack=s1)
    load_win, get_hT, proj_tok, proj_ch, decay_tok, state_step = (L[k] for k in ("load_win", "get_hT", "proj_tok", "proj_ch", "decay_tok", "state_step"))

    ET = T("ET", [128, 512], F32)
    QtT = {d: T(f"QtT{d}", [128, 512], BF16) for d in "fb"}
    KtT = {d: T(f"KtT{d}", [128, 512], BF16) for d in "fb"}
    scT = {d: T(f"scT{d}", [128, 512], BF16) for d in "fb"}
    osq, on, sg, gs = L["sig"], L["ff"], L["einv"], L["kk"]
    ss4 = T("ss4", [128, 4], F32)
    yb = T("yb", [128, 512], BF16)

    for d in "fb":
        P.op("pool", k_memset(scT[d].t[:], 0.0), writes=[scT[d]])
    load_win([G_ZB, G_I, G_ZF, G_Q, G_G])
    order_b = [("ctx", 1), ("ctx", 0)] + [("lat", c) for c in range(15, -1, -1)]
    for p, (kind, c) in enumerate(order_b):
        h_tl, h_ap = get_hT(kind, c)
        zbb = proj_tok(h_tl, h_ap, 0)
        decay_tok(zbb, "b")
        ib_ = proj_tok(h_tl, h_ap, 1)
        P.op("act", k_acp(V.t[:], ib_.t[:, 0:512]), reads=[ib_], writes=[V])
        state_step("b", p, c if kind == "lat" else None, p < 17)
        if kind != "lat":
            continue
        zfb = proj_tok(h_tl, h_ap, 2)
        decay_tok(zfb, "f")
        qb = proj_ch(h_tl, h_ap, 3)
        for d in "fb":
            tm = cst.t[:, (CS_TMF if d == "f" else CS_TMB):(CS_TMF if d == "f" else CS_TMB) + 128]
            b = bank()
            P.op("pe", k_mm([(b.t[:, h * 128:(h + 1) * 128], logf[d].t[:, h * 128:(h + 1) * 128], tm, True, True)
                             for h in range(4)]), reads=[logf[d], cst], writes=[b])
            P.op("act", k_act(ET.t[:], b.t[:, 0:512], AF.Exp), reads=[b], writes=[ET])
            P.op("dve", k_tt(QtT[d].t[:], qb.t[:, 0:512], ET.t[:], ALU.mult), reads=[qb, ET], writes=[QtT[d]])
            bt = bank()
            btv = bt.t[:].bitcast(BF16)
            P.op("pe", k_tr([(btv[:, h * 128:(h + 1) * 128], Kt[d].t[:, h * 128:(h + 1) * 128], identb.t[:]) for h in range(4)]),
                 reads=[Kt[d], identb], writes=[bt])
            P.op("act", k_acp(KtT[d].t[:], btv[:, 0:512]), reads=[bt], writes=[KtT[d]])
            bs_ = bank()
            P.op("pe", k_mm([(bs_.t[:, h * 128:(h + 1) * 128], KtT[d].t[:, h * 128:(h + 1) * 128],
                              QtT[d].t[:, h * 128:(h + 1) * 128], True, True) for h in range(4)]),
                 reads=[KtT[d], QtT[d]], writes=[bs_])
            mo = 0 if d == "f" else 512
            v3 = lambda ap: ap.rearrange("p (h t) -> p h t", h=4)
            pa, pb = (slice(0, 64), slice(64, 128)) if d == "f" else (slice(64, 128), slice(0, 64))
            cb = slice(64, 128) if d == "f" else slice(0, 64)
            P.op("dve", k_tt(scT[d].t[pa, :], bs_.t[pa, 0:512], masks.t[pa, mo:mo + 512], ALU.mult),
                 reads=[bs_, masks], writes=[scT[d]])
            P.op("dve", k_tt(v3(scT[d].t[pb, :])[:, :, cb], v3(bs_.t[pb, 0:512])[:, :, cb],
                             v3(masks.t[pb, mo:mo + 512])[:, :, cb], ALU.mult),
                 reads=[bs_, masks, scT[d]], writes=[scT[d]])
        bo = bank()
        lst = []
        for h in range(4):
            hs = slice(h * 128, (h + 1) * 128)
            lst.append((bo.t[:, hs], scT["f"].t[:, hs], V.t[:, hs], True, False))
            lst.append((bo.t[:, hs], scT["b"].t[:, hs], V.t[:, hs], False, False))
            lst.append((bo.t[:, hs], QtT["f"].t[:, hs], Sf.t[:, c, hs], False, False))
            lst.append((bo.t[:, hs], QtT["b"].t[:, hs], Sbf.t[:, hs], False, True))
        P.op("pe", k_mm(lst), reads=[scT["f"], scT["b"], V, QtT["f"], QtT["b"], RSf[c], Sbf], writes=[bo])
        P.op("act", k_act(osq.t[:], bo.t[:, 0:512], AF.Square), reads=[bo], writes=[osq])
        P.op("dve", k_red(ss4.t[:], osq.t[:].rearrange("p (h v) -> p h v", h=4)), reads=[osq], writes=[ss4])
        P.op("act", k_act(ss4.t[:], ss4.t[:], AF.Sqrt, scale=1.0 / 128.0, bias=EPS), reads=[ss4], writes=[ss4])
        P.op("dve", k_rcp(ss4.t[:], ss4.t[:]), reads=[ss4], writes=[ss4])
        P.op("dve", k_tt(on.t[:].rearrange("p (h v) -> p h v", h=4), bo.t[:, 0:512].rearrange("p (h v) -> p h v", h=4),
                         ss4.t[:].unsqueeze(2).to_broadcast([128, 4, 128]), ALU.mult), reads=[bo, ss4], writes=[on])
        P.op("pool", k_tt(on.t[:], on.t[:], HN.t[:], ALU.mult), reads=[on, HN], writes=[on])
        gb = proj_tok(h_tl, h_ap, 4)
        P.op("act", k_act(sg.t[:], gb.t[:, 0:512], AF.Sigmoid), reads=[gb], writes=[sg])
        P.op("dve", k_tt(gs.t[:], gb.t[:, 0:512], sg.t[:], ALU.mult), reads=[gb, sg], writes=[gs])
        P.op("dve", k_tt(yb.t[:], on.t[:], gs.t[:], ALU.mult), reads=[on, gs], writes=[yb])
        by = bank()
        byv = by.t[:].bitcast(BF16)
        P.op("pe", k_tr([(byv[:, h * 128:(h + 1) * 128], yb.t[:, h * 128:(h + 1) * 128], identb.t[:]) for h in range(4)]),
             reads=[yb, identb], writes=[by])
        P.op("act", k_acp(ybT.t[:, :, c * 128:(c + 1) * 128], byv[:, 0:512].rearrange("p (h t) -> p h t", h=4)),
             reads=[by], writes=[RYB[c]])

    if env["dbg_cols"] and stage < 2:
        env["dump_any"](RYB, ybT.t[:].rearrange("p a b -> p (a b)"), 8192, s1)


def sweep_c(nc, P, s1, L, env):
    xT, RX, pc, cst, identb, mc = (env[k] for k in ("xT", "RX", "pc", "cst", "identb", "mc"))
    bank, dump, stage = env["bank"], env["dump"], env["stage"]
    wout_v = L["wout_v"]
    win, ybT, RYB = (L[k] for k in ("win", "ybT", "RYB"))
    load_win, get_hT, proj_tok, proj_ch = (L[k] for k in ("load_win", "get_hT", "proj_tok", "proj_ch"))
    prow_d, wsT_d = env["prow_d"], env["wsT_d"]

    def T(name, shape, dt, dma=False):
        return P.tile(name, shape, dt, dma=dma, stack=s1)

    BS = T("BS", [128, 512], F32, dma=True)
    WsT = T("WsT", [128, 512], BF16, dma=True)
    P.dma("sp", k_dma(BS.t[:], prow_d[:, 2560:3072].partition_broadcast(128)), BS)
    P.dma("pool", k_dma(WsT.t[:], wsT_d), WsT)

    wout = T("wout", [128, 8, 1024], BF16, dma=True)
    P.dma("pool", k_dma(wout.t[:, :, 0:512], wout_v[:, :, 0:512]), wout)
    P.dma("pool", k_dma(wout.t[:, :, 512:1024], wout_v[:, :, 512:1024]), wout)
    gu = T("gu", [128, 512], F32)
    gv = T("gv", [128, 512], F32)
    cen = T("cen", [128, 512], F32)
    sqc = T("sqc", [128, 512], F32)
    st4 = T("st4", [128, 4], F32)
    vn = T("vn", [128, 512], BF16)
    za = T("za", [128, 512], F32)
    yaT = T("yaT", [128, 4, 128], BF16)
    xtmp = T("xtmp", [128, 8, 128], F32)
    lng = pc.t[:, PC_LNG:PC_LNG + 4]

    load_win([G_U, G_V])
    for c in range(16):
        h_tl, h_ap = get_hT("lat", c)
        ub = proj_ch(h_tl, h_ap, 0)
        P.op("act", k_act(gu.t[:], ub.t[:, 0:512], AF.Gelu_apprx_tanh), reads=[ub], writes=[gu])
        vb = proj_tok(h_tl, h_ap, 1)
        P.op("act", k_act(gv.t[:], vb.t[:, 0:512], AF.Gelu_apprx_tanh), reads=[vb], writes=[gv])
        v3 = lambda t: t.t[:].rearrange("p (h d) -> p h d", h=4)
        b4 = lambda t: t.t[:].unsqueeze(2).to_broadcast([128, 4, 128])
        P.op("dve", k_red(st4.t[:], v3(gv)), reads=[gv], writes=[st4])
        P.op("dve", k_ts(st4.t[:], st4.t[:], 1.0 / 128.0, ALU.mult), reads=[st4], writes=[st4])
        P.op("dve", k_tt(v3(cen), v3(gv), b4(st4), ALU.subtract), reads=[gv, st4], writes=[cen])
        P.op("act", k_act(sqc.t[:], cen.t[:], AF.Square), reads=[cen], writes=[sqc])
        P.op("dve", k_red(st4.t[:], v3(sqc)), reads=[sqc], writes=[st4])
        P.op("act", k_act(st4.t[:], st4.t[:], AF.Sqrt, scale=1.0 / 128.0, bias=EPS), reads=[st4], writes=[st4])
        P.op("dve", k_rcp(st4.t[:], st4.t[:]), reads=[st4], writes=[st4])
        P.op("dve", k_tt(v3(vn), v3(cen), b4(st4), ALU.mult), reads=[cen, st4], writes=[vn])
        bz = bank()
        P.op("pe", k_mm([(bz.t[:, h * 128:(h + 1) * 128], vn.t[:, h * 128:(h + 1) * 128], WsT.t[:, h * 128:(h + 1) * 128], True, True)
                         for h in range(4)]), reads=[vn, WsT], writes=[bz])
        P.op("dve", k_tt(v3(za), bz.t[:, 0:512].rearrange("p (h d) -> p h d", h=4),
                         lng.unsqueeze(2).to_broadcast([128, 4, 128]), ALU.mult), reads=[bz, pc], writes=[za])
        P.op("pool", k_tt(za.t[:], za.t[:], BS.t[:], ALU.add), reads=[za, BS], writes=[za])
        P.op("dve", k_tt(yaT.t[:].rearrange("p h t -> p (h t)"), za.t[:], gu.t[:], ALU.mult), reads=[za, gu], writes=[yaT])
        bw = [bank(), bank()]
        for half in range(2):
            lst = []
            for jj in range(4):
                j = half * 4 + jj
                for k in range(8):
                    rhs = yaT.t[:, k, :] if k < 4 else ybT.t[:, k - 4, c * 128:(c + 1) * 128]
                    lst.append((bw[half].t[:, jj * 128:(jj + 1) * 128], wout.t[:, k, j * 128:(j + 1) * 128], rhs, k == 0, k == 7))
            P.op("pe", k_mm(lst), reads=[yaT, RYB[c], wout], writes=[bw[half]])
            P.op("dve", k_tt(xtmp.t[:, half * 4:(half + 1) * 4, :], bw[half].t[:, 0:512].rearrange("p (j t) -> p j t", j=4),
                             mc.t[:, 4, half * 4:(half + 1) * 4].unsqueeze(2).to_broadcast([128, 4, 128]), ALU.mult),
                 reads=[bw[half], mc], writes=[xtmp])
        P.op("pool", k_tt(xT.t[:, :, c * 128:(c + 1) * 128], xT.t[:, :, c * 128:(c + 1) * 128], xtmp.t[:], ALU.add),
             reads=[xtmp, RX[c]], writes=[RX[c]])


CAP = 256
NBLK = CAP // 128
NROUND = 2048 // CAP
NSINGLE = 3
NROWS = 2048 + 128


def build_moe(nc, P, st, env):
    xT, RX, pc, cst, identb, onesb, mc, dummy = (env[k] for k in ("xT", "RX", "pc", "cst", "identb", "onesb", "mc", "dummy"))
    bank, dump, stage = env["bank"], env["dump"], env["stage"]
    rw_d, rb_d, w1_d, w2_d, b2_d, sel_d, out_d, cst_d = (env[k] for k in ("rw_d", "rb_d", "w1_d", "w2_d", "b2_d", "sel_d", "out_d", "cst_d"))
    ident = cst.t[:, CS_ID:CS_ID + 128]
    RXG = lambda g: RX[g * 4:(g + 1) * 4]
    U32 = mybir.dt.uint32
    I32 = mybir.dt.int32

    h2rows_d = nc.dram_tensor("h2rows", [NROWS, 1024], BF16).ap()
    moe_d = nc.dram_tensor("moe_acc", [NROWS, 1024], F32).ap()
    h2T_d = nc.dram_tensor("h2T_scr", [1024, 2048], BF16).ap()
    xscr_d = nc.dram_tensor("x_scr", [1024, 2048], F32).ap()
    Rxscr = P.region("x_scr", dma=True)
    Rxload = env["Rxload"]
    Rh2rows = P.region("h2rows", dma=True)
    Rmoe = P.region("moe_acc", dma=True)
    Rmoe0 = P.region("moe_zero", dma=True)
    Rh2Td = P.region("h2T_scr", dma=True)

    with ExitStack() as s2:
        def T(name, shape, dt, dma=False, stack=None):
            return P.tile(name, shape, dt, dma=dma, stack=stack or s2)

        posm = T("posm", [128, 16, 32], F32)
        Rm = T("Rm", [128, 16, 32, 3], F32)
        flags = T("flags", [128, NROUND, 32], I32)
        lsb = T("lsb", [128, 128], BF16, dma=True)
        P.dma("pool", k_dma(lsb.t[:], cst_d[:, CS_LS:CS_LS + 128]), lsb)

        with ExitStack() as s3:
            h2T = T("h2T", [128, 8, 2048], BF16, stack=s3)
            b2sb = T("b2sb", [32, 1024], F32, dma=True, stack=s3)
            P.dma("sp", k_dma(b2sb.t[:], b2_d), b2sb)
            RH2 = [P.region(f"h2_{g}") for g in range(4)]
            rw = T("rw", [128, 8, 32], F32, dma=True, stack=s3)
            rb = T("rb", [32, 1], F32, dma=True, stack=s3)
            sq = T("m_sq", [128, 8, 512], BF16, stack=s3)
            rs = T("m_rs", [128, 512], F32, stack=s3)
            h2f = T("h2f", [128, 8, 512], F32, stack=s3)
            lgT = T("lgT", [32, 2048], F32, stack=s3)
            cwTf = T("cwTf", [32, 2048], F32, stack=s3)
            lt = T("lt", [128, 32], F32, stack=s3)
            m8 = T("m8", [128, 8], F32, stack=s3)
            negm = T("negm", [128, 1], F32, stack=s3)
            ex = T("ex", [128, 32], F32, stack=s3)
            den = T("den", [128, 1], F32, stack=s3)
            cwa = T("cwa", [128, 16, 32], F32, stack=s3)
            mska = T("mska", [128, 16, 32], BF16, stack=s3)
            hrow = T("hrow", [128, 1024], BF16, stack=s3)
            zrow = T("zrow", [128, 1024], F32, stack=s3)
            P.dma("sp", k_dma(rw.t[:], rw_d.rearrange("(k p) e -> p k e", p=128)), rw)
            P.dma("sp", k_dma(rb.t[:], rb_d), rb)
            P.op("pool", k_memset(zrow.t[:], 0.0), writes=[zrow])
            for i in range(NROWS // 128):
                P.dma("sp", k_dma(moe_d[i * 128:(i + 1) * 128, :], zrow.t[:]), Rmoe0, reads=[zrow])
            P.op("pool", k_memset(hrow.t[:], 0.0), writes=[hrow])
            P.dma("sp", k_dma(h2rows_d[2048:2176, :], hrow.t[:]), Rh2rows, reads=[hrow])
            for g in range(4):
                gs_ = slice(g * 512, (g + 1) * 512)
                P.op("act", k_act(sq.t[:], xT.t[:, :, gs_], AF.Square), reads=RXG(g), writes=[sq])
                b = bank()
                P.op("pe", k_mm([(b.t[:, 0:512], onesb.t[:], sq.t[:, k, :], k == 0, k == 7) for k in range(8)]),
                     reads=[sq, onesb], writes=[b])
                P.op("act", k_act(rs.t[:], b.t[:, 0:512], AF.Sqrt, scale=1.0 / 1024.0, bias=EPS), reads=[b], writes=[rs])
                P.op("dve", k_rcp(rs.t[:], rs.t[:]), reads=[rs], writes=[rs])
                P.op("dve", k_tt(h2f.t[:], xT.t[:, :, gs_], rs.t[:].unsqueeze(1).to_broadcast([128, 8, 512]), ALU.mult),
                     reads=RXG(g) + [rs], writes=[h2f])
                P.op("pool", k_tt(h2f.t[:], h2f.t[:], mc.t[:, 5, :].unsqueeze(2).to_broadcast([128, 8, 512]), ALU.mult),
                     reads=[h2f, mc], writes=[h2f])
                P.op("dve", k_tt(h2f.t[:], h2f.t[:], mc.t[:, 6, :].unsqueeze(2).to_broadcast([128, 8, 512]), ALU.add),
                     reads=[h2f, mc], writes=[h2f])
                P.op("act", k_acp(h2T.t[:, :, gs_], h2f.t[:]), reads=[h2f], writes=[RH2[g]])
                b = bank()
                P.op("pe", k_mm([(b.t[0:32, 0:512], rw.t[:, k, :], h2f.t[:, k, :], k == 0, k == 7) for k in range(8)]),
                     reads=[rw, h2f], writes=[b])
                P.op("dve", k_ts(lgT.t[:, gs_], b.t[0:32, 0:512], rb.t[:, 0:1], ALU.add), reads=[b, rb], writes=[lgT])
            for k in range(8):
                P.dma("sp", k_dma(h2T_d[k * 128:(k + 1) * 128, :], h2T.t[:, k, :]), Rh2Td, reads=RH2)
            for tix in range(16):
                ts_ = slice(tix * 128, (tix + 1) * 128)
                b = bank()
                P.op("pe", k_tr([(b.t[:, 0:32], lgT.t[:, ts_], cst.t[0:32, CS_ID:CS_ID + 32])]), reads=[lgT, cst], writes=[b])
                P.op("dve", k_cp(lt.t[:], b.t[:, 0:32]), reads=[b], writes=[lt])
                P.op("dve", lambda e: e.max(out=m8.t[:], in_=lt.t[:]), reads=[lt], writes=[m8])
                P.op("dve", k_ts(negm.t[:], m8.t[:, 0:1], -1.0, ALU.mult), reads=[m8], writes=[negm])
                P.op("dve", k_ts(mska.t[:, tix, :], lt.t[:], m8.t[:, 3:4], ALU.is_ge), reads=[lt, m8], writes=[mska])
                P.op("act", k_act(ex.t[:], lt.t[:], AF.Exp, bias=negm.t[:, 0:1], scale=1.0), reads=[lt, negm], writes=[ex])
                P.op("dve", k_tt(ex.t[:], ex.t[:], mska.t[:, tix, :], ALU.mult), reads=[ex, mska], writes=[ex])
                P.op("dve", k_red(den.t[:], ex.t[:]), reads=[ex], writes=[den])
                P.op("dve", k_rcp(den.t[:], den.t[:]), reads=[den], writes=[den])
                P.op("dve", k_ts(cwa.t[:, tix, :], ex.t[:], den.t[:, 0:1], ALU.mult), reads=[ex, den], writes=[cwa])
                b = bank()
                P.op("pe", k_tr([(b.t[0:32, 0:128], cwa.t[:, tix, :], ident)]), reads=[cwa, cst], writes=[b])
                P.op("act", k_acp(cwTf.t[:, ts_], b.t[0:32, 0:128]), reads=[b], writes=[cwTf])
                b = bank()
                bv = b.t[:].bitcast(BF16)
                P.op("pe", k_tr([(bv[:, k * 128:(k + 1) * 128], h2T.t[:, k, ts_], identb.t[:]) for k in range(8)]),
                     reads=RH2 + [identb], writes=[b])
                P.op("act", k_acp(hrow.t[:], bv[:, 0:1024]), reads=[b], writes=[hrow])
                P.dma("sp", k_dma(h2rows_d[ts_, :], hrow.t[:]), Rh2rows, reads=[hrow])
            bp = bank()
            bpv = bp.t[:, 0:512].rearrange("p (i e) -> p i e", e=32)
            lst = []
            for i in range(16):
                for i2 in range(i + 1):
                    lst.append((bpv[:, i, :], (lsb.t[:] if i2 == i else onesb.t[:]), mska.t[:, i2, :], i2 == 0, i2 == i))
            P.op("pe", k_mm(lst), reads=[mska, lsb, onesb], writes=[bp])
            bc = bank()
            P.op("pe", k_mm([(bc.t[:, 0:32], onesb.t[:], mska.t[:, i, :], i == 0, i == 15) for i in range(16)]),
                 reads=[mska, onesb], writes=[bc])
            P.op("dve", k_stt(posm.t[:], bpv, 1.0, mska.t[:], ALU.add, ALU.mult), reads=[bp, mska], writes=[posm])
            P.op("dve", k_ts(posm.t[:], posm.t[:], -1.0, ALU.add), reads=[posm], writes=[posm])
            for rd in range(NROUND):
                P.op("dve", k_ts(flags.t[:, rd, :], bc.t[:, 0:32], float(rd * CAP) + 0.5, ALU.is_gt), reads=[bc], writes=[flags])
            P.op("pool", k_memset(Rm.t[:], 1.0), writes=[Rm])
            P.op("dve", k_cp(Rm.t[:, :, :, 2], cwa.t[:]), reads=[cwa, Rm], writes=[Rm])
            P.op("dve", k_cp(Rm.t[:, :, :, 0], cst.t[:, CS_TOK:CS_TOK + 16].unsqueeze(2).to_broadcast([128, 16, 32])),
                 reads=[cst, Rm], writes=[Rm])
            for j in range(8):
                for g in range(4):
                    gs_ = slice(g * 512, (g + 1) * 512)
                    b = bank()
                    P.op("pe", k_mm([(b.t[:, 0:512], b2sb.t[:, j * 128:(j + 1) * 128], cwTf.t[:, gs_], True, True)]),
                         reads=[b2sb, cwTf], writes=[b])
                    P.op("dve", k_stt(xT.t[:, j, gs_], b.t[:, 0:512], mc.t[:, 7, j:j + 1], xT.t[:, j, gs_], ALU.mult, ALU.add),
                         reads=[b, mc] + RXG(g), writes=RXG(g))
            P.barrier(dummy)

        s4 = ExitStack()
        TE = lambda name, shape, dt, dma=False: P.tile(name, shape, dt, dma=dma, stack=s4)
        b1u1 = TE("b1u1", [128, 32, 8], F32)
        rcol = TE("rcol", [128, NROUND], F32)
        for rd_ in range(NROUND):
            P.op("pool", k_memset(rcol.t[:, rd_:rd_ + 1], float(rd_ * CAP)), writes=[rcol])
        iota_t = TE("iota", [128, CAP], F32, dma=True)
        P.dma("sp", k_dma(iota_t.t[:], cst_d[:, CS_IOTA:CS_IOTA + CAP]), iota_t)
        stg = [TE(f"stg{i}", [128, 2048], F32, dma=True) for i in range(6)]
        for k in range(8):
            P.dma("sp", k_dma(xscr_d[k * 128:(k + 1) * 128, :], xT.t[:, k, :]), Rxscr, reads=RX)
        P.barrier(dummy)
        xflat = xT.t[:].rearrange("p k t -> p (k t)")
        w1e = [xflat[:, b_ * 8192:(b_ + 1) * 8192].bitcast(BF16).rearrange("p (k c) -> p k c", k=8) for b_ in range(2)]
        RW1 = [[P.region(f"w1e{b_}_{s_}") for s_ in range(8)] for b_ in range(2)]
        w2e = [TE(f"w2e{b_}", [128, 8, 1024], BF16) for b_ in range(2)]
        RW2 = [[P.region(f"w2e{b_}_{h}") for h in range(4)] for b_ in range(2)]
        Pm = [TE(f"Pm{i}", [128, CAP], F32) for i in range(4)]
        idxr = TE("idxr", [3, CAP], F32)
        idxf = TE("idxf", [128, NBLK, 3], F32)
        tmpi = TE("tmpi", [128, NBLK], F32)
        idxu = [TE(f"idxu{i}", [128, NBLK], U32) for i in range(2)]
        cws = [TE(f"cws{i}", [128, NBLK], F32) for i in range(2)]
        Xg = [TE(f"Xg{i}", [128, 1024], BF16, dma=True) for i in range(3)]
        XT = [TE(f"XT{i}", [128, 8, CAP], BF16) for i in range(2)]
        actT = TE("actT", [128, 8, CAP], BF16)
        RACT = [P.region(f"act{j}") for j in range(8)]
        ga = [TE(f"ga{i}", [128, CAP], F32) for i in range(4)]
        uu = [TE(f"uu{i}", [128, CAP], F32) for i in range(4)]
        Ysb = [TE(f"Ysb{i}", [128, 1024], F32) for i in range(NBLK)]
        w1_v = w1_d.rearrange("e (k p) c -> e p k c", p=128)
        w2_v = w2_d.rearrange("e (k p) c -> e p k c", p=128)
        b1v = pc.t[:, PC_B1:PC_B1 + 512].rearrange("p (e j) -> p e j", j=16)
        P.op("dve", k_ts(b1u1.t[:], b1v[:, :, 8:16], 1.0, ALU.add), reads=[pc], writes=[b1u1])
        padrow = cst.t[:, CS_PAD:CS_PAD + 1]
        cnt = {"stg": 0, "pm": 0, "x": 0, "wk": 0, "rd": 0, "cast": 0}
        cast_engs = ("act", "dve")

        def load_cast(src_ap, stg_view, dst_reg, dst_ap):
            sg_ = stg[cnt["stg"] % 6]
            cnt["stg"] += 1
            eng = cast_engs[cnt["cast"] % 2]
            cnt["cast"] += 1
            sv = stg_view(sg_.t)
            P.dma("sp", k_dma(sv, src_ap), sg_)
            if eng == "act":
                P.op("act", k_act(dst_ap, sv, AF.Copy), reads=[sg_], writes=[dst_reg])
            else:
                P.op(eng, k_cp(dst_ap, sv), reads=[sg_], writes=[dst_reg])

        def weight_pieces(e):
            wb = e % 2
            lst = []
            for k in range(8):
                lst.append(lambda k=k: load_cast(w1_d[e, k * 128:(k + 1) * 128, :], (lambda t: t[:]), RW1[wb][k], w1e[wb][:, k, :]))
            for q_ in range(4):
                lst.append(lambda q_=q_: load_cast(w2_v[e, :, 2 * q_:2 * q_ + 2, :], (lambda t: t[:].rearrange("p (k c) -> p k c", k=2)),
                                                   RW2[wb][q_], w2e[wb].t[:, 2 * q_:2 * q_ + 2, :]))
            return lst

        for f_ in weight_pieces(0):
            f_()
        for e in range(32):
            wb = e % 2
            pending = weight_pieces(e + 1) if e + 1 < 32 else []
            for rd in range(NROUND):
                fcol = rd * 32 + e
                if 0 < rd <= NSINGLE:
                    P.cond_begin(flags, flags.t[:].rearrange("p r e -> p (r e)")[0:1, fcol:fcol + 1])
                iu = idxu[cnt["rd"] % 2]
                cw_ = cws[cnt["rd"] % 2]
                xt = XT[cnt["rd"] % 2]
                cnt["rd"] += 1
                iota = iota_t.t[:]
                bi = bank()
                for i in range(16):
                    pm = Pm[cnt["pm"] % 4]
                    cnt["pm"] += 1
                    P.op("dve", k_ts(pm.t[:], iota, rcol.t[:, rd:rd + 1], ALU.add, posm.t[:, i, e:e + 1], ALU.is_equal),
                         reads=[iota_t, rcol, posm], writes=[pm])
                    P.op("pe", k_mm([(bi.t[0:3, 0:CAP], Rm.t[:, i, e, :], pm.t[:], i == 0, i == 15)]), reads=[pm, Rm], writes=[bi])
                P.op("act", k_acp(idxr.t[:], bi.t[0:3, 0:CAP]), reads=[bi], writes=[idxr])
                bi2 = bank()
                biv = bi2.t[:, 0:NBLK * 3].rearrange("p (j c) -> p j c", c=3)
                P.op("pe", k_tr([(biv[:, j, :], idxr.t[0:3, j * 128:(j + 1) * 128], cst.t[0:3, CS_ID:CS_ID + 3]) for j in range(NBLK)]),
                     reads=[idxr, cst], writes=[bi2])
                P.op("dve", k_cp(idxf.t[:], biv), reads=[bi2], writes=[idxf])
                P.op("dve", k_ts(tmpi.t[:], idxf.t[:, :, 1], -1.0, ALU.mult, 1.0, ALU.add), reads=[idxf], writes=[tmpi])
                P.op("dve", k_stt(iu.t[:], tmpi.t[:], padrow, idxf.t[:, :, 0], ALU.mult, ALU.add), reads=[tmpi, idxf, cst], writes=[iu])
                P.op("dve", k_ts(cw_.t[:], idxf.t[:, :, 2], 1.0 / 1.702, ALU.mult), reads=[idxf], writes=[cw_])
                for j in range(NBLK):
                    xg = Xg[cnt["x"] % 3]
                    cnt["x"] += 1
                    P.dma("pool", (lambda en, xg=xg, iu=iu, j=j: en.indirect_dma_start(
                        out=xg.t[:], out_offset=None, in_=h2rows_d,
                        in_offset=bass.IndirectOffsetOnAxis(ap=iu.t[:, j:j + 1], axis=0))), xg, reads=[iu, Rh2rows])
                    b = bank()
                    bv = b.t[:].bitcast(BF16)
                    P.op("pe", k_tr([(bv[:, k * 128:(k + 1) * 128], xg.t[:, k * 128:(k + 1) * 128], identb.t[:]) for k in range(8)]),
                         reads=[xg, identb], writes=[b])
                    P.op("act", k_acp(xt.t[:, :, j * 128:(j + 1) * 128], bv[:, 0:1024].rearrange("p (k r) -> p k r", k=8)),
                         reads=[b], writes=[xt])
                for j in range(8):
                    bg = bank()
                    bu = bank()
                    P.op("pe", k_mm([(bg.t[:, 0:CAP], w1e[wb][:, k, j * 128:(j + 1) * 128], xt.t[:, k, :], k == 0, k == 7)
                                     for k in range(8)]), reads=RW1[wb] + [xt], writes=[bg])
                    P.op("pe", k_mm([(bu.t[:, 0:CAP], w1e[wb][:, k, 1024 + j * 128:1024 + (j + 1) * 128], xt.t[:, k, :], k == 0, k == 7)
                                     for k in range(8)]), reads=RW1[wb] + [xt], writes=[bu])
                    g_, u_ = ga[cnt["wk"] % 4], uu[cnt["wk"] % 4]
                    cnt["wk"] += 1
                    P.op("dve", k_ts(g_.t[:], bg.t[:, 0:CAP], b1v[:, e, j:j + 1], ALU.add, 7.0, ALU.min), reads=[bg, pc], writes=[g_])
                    P.op("act", k_act(g_.t[:], g_.t[:], AF.Silu, scale=1.702), reads=[g_], writes=[g_])
                    P.op("dve", k_ts(u_.t[:], bu.t[:, 0:CAP], b1u1.t[:, e, j:j + 1], ALU.add, 8.0, ALU.min), reads=[bu, b1u1], writes=[u_])
                    P.op("dve", k_stt(actT.t[:, j, :], u_.t[:], -6.0, g_.t[:], ALU.max, ALU.mult), reads=[u_, g_], writes=[RACT[j]])
                    if rd == 0 and pending:
                        pending.pop(0)()
                for half in range(2):
                    for j in range(NBLK):
                        b = bank()
                        P.op("pe", k_mm([(b.t[:, 0:512], actT.t[:, k, j * 128:(j + 1) * 128], w2e[wb].t[:, k, half * 512:(half + 1) * 512], k == 0, k == 7)
                                         for k in range(8)]), reads=RW2[wb] + RACT, writes=[b])
                        P.op("act", k_act(Ysb[j].t[:, half * 512:(half + 1) * 512], b.t[:, 0:512], AF.Copy, scale=cw_.t[:, j:j + 1]),
                             reads=[b, cw_], writes=[Ysb[j]])
                        if rd == 0 and pending:
                            pending.pop(0)()
                for j in range(NBLK):
                    P.dma("pool", (lambda en, j=j, iu=iu: en.indirect_dma_start(
                        out=moe_d, out_offset=bass.IndirectOffsetOnAxis(ap=iu.t[:, j:j + 1], axis=0),
                        in_=Ysb[j].t[:], in_offset=None, compute_op=ALU.add)), Rmoe, reads=[Ysb[j], iu, Rmoe0], serialize=True)
                if rd == NROUND - 1:
                    for _ in range(NSINGLE):
                        P.cond_end()
                elif rd == 0:
                    while pending:
                        pending.pop(0)()
            while pending:
                pending.pop(0)()
        P.barrier(dummy)
        s4.close()
        for k in range(8):
            P.dma("sp", k_dma(xT.t[:, k, :], xscr_d[k * 128:(k + 1) * 128, :]), Rxload, reads=[Rxscr], writes=[Rxload] + RX)

        s5 = ExitStack()
        Mt = [P.tile(f"Mt{i}", [128, 1024], F32, dma=True, stack=s5) for i in range(2)]
        xtmp = P.tile("c_xtmp", [128, 8, 128], F32, stack=s5)
        for tix in range(16):
            ts_ = slice(tix * 128, (tix + 1) * 128)
            mt = Mt[tix % 2]
            P.dma("sp", k_dma(mt.t[:], moe_d[ts_, :]), mt, reads=[Rmoe])
            if env["dbg_cols"]:
                dump(mt, mt.t[:], 1024)
            for half in range(2):
                b = bank()
                P.op("pe", k_tr([(b.t[:, kk_ * 128:(kk_ + 1) * 128], mt.t[:, (half * 4 + kk_) * 128:(half * 4 + kk_ + 1) * 128], ident)
                                 for kk_ in range(4)]), reads=[mt, cst], writes=[b])
                P.op("dve", k_tt(xtmp.t[:, half * 4:(half + 1) * 4, :], b.t[:, 0:512].rearrange("p (j t) -> p j t", j=4),
                                 mc.t[:, 7, half * 4:(half + 1) * 4].unsqueeze(2).to_broadcast([128, 4, 128]), ALU.mult),
                     reads=[b, mc], writes=[xtmp])
            P.op("pool", k_tt(xT.t[:, :, ts_], xT.t[:, :, ts_], xtmp.t[:], ALU.add), reads=[xtmp, RX[tix]], writes=[RX[tix]])
        P.barrier(dummy)
        s5.close()

        sqf = T("f_sq", [128, 8, 512], BF16)
        rsf = T("f_rs", [128, 512], F32)
        of = T("of", [128, 8, 512], F32)
        Rout = P.region("out", dma=True)
        fn = pc.t[:, PC_FN:PC_FN + 8]
        out_v = out_d.rearrange("(k p) t -> p k t", p=128)
        for g in range(4):
            gs_ = slice(g * 512, (g + 1) * 512)
            P.op("act", k_act(sqf.t[:], xT.t[:, :, gs_], AF.Square), reads=RXG(g), writes=[sqf])
            b = bank()
            P.op("pe", k_mm([(b.t[:, 0:512], onesb.t[:], sqf.t[:, k, :], k == 0, k == 7) for k in range(8)]),
                 reads=[sqf, onesb], writes=[b])
            P.op("act", k_act(rsf.t[:], b.t[:, 0:512], AF.Sqrt, scale=1.0 / 1024.0, bias=EPS), reads=[b], writes=[rsf])
            P.op("dve", k_rcp(rsf.t[:], rsf.t[:]), reads=[rsf], writes=[rsf])
            P.op("dve", k_tt(of.t[:], xT.t[:, :, gs_], rsf.t[:].unsqueeze(1).to_broadcast([128, 8, 512]), ALU.mult),
                 reads=RXG(g) + [rsf], writes=[of])
            P.op("pool", k_tt(of.t[:], of.t[:], fn.unsqueeze(2).to_broadcast([128, 8, 512]), ALU.mult), reads=[of, pc], writes=[of])
            P.dma("sp", k_dma(out_v[:, :, gs_], of.t[:]), Rout, reads=[of], writes=[Rout])


_CACHE = {}


def make_in_maps(x, c, ctx, c_ctx, w_mod, b_mod, norm1, w_in, sgu_ln, sgu_w, sgu_b, lb_fwd, lb_bwd,
                 hgrn_norm, w_out, norm2, router_w, router_b, w1, b1, w2, b2, final_norm):
    f = lambda a: np.ascontiguousarray(np.asarray(a, dtype=np.float32))
    cst, masks, sel = _consts()
    pcol = np.zeros((NCORES, 128, NPC), np.float32)
    cc = col_layout(f(c_ctx))
    for b in range(NCORES):
        cb = col_layout(f(c[b]))
        pcol[b, :, PC_C:PC_C + 16:2] = cb
        pcol[b, :, PC_C + 1:PC_C + 16:2] = cc
    pcol[:, :, PC_BMOD:PC_BMOD + 48] = col_layout(f(b_mod[0]))
    pcol[:, :, PC_N1:PC_N1 + 8] = col_layout(f(norm1[0]))
    pcol[:, :, PC_N2:PC_N2 + 8] = col_layout(f(norm2[0]))
    pcol[:, :, PC_FN:PC_FN + 8] = col_layout(f(final_norm))
    pcol[:, :, PC_LNG:PC_LNG + 4] = f(sgu_ln[0]).T
    b1c = f(b1[0]).reshape(32, 16, 128).transpose(2, 0, 1).reshape(128, 512)
    pcol[:, :, PC_B1:PC_B1 + 512] = b1c
    prow = np.concatenate([f(lb_fwd[0]), f(lb_fwd[1]), f(lb_bwd[0]), f(lb_bwd[1]), f(hgrn_norm[0]),
                           f(sgu_b[0]).reshape(-1)])[None, :]
    wsT = np.ascontiguousarray(f(sgu_w[0]).transpose(2, 0, 1).reshape(128, 512))
    shared = {
        "prow": f(prow), "w_mod": f(w_mod[0]), "w_in": f(w_in[0]), "w_out": f(w_out[0]),
        "router_w": f(router_w[0]), "router_b": f(router_b[0]).reshape(32, 1), "w1": f(w1[0]), "w2": f(w2[0]),
        "b2": f(b2[0]), "sgu_wT": wsT, "cst": cst, "masks": masks, "sel": sel,
    }
    maps = []
    for b in range(NCORES):
        m = dict(shared)
        m["xT"] = np.ascontiguousarray(f(x[b]).T)
        m["ctxT"] = np.ascontiguousarray(f(ctx[b]).T)
        m["pcol"] = pcol[b]
        maps.append(m)
    return maps


def kernel(**inputs):
    nc = build_program()
    maps = make_in_maps(**inputs)
    res = run_bass_kernel_spmd(nc, maps, core_ids=list(range(NCORES)))
    out = np.stack([np.ascontiguousarray(res.results[b]["outT"].T) for b in range(NCORES)], axis=0)
    return out.astype(np.float32)
```
